# Optimizing a Trainium2 kernel written in Bass

```python
import jax, jax.numpy as jnp
from jax import lax
import numpy as np

D_MODEL = 4096
BATCH = 1
SEQ = 8192
DEPTH = 2

HEAD_DIM = 128
N_HEADS = D_MODEL // HEAD_DIM
N_HEADS_A = 3 * N_HEADS // 8
N_HEADS_B = 3 * N_HEADS // 8
N_HEADS_C = N_HEADS - N_HEADS_A - N_HEADS_B
N_KV_A = 2
IDX_HEADS = 16
IDX_DIM = 64
IDX_TOPK_MAX = 256
CHUNK = 64
N_LEFT_CHUNKS = 8
BAND = (N_LEFT_CHUNKS + 1) * CHUNK
REL_CLIP = 256
Q_BLOCK = 128
ROPE_THETA = 10000.0
D_FF = ((8 * D_MODEL + 3 * 256 - 1) // (3 * 256)) * 256
EPS = 1e-6
PROJ_SIZES = (
    N_HEADS_A * HEAD_DIM,
    N_KV_A * HEAD_DIM,
    N_KV_A * HEAD_DIM,
    IDX_HEADS * IDX_DIM,
    IDX_DIM,
    IDX_HEADS,
    N_HEADS_B * HEAD_DIM,
    N_HEADS_B * HEAD_DIM,
    N_HEADS_B * HEAD_DIM,
    N_HEADS_C * HEAD_DIM,
    N_HEADS_C * HEAD_DIM,
    N_HEADS_C * HEAD_DIM,
)
PROJ_WIDTH = sum(PROJ_SIZES)

kernel_name = "hybrid_dsa_stickbreak_chunkband_adaln"


def rms_norm(x, g):
    xf = x.astype(jnp.float32)
    y = xf * lax.rsqrt(jnp.mean(xf * xf, axis=-1, keepdims=True) + EPS)
    return (y * g.astype(jnp.float32)).astype(x.dtype)


def rope(x, pos_f):
    d = x.shape[-1]
    inv = ROPE_THETA ** (-jnp.arange(0, d, 2, dtype=jnp.float32) / d)
    ang = pos_f[:, None] * inv[None, :]
    cos = jnp.cos(ang)[None, :, None, :].astype(x.dtype)
    sin = jnp.sin(ang)[None, :, None, :].astype(x.dtype)
    x1, x2 = jnp.split(x, 2, axis=-1)
    return jnp.concatenate([x1 * cos - x2 * sin, x2 * cos + x1 * sin], axis=-1)


def to_blocks(a):
    b, s = a.shape[:2]
    return jnp.moveaxis(a.reshape(b, s // Q_BLOCK, Q_BLOCK, *a.shape[2:]), 1, 0)


def from_blocks(a):
    nb, b, q = a.shape[:3]
    return jnp.moveaxis(a, 0, 1).reshape(b, nb * q, *a.shape[3:])


def dsa_mixer(q, k, v, iq, ik, iw, pos):
    b, s = q.shape[:2]
    topk = min(IDX_TOPK_MAX, s // 4)
    chunk_id = pos // CHUNK
    rep = N_HEADS_A // N_KV_A
    idx_scale = (IDX_HEADS ** -0.5) * (IDX_DIM ** -0.5)
    attn_scale = HEAD_DIM ** -0.5
    gather = jax.vmap(lambda a, i: a[i])

    def block(args):
        qb, iqb, iwb, start = args
        q_chunk = (start + jnp.arange(Q_BLOCK, dtype=jnp.int32)) // CHUNK
        admissible = chunk_id[None, :] <= q_chunk[:, None]
        dots = jnp.einsum('bqhe,bse->bqhs', iqb, ik).astype(jnp.float32)
        score = jnp.einsum('bqh,bqhs->bqs', iwb.astype(jnp.float32), jax.nn.relu(dots)) * idx_scale
        score = jnp.where(admissible[None], score, -jnp.inf)
        _, sel = lax.top_k(score, topk)
        valid = chunk_id[sel] <= q_chunk[None, :, None]
        ks = gather(k, sel)
        vs = gather(v, sel)
        qg = qb.reshape(b, Q_BLOCK, N_KV_A, rep, HEAD_DIM)
        sc = jnp.einsum('bqgrd,bqkgd->bqgrk', qg, ks).astype(jnp.float32) * attn_scale
        sc = jnp.where(valid[:, :, None, None, :], sc, -jnp.inf)
        p = jax.nn.softmax(sc, axis=-1).astype(vs.dtype)
        o = jnp.einsum('bqgrk,bqkgd->bqgrd', p, vs)
        return o.reshape(b, Q_BLOCK, N_HEADS_A, HEAD_DIM)

    starts = jnp.arange(s // Q_BLOCK, dtype=jnp.int32) * Q_BLOCK
    out = lax.map(block, (to_blocks(q), to_blocks(iq), to_blocks(iw), starts))
    return from_blocks(out)


def stick_breaking_mixer(q, k, v, pos):
    s = q.shape[1]
    scale = HEAD_DIM ** -0.5

    def block(args):
        qb, start = args
        tq = start + jnp.arange(Q_BLOCK, dtype=jnp.int32)
        causal = pos[None, :] < tq[:, None]
        z = jnp.einsum('bqhd,bshd->bhqs', qb, k).astype(jnp.float32) * scale
        log_keep = jnp.where(causal, jax.nn.log_sigmoid(-z), 0.0)
        between = lax.cumsum(log_keep, axis=3, reverse=True) - log_keep
        log_a = jax.nn.log_sigmoid(z) + between
        a = jnp.where(causal, jnp.exp(log_a), 0.0).astype(v.dtype)
        return jnp.einsum('bhqs,bshd->bqhd', a, v)

    starts = jnp.arange(s // Q_BLOCK, dtype=jnp.int32) * Q_BLOCK
    return from_blocks(lax.map(block, (to_blocks(q), starts)))


def chunk_band_mixer(q, k, v, rel_bias):
    b, s, h, d = q.shape
    n = s // CHUNK
    qc = q.reshape(b, n, CHUNK, h, d)

    def band(a):
        a = jnp.pad(a.reshape(b, n, CHUNK, h, d), ((0, 0), (N_LEFT_CHUNKS, 0), (0, 0), (0, 0), (0, 0)))
        return jnp.concatenate([a[:, o:o + n] for o in range(N_LEFT_CHUNKS + 1)], axis=2)

    kb, vb = band(k), band(v)
    slot = np.arange(BAND) // CHUNK
    kj = np.arange(BAND) % CHUNK
    qi = np.arange(CHUNK)
    dist = (N_LEFT_CHUNKS - slot)[None, :] * CHUNK + qi[:, None] - kj[None, :]
    rel_idx = np.clip(dist, -REL_CLIP, REL_CLIP) + REL_CLIP
    bias = rel_bias[:, rel_idx].astype(jnp.float32)
    valid = (np.arange(n)[:, None] - N_LEFT_CHUNKS + slot[None, :]) >= 0
    sc = jnp.einsum('bnqhd,bnkhd->bnhqk', qc, kb).astype(jnp.float32) * (d ** -0.5) + bias[None, None]
    sc = jnp.where(valid[None, :, None, None, :], sc, -jnp.inf)
    p = jax.nn.softmax(sc, axis=-1).astype(vb.dtype)
    o = jnp.einsum('bnhqk,bnkhd->bnqhd', p, vb)
    return o.reshape(b, s, h, d)


def setup_inputs(seed: int = 0) -> dict:
    key = jax.random.key(seed)
    ks = jax.random.split(key, 13)
    f32 = jnp.float32
    nrm = lambda k, shape, sc: jax.random.normal(k, shape, f32) * sc
    return {
        "x": nrm(ks[0], (BATCH, SEQ, D_MODEL), 1.0),
        "c": nrm(ks[1], (BATCH, D_MODEL), 1.0),
        "w_ada": nrm(ks[2], (DEPTH, D_MODEL, 6 * D_MODEL), 0.5 * D_MODEL ** -0.5),
        "b_ada": nrm(ks[3], (DEPTH, 6 * D_MODEL), 0.02),
        "norm_attn_g": 1.0 + nrm(ks[4], (DEPTH, D_MODEL), 0.02),
        "w_in": nrm(ks[5], (DEPTH, D_MODEL, PROJ_WIDTH), D_MODEL ** -0.5),
        "rel_bias": nrm(ks[6], (DEPTH, N_HEADS_C, 2 * REL_CLIP + 1), 0.1),
        "head_norm_g": 1.0 + nrm(ks[7], (DEPTH, N_HEADS * HEAD_DIM), 0.02),
        "w_out": nrm(ks[8], (DEPTH, N_HEADS * HEAD_DIM, D_MODEL), (N_HEADS * HEAD_DIM) ** -0.5),
        "norm_ffn_g": 1.0 + nrm(ks[9], (DEPTH, D_MODEL), 0.02),
        "w_gate_up": nrm(ks[10], (DEPTH, D_MODEL, 2 * D_FF), D_MODEL ** -0.5),
        "w_down": nrm(ks[11], (DEPTH, D_FF, D_MODEL), D_FF ** -0.5),
        "final_norm_g": 1.0 + nrm(ks[12], (D_MODEL,), 0.02),
    }


def reference(x, c, w_ada, b_ada, norm_attn_g, w_in, rel_bias, head_norm_g, w_out,
              norm_ffn_g, w_gate_up, w_down, final_norm_g):
    b, s, _ = x.shape
    pos = jnp.arange(s, dtype=jnp.int32)
    pos_f = pos.astype(jnp.float32)
    cond = jax.nn.silu(c)
    offsets = np.cumsum(np.array(PROJ_SIZES))[:-1].tolist()
    for l in range(DEPTH):
        mod = (cond @ w_ada[l] + b_ada[l])[:, None, :]
        sh1, sc1, g1, sh2, sc2, g2 = jnp.split(mod, 6, axis=-1)

        h = rms_norm(x, norm_attn_g[l]) * (1.0 + sc1) + sh1
        proj = h @ w_in[l]
        qa, ka, va, iq, ik, iw, qb, kb, vb, qc, kc, vc = jnp.split(proj, offsets, axis=-1)
        heads = lambda t, n, d: t.reshape(b, s, n, d)
        qa = rope(heads(qa, N_HEADS_A, HEAD_DIM), pos_f)
        ka = rope(heads(ka, N_KV_A, HEAD_DIM), pos_f)
        va = heads(va, N_KV_A, HEAD_DIM)
        iq = rope(heads(iq, IDX_HEADS, IDX_DIM), pos_f)
        ik = rope(heads(ik, 1, IDX_DIM), pos_f)[:, :, 0, :]
        o_a = dsa_mixer(qa, ka, va, iq, ik, iw, pos)
        o_b = stick_breaking_mixer(heads(qb, N_HEADS_B, HEAD_DIM), heads(kb, N_HEADS_B, HEAD_DIM),
                                   heads(vb, N_HEADS_B, HEAD_DIM), pos)
        o_c = chunk_band_mixer(heads(qc, N_HEADS_C, HEAD_DIM), heads(kc, N_HEADS_C, HEAD_DIM),
                               heads(vc, N_HEADS_C, HEAD_DIM), rel_bias[l])
        mix = jnp.concatenate([o_a.astype(x.dtype), o_b.astype(x.dtype), o_c.astype(x.dtype)], axis=2)
        mix = rms_norm(mix, head_norm_g[l].reshape(N_HEADS, HEAD_DIM)).reshape(b, s, N_HEADS * HEAD_DIM)
        x = x + g1 * (mix @ w_out[l])

        h = rms_norm(x, norm_ffn_g[l]) * (1.0 + sc2) + sh2
        gate, up = jnp.split(h @ w_gate_up[l], 2, axis=-1)
        x = x + g2 * ((jax.nn.silu(gate) * up) @ w_down[l])
    return rms_norm(x, final_norm_g)
```

```python
import contextlib

import numpy as np
import concourse.bass as bass
import concourse.mybir as mybir
from concourse.bass_utils import run_bass_kernel_spmd

F32 = mybir.dt.float32
BF16 = mybir.dt.bfloat16
AF = mybir.ActivationFunctionType
ALU = mybir.AluOpType
AX = mybir.AxisListType

ENGS = ("pe", "act", "dve", "pool", "sp")


class Op:
    __slots__ = ("eng", "fn", "deps", "idx", "has_dep", "tok", "is_dma", "dsem")

    def __init__(self, eng, fn, deps, is_dma):
        self.eng = eng
        self.fn = fn
        self.deps = deps
        self.has_dep = False
        self.tok = None
        self.is_dma = is_dma
        self.dsem = None


class Prog:
    def __init__(self, nc, n_dma_sems=12):
        self.nc = nc
        self.ops = []
        self.last_writer = {}
        self.readers = {}
        self.n_dma_sems = n_dma_sems

    def op(self, eng, fn, reads=(), writes=(), dma=False):
        deps = []
        for b in reads:
            w = self.last_writer.get(b)
            if w is not None:
                deps.append(w)
        for b in writes:
            w = self.last_writer.get(b)
            if w is not None:
                deps.append(w)
            deps.extend(self.readers.get(b, ()))
        o = Op(eng, fn, deps, dma)
        for d in deps:
            d.has_dep = True
        for b in writes:
            self.last_writer[b] = o
            self.readers[b] = []
        for b in reads:
            self.readers.setdefault(b, []).append(o)
        self.ops.append(o)
        return o

    def dma(self, eng, out, in_, reads=(), writes=()):
        return self.op(eng, lambda e: e.dma_start(out=out, in_=in_), reads, writes, dma=True)

    def emit(self, final_wait_ops=()):
        nc = self.nc
        import contextlib
        fw_set = set(id(o) for o in final_wait_ops)
        with contextlib.ExitStack() as st:
            esem = {e: st.enter_context(nc.semaphore("s_" + e)) for e in ENGS}
            dsems = {e: [st.enter_context(nc.semaphore("d_%s%d" % (e, i))) for i in range(self.n_dma_sems)]
                     for e in ("sp", "pool", "act")}
            ecount = {e: 0 for e in ENGS}
            dcount = {e: [0] * self.n_dma_sems for e in dsems}
            drr = {e: 0 for e in dsems}
            per_eng = {e: [] for e in ENGS}
            for o in self.ops:
                per_eng[o.eng].append(o)
                if o.is_dma:
                    i = drr[o.eng] % self.n_dma_sems
                    drr[o.eng] += 1
                    prev = dcount[o.eng][i]
                    dcount[o.eng][i] += 16
                    o.dsem = (dsems[o.eng][i], prev)
                    o.tok = (dsems[o.eng][i], dcount[o.eng][i])
                else:
                    if o.has_dep or id(o) in fw_set:
                        ecount[o.eng] += 1
                        o.tok = (esem[o.eng], ecount[o.eng])
            block = st.enter_context(nc.Block())
            handles = {"pe": block.tensor, "act": block.scalar, "dve": block.vector,
                       "pool": block.gpsimd, "sp": block.sync}

            def make(ename):
                ops = per_eng[ename]

                def body(eng):
                    waited = {}
                    for o in ops:
                        need = {}
                        for d in o.deps:
                            if d.eng == "pe" and ename == "pe" and not d.is_dma:
                                continue
                            s, v = d.tok
                            k = id(s)
                            if waited.get(k, 0) >= v:
                                continue
                            if k not in need or need[k][1] < v:
                                need[k] = (s, v)
                        if o.is_dma:
                            s, prev = o.dsem
                            k = id(s)
                            if prev > 0 and waited.get(k, 0) < prev:
                                if k not in need or need[k][1] < prev:
                                    need[k] = (s, prev)
                        for k, (s, v) in need.items():
                            eng.wait_ge(s, v)
                            waited[k] = v
                        ins = o.fn(eng)
                        if o.tok is not None:
                            s, v = o.tok
                            ins.then_inc(s, 16 if o.is_dma else 1)
                    if ename == "sp":
                        for o in final_wait_ops:
                            s, v = o.tok
                            eng.wait_ge(s, v)
                return body

            for e in ENGS:
                if per_eng[e] or e == "sp":
                    handles[e](make(e))

import contextlib
import numpy as np

D = 4096
S = 8192
EPS = 1e-6


def build_norm(final=False, T=1024):
    nc = bass.Bass("TRN2", target_bir_lowering=False)
    xT = nc.dram_tensor("xT", [D, T], F32, kind="ExternalInput").ap()
    gv = nc.dram_tensor("gv", [128, 32], F32, kind="ExternalInput").ap()
    scv = nc.dram_tensor("scv", [128, 32], F32, kind="ExternalInput").ap()
    shv = nc.dram_tensor("shv", [128, 32], F32, kind="ExternalInput").ap()
    odt = F32 if final else BF16
    hT = nc.dram_tensor("hT", [D, T], odt, kind="ExternalOutput").ap()
    xv = xT.rearrange("(kc p) t -> p kc t", p=128)
    hv = hT.rearrange("(kc p) t -> p kc t", p=128)
    with contextlib.ExitStack() as st:
        sb = lambda n, s, d: st.enter_context(nc.sbuf_tensor(n, s, d))
        g_sb = sb("g_sb", [128, 32], F32)
        sc_sb = sb("sc_sb", [128, 32], F32)
        sh_sb = sb("sh_sb", [128, 32], F32)
        a_sb = sb("a_sb", [128, 32], F32)
        ones = sb("ones", [128, 128], F32)
        xs = sb("xs", [128, 32, 512], F32)
        sq = [sb("sq%d" % i, [128, 512], F32) for i in range(2)]
        rstd = sb("rstd", [128, 512], F32)
        tmp = [sb("tmp%d" % i, [128, 512], F32) for i in range(2)]
        ho = [sb("ho%d" % i, [128, 8, 512], odt) for i in range(2)]
        ps = st.enter_context(nc.psum_tensor("ps", [128, 512], F32))
        P = Prog(nc)
        P.dma("sp", g_sb[:], gv, writes=["g"])
        P.dma("sp", sc_sb[:], scv, writes=["sc"])
        P.dma("sp", sh_sb[:], shv, writes=["sh"])
        P.op("dve", lambda e: e.memset(ones[:], 1.0), writes=["ones"])
        P.op("dve", lambda e: e.scalar_tensor_tensor(out=a_sb[:], in0=sc_sb[:], scalar=1.0, in1=g_sb[:],
                                                     op0=ALU.add, op1=ALU.mult),
             reads=["sc", "g"], writes=["a"])
        outs = []
        for half in range(T // 512):
            t0 = half * 512
            for q in range(4):
                P.dma("sp", xs[:, q * 8:(q + 1) * 8, :], xv[:, q * 8:(q + 1) * 8, t0:t0 + 512], writes=[("xs", q)])
            for kc in range(32):
                s = kc % 2
                P.op("act", lambda e, kc=kc, s=s: e.activation(out=sq[s][:], in_=xs[:, kc, :], func=AF.Square),
                     reads=[("xs", kc // 8)], writes=[("sq", s)])
                P.op("pe", lambda e, kc=kc, s=s: e.matmul(ps[:], lhsT=ones[:], rhs=sq[s][:], start=(kc == 0), stop=(kc == 31)),
                     reads=[("sq", s), "ones"], writes=["ps"])
            P.op("dve", lambda e: e.tensor_scalar(out=rstd[:], in0=ps[:], scalar1=1.0 / D, scalar2=EPS,
                                                  op0=ALU.mult, op1=ALU.add), reads=["ps"], writes=["rstd"])
            P.op("act", lambda e: e.activation(out=rstd[:], in_=rstd[:], func=AF.Sqrt), reads=["rstd"], writes=["rstd"])
            P.op("dve", lambda e: e.reciprocal(out=rstd[:], in_=rstd[:]), reads=["rstd"], writes=["rstd"])
            for kc in range(32):
                s = kc % 2
                hs = (kc // 8) % 2
                P.op("dve", lambda e, kc=kc, s=s: e.tensor_tensor(out=tmp[s][:], in0=xs[:, kc, :], in1=rstd[:], op=ALU.mult),
                     reads=[("xs", kc // 8), "rstd"], writes=[("tmp", s)])
                P.op("act", lambda e, kc=kc, s=s, hs=hs: e.activation(out=ho[hs][:, kc % 8, :], in_=tmp[s][:], func=AF.Identity,
                                                                       bias=sh_sb[:, kc:kc + 1], scale=a_sb[:, kc:kc + 1]),
                     reads=[("tmp", s), "a", "sh"], writes=[("ho", hs, kc % 8)])
                if kc % 8 == 7:
                    q = kc // 8
                    outs.append(P.dma("sp", hv[:, q * 8:(q + 1) * 8, t0:t0 + 512], ho[hs][:],
                                      reads=[("ho", hs, i) for i in range(8)], writes=[("hv", half, q)]))
        P.emit(final_wait_ops=outs)
    return nc


def build_gemm(mode, K, ncols, T=S):
    KC = K // 128
    nc = bass.Bass("TRN2", target_bir_lowering=False)
    wcols = 2 * ncols if mode == "glu" else ncols
    aT = nc.dram_tensor("aT", [K, T], BF16, kind="ExternalInput").ap()
    W = nc.dram_tensor("W", [K, wcols], F32, kind="ExternalInput").ap()
    av = aT.rearrange("(kc p) t -> p kc t", p=128)
    wv = W.rearrange("(kc p) n -> p kc n", p=128)
    if mode == "res":
        xT = nc.dram_tensor("xT", [ncols, T], F32, kind="ExternalInput").ap()
        gvec = nc.dram_tensor("gvec", [128, (ncols + 127) // 128], F32, kind="ExternalInput").ap()
        yT = nc.dram_tensor("yT", [ncols, T], F32, kind="ExternalOutput").ap()
    else:
        yT = nc.dram_tensor("yT", [ncols, T], BF16, kind="ExternalOutput").ap()
    blocks = [(c0, min(128, ncols - c0)) for c0 in range(0, ncols, 128)]
    gsz = 2 if mode == "glu" else 4
    groups = [blocks[i:i + gsz] for i in range(0, len(blocks), gsz)]
    KSUB = 16 if KC <= 32 else 8
    nsub = (KC + KSUB - 1) // KSUB
    NAB = 4
    with contextlib.ExitStack() as st:
        sb = lambda n, s, d: st.enter_context(nc.sbuf_tensor(n, s, d))
        nwb = 2 if len(groups) > 1 else 1
        wb = [sb("wb%d" % i, [128, KC, 512], BF16) for i in range(nwb)]
        ab = [sb("ab%d" % i, [128, KSUB, 512], BF16) for i in range(NAB)]
        pss = [st.enter_context(nc.psum_tensor("ps%d" % i, [128, 512], F32)) for i in range(8)]
        if mode == "res":
            g_sb = sb("g_sb", [128, (ncols + 127) // 128], F32)
            xt = [sb("xt%d" % i, [128, 512], F32) for i in range(16)]
            yo = [sb("yo%d" % i, [128, 512], F32) for i in range(4)]
        elif mode == "glu":
            sg = [sb("sg%d" % i, [128, 512], F32) for i in range(2)]
            yo = [sb("yo%d" % i, [128, 512], BF16) for i in range(4)]
        else:
            yo = [sb("yo%d" % i, [128, 512], BF16) for i in range(4)]
        P = Prog(nc)
        if mode == "res":
            P.dma("sp", g_sb[:], gvec, writes=["g"])
        outs = []
        units = []
        for gi, grp in enumerate(groups):
            for tb in range(T // 512):
                for su in range(nsub):
                    units.append((gi, tb, su))
        ginfo = {}
        for gi, grp in enumerate(groups):
            wl = []
            off = 0
            for (c0, cw) in grp:
                wl.append((c0, cw, off)); off += cw
            if mode == "glu":
                for (c0, cw) in grp:
                    wl.append((ncols + c0, cw, off)); off += cw
            ginfo[gi] = wl
        PFU = NAB - 1
        yon = [0]
        xtn = [0]
        xt_slot = {}

        def issue_load(u):
            gi, tb, su = units[u]
            t0 = tb * 512
            if su == 0 and tb == 0:
                ws = gi % nwb
                for (wc0, cw, o) in ginfo[gi]:
                    for q in range(0, KC, 8):
                        q1 = min(KC, q + 8)
                        P.dma("pool", wb[ws][:, q:q1, o:o + cw], wv[:, q:q1, wc0:wc0 + cw], writes=[("wb", ws, o, q)])
            k0 = su * KSUB
            k1 = min(KC, k0 + KSUB)
            s = u % NAB
            P.dma("sp", ab[s][:, 0:k1 - k0, :], av[:, k0:k1, t0:t0 + 512], writes=[("ab", s)])
            if mode == "res" and su == 0:
                for bi, (c0, cw) in enumerate(groups[gi]):
                    xs_ = xtn[0] % 16
                    xtn[0] += 1
                    xt_slot[(gi, tb, bi)] = xs_
                    P.dma("sp", xt[xs_][0:cw, :], xT[c0:c0 + cw, t0:t0 + 512], writes=[("xt", xs_)])

        for u in range(min(PFU, len(units))):
            issue_load(u)
        for u, (gi, tb, su) in enumerate(units):
            if u + PFU < len(units):
                issue_load(u + PFU)
            grp = groups[gi]
            wl = ginfo[gi]
            ws = gi % nwb
            t0 = tb * 512
            pset = (gi * (T // 512) + tb) % 2
            banks = [pss[pset * 4 + i] for i in range(len(wl))]
            bkeys = [("ps", pset * 4 + i) for i in range(len(wl))]
            wreads = [("wb", ws, o, q) for (_, _, o) in wl for q in range(0, KC, 8)]
            k0 = su * KSUB
            k1 = min(KC, k0 + KSUB)
            s = u % NAB

            def mm(e, s=s, k0=k0, k1=k1, banks=banks, wl=wl, ws=ws):
                ins = None
                for bi, (wc0, cw, o) in enumerate(wl):
                    for kc in range(k0, k1):
                        ins = e.matmul(banks[bi][0:cw, :], lhsT=wb[ws][:, kc, o:o + cw], rhs=ab[s][:, kc - k0, :],
                                       start=(kc == 0), stop=(kc == KC - 1))
                return ins
            P.op("pe", mm, reads=[("ab", s)] + wreads, writes=bkeys)
            if su != nsub - 1:
                continue
            nb = len(grp)
            for bi, (c0, cw) in enumerate(grp):
                ys = yon[0] % 4
                yon[0] += 1
                if mode == "raw":
                    P.op("act", lambda e, bi=bi, cw=cw, ys=ys, banks=banks: e.activation(out=yo[ys][0:cw, :], in_=banks[bi][0:cw, :], func=AF.Copy),
                         reads=[bkeys[bi]], writes=[("yo", ys)])
                elif mode == "glu":
                    s2 = bi % 2
                    P.op("act", lambda e, bi=bi, cw=cw, s2=s2, banks=banks: e.activation(out=sg[s2][0:cw, :], in_=banks[bi][0:cw, :], func=AF.Silu),
                         reads=[bkeys[bi]], writes=[("sg", s2)])
                    P.op("dve", lambda e, bi=bi, cw=cw, s2=s2, ys=ys, banks=banks, nb=nb: e.tensor_tensor(out=yo[ys][0:cw, :], in0=banks[nb + bi][0:cw, :], in1=sg[s2][0:cw, :], op=ALU.mult),
                         reads=[bkeys[nb + bi], ("sg", s2)], writes=[("yo", ys)])
                else:
                    cb = c0 // 128
                    xs_ = xt_slot[(gi, tb, bi)]
                    P.op("dve", lambda e, bi=bi, cw=cw, ys=ys, cb=cb, banks=banks, xs_=xs_: e.scalar_tensor_tensor(
                        out=yo[ys][0:cw, :], in0=banks[bi][0:cw, :], scalar=g_sb[0:cw, cb:cb + 1], in1=xt[xs_][0:cw, :],
                        op0=ALU.mult, op1=ALU.add),
                        reads=[bkeys[bi], ("xt", xs_), "g"], writes=[("yo", ys)])
                outs.append(P.dma("sp", yT[c0:c0 + cw, t0:t0 + 512], yo[ys][0:cw, :], reads=[("yo", ys)],
                                  writes=[("y", c0, tb)]))
        P.emit(final_wait_ops=outs)
    return nc

D = 4096
NCOL = 3072


def build_k0():
    nc = bass.Bass("TRN2", target_bir_lowering=False)
    cT = nc.dram_tensor("cT", [128, 32], F32, kind="ExternalInput").ap()
    w = nc.dram_tensor("w", [2 * D, NCOL], F32, kind="ExternalInput").ap()
    b = nc.dram_tensor("b", [1, 2 * NCOL], F32, kind="ExternalInput").ap()
    y = nc.dram_tensor("y", [1, 2 * NCOL], F32, kind="ExternalOutput").ap()
    import contextlib
    with contextlib.ExitStack() as st:
        sb = lambda n, s, d: st.enter_context(nc.sbuf_tensor(n, s, d))
        cs = sb("cs", [128, 32], F32)
        cs2 = sb("cs2", [128, 32], F32)
        bsb = sb("bsb", [1, 2 * NCOL], F32)
        ysb = sb("ysb", [1, 2 * NCOL], F32)
        wt = [sb("wt%d" % i, [128, 32, 512], F32) for i in range(2)]
        ps = [st.enter_context(nc.psum_tensor("ps%d" % i, [128, 512], F32)) for i in range(2)]
        P = Prog(nc)
        P.dma("sp", cs[:], cT, writes=["cs"])
        P.dma("sp", bsb[:], b, writes=["bsb"])
        P.op("act", lambda e: e.activation(out=cs2[:], in_=cs[:], func=AF.Silu), reads=["cs"], writes=["cs2"])
        nb = 0
        for l in range(2):
            wl = w[l * D:(l + 1) * D, :].rearrange("(kc p) n -> p kc n", p=128)
            for j in range(NCOL // 512):
                s = nb % 2
                for q in range(4):
                    P.dma("sp", wt[s][:, q * 8:(q + 1) * 8, :], wl[:, q * 8:(q + 1) * 8, j * 512:(j + 1) * 512],
                          writes=[("wt", s, q)])

                def mm(e, s=s):
                    for kc in range(32):
                        ins = e.matmul(ps[s][0:1, :], lhsT=cs2[:, kc:kc + 1], rhs=wt[s][:, kc, :],
                                       start=(kc == 0), stop=(kc == 31))
                    return ins
                P.op("pe", mm, reads=["cs2"] + [("wt", s, q) for q in range(4)], writes=[("ps", s)])
                o0 = l * NCOL + j * 512
                P.op("dve", lambda e, s=s, o0=o0: e.tensor_tensor(out=ysb[0:1, o0:o0 + 512], in0=ps[s][0:1, :],
                                                                   in1=bsb[0:1, o0:o0 + 512], op=ALU.add),
                     reads=[("ps", s), "bsb"], writes=[("ysb", nb)])
                nb += 1
        fin = P.dma("sp", y, ysb[:], reads=[("ysb", i) for i in range(nb)], writes=["y"])
        P.emit(final_wait_ops=[fin])
    return nc


import contextlib
import numpy as np

S = 8192
HD = 128
SCALE = HD ** -0.5
EPS = 1e-6
KPAD = S + 512


def build_sb(NU=3, NSLOT=32):
    nc = bass.Bass("TRN2", target_bir_lowering=False)
    qT = nc.dram_tensor("qT", [NU * 128, NSLOT * 128], BF16, kind="ExternalInput").ap()
    kT = nc.dram_tensor("kT", [NU * 128, KPAD], BF16, kind="ExternalInput").ap()
    vv = nc.dram_tensor("vv", [NU * KPAD, 128], BF16, kind="ExternalInput").ap()
    hg = nc.dram_tensor("hg", [NU * 128, 128], F32, kind="ExternalInput").ap()
    cst = nc.dram_tensor("cst", [128, 3 * 128], F32, kind="ExternalInput").ap()
    oo = nc.dram_tensor("oo", [NU * NSLOT * 128, 128], BF16, kind="ExternalOutput").ap()
    with contextlib.ExitStack() as st:
        sb = lambda n, s, d: st.enter_context(nc.sbuf_tensor(n, s, d))
        csb = sb("csb", [128, 384], F32)
        ident = sb("ident", [128, 128], BF16)
        zeros = sb("zeros", [128, 512], F32)
        q_sb = [sb("q%d" % i, [128, NSLOT * 128], BF16) for i in range(NU)]
        k_sb = [sb("k%d" % i, [128, KPAD], BF16) for i in range(NU)]
        v_sb = [sb("v%d" % i, [128, KPAD // 128, 128], BF16) for i in range(NU)]
        hg_sb = [sb("hg%d" % i, [128, 128], F32) for i in range(NU)]
        NS = 3
        NC4 = 4
        E = [sb("E%d" % i, [128, 512], F32) for i in range(NS)]
        SP = [sb("SP%d" % i, [128, 512], F32) for i in range(NS)]
        Cb = [sb("C%d" % i, [128, 512], F32) for i in range(NC4)]
        ARG = [sb("ARG%d" % i, [128, 512], F32) for i in range(NS)]
        A = [sb("A%d" % i, [128, 512], BF16) for i in range(NS)]
        AT = [sb("AT%d" % i, [128, 512], BF16) for i in range(NS)]
        junk = sb("junk", [128, 128], F32)
        ss = [sb("ss%d" % i, [128, 1], F32) for i in range(NU)]
        OO = [sb("OO%d" % i, [128, 128], BF16) for i in range(NU)]
        zb = [st.enter_context(nc.psum_tensor("zb%d" % i, [128, 512], F32)) for i in range(4)]
        tp = [st.enter_context(nc.psum_tensor("tp%d" % i, [128, 512], BF16)) for i in range(2)]
        ob = [st.enter_context(nc.psum_tensor("ob%d" % i, [128, 128], F32)) for i in range(2)]
        Mk = csb[:, 0:128]
        Mneg = csb[:, 128:256]
        P = Prog(nc)
        P.dma("sp", csb[:], cst, writes=["csb"])
        P.op("dve", lambda e: e.tensor_copy(out=ident[:], in_=csb[:, 256:384]), reads=["csb"], writes=["ident"])
        P.op("dve", lambda e: e.memset(zeros[:], 0.0), writes=["zeros"])
        outs = []
        for u in range(NU):
            P.dma("sp", q_sb[u][:], qT[u * 128:(u + 1) * 128, :], writes=[("q", u)])
            for h in range(2):
                c0 = h * (KPAD // 2)
                P.dma("sp", k_sb[u][:, c0:c0 + KPAD // 2], kT[u * 128:(u + 1) * 128, c0:c0 + KPAD // 2], writes=[("k", u, h)])
                nt = KPAD // 128 // 2
                P.dma("sp", v_sb[u][:, h * nt:(h + 1) * nt, :],
                      vv[u * KPAD + h * nt * 128:u * KPAD + (h + 1) * nt * 128, :].rearrange("(t p) d -> p t d", p=128),
                      writes=[("v", u, h)])
            P.dma("sp", hg_sb[u][:], hg[u * 128:(u + 1) * 128, :], writes=[("hg", u)])
        items = []
        last = {}
        def add(u, p, m, nblk, start):
            items.append(dict(u=u, p=p, m=m, nblk=nblk, c0=start + 512 * m, prev=last.get(u)))
            last[u] = len(items) - 1
        for p in range(NSLOT):
            start = 128 * (2 * (NSLOT - 1) - 2 * p)
            nblk = p // 2 + 1
            for m in range(nblk):
                for u in range(min(2, NU)):
                    add(u, p, m, nblk, start)
        for u in range(2, NU):
            for p in range(NSLOT):
                start = 128 * (2 * (NSLOT - 1) - 2 * p)
                nblk = p // 2 + 1
                for m in range(nblk):
                    add(u, p, m, nblk, start)

        def S0(n):
            it = items[n]
            z = n % 4
            u, p, c0 = it["u"], it["p"], it["c0"]
            P.op("pe", lambda e: e.matmul(zb[z][:], lhsT=q_sb[u][:, 128 * p:128 * p + 128], rhs=k_sb[u][:, c0:c0 + 512], start=True, stop=True),
                 reads=[("q", u), ("k", u, 0), ("k", u, 1)], writes=[("zb", z)])

        def S1(n):
            b = n % NS
            z = n % 4
            P.op("act", lambda e: e.activation(out=E[b][:], in_=zb[z][:], func=AF.Exp, scale=SCALE), reads=[("zb", z)], writes=[("E", b)])
            P.op("act", lambda e: e.activation(out=SP[b][:], in_=E[b][:], func=AF.Ln, bias=1.0), reads=[("E", b)], writes=[("SP", b)])

        def S2(n):
            it = items[n]
            b = n % NS
            cb = n % NC4
            m, u = it["m"], it["u"]
            if m == 0:
                P.op("dve", lambda e: e.tensor_tensor(out=SP[b][:, 0:128], in0=SP[b][:, 0:128], in1=Mk, op=ALU.mult),
                     reads=[("SP", b), "csb"], writes=[("SP", b)])
                init = 0.0
                rd = []
            else:
                pb = it["prev"] % NC4
                init = Cb[pb][:, 511:512]
                rd = [("C", pb)]
            P.op("dve", lambda e: e.tensor_tensor_scan(out=Cb[cb][:], data0=SP[b][:], data1=zeros[:], initial=init, op0=ALU.add, op1=ALU.add),
                 reads=[("SP", b), "zeros"] + rd, writes=[("C", cb)])
            P.op("dve", lambda e: e.scalar_tensor_tensor(out=ARG[b][:], in0=zb[n % 4][:], scalar=SCALE, in1=Cb[cb][:], op0=ALU.mult, op1=ALU.subtract),
                 reads=[("zb", n % 4), ("C", cb)], writes=[("ARG", b)])
            if m == 0:
                P.op("dve", lambda e: e.tensor_tensor(out=ARG[b][:, 0:128], in0=ARG[b][:, 0:128], in1=Mneg, op=ALU.add),
                     reads=[("ARG", b), "csb"], writes=[("ARG", b)])

        def S3(n):
            b = n % NS
            t = n % 2
            P.op("act", lambda e: e.activation(out=A[b][:], in_=ARG[b][:], func=AF.Exp), reads=[("ARG", b)], writes=[("A", b)])

            def tr(e):
                for c in range(4):
                    ins = e.transpose(out=tp[t][:, 128 * c:128 * c + 128], in_=A[b][:, 128 * c:128 * c + 128], identity=ident[:])
                return ins
            P.op("pe", tr, reads=[("A", b), "ident"], writes=[("tp", t)])

        def S4(n):
            it = items[n]
            b = n % NS
            t = n % 2
            u, p, c0, m, nblk = it["u"], it["p"], it["c0"], it["m"], it["nblk"]
            P.op("act", lambda e: e.activation(out=AT[b][:], in_=tp[t][:], func=AF.Copy), reads=[("tp", t)], writes=[("AT", b)])

            def pv(e):
                for c in range(4):
                    ins = e.matmul(ob[u % 2][:], lhsT=AT[b][:, 128 * c:128 * c + 128], rhs=v_sb[u][:, c0 // 128 + c, :],
                                   start=(m == 0 and c == 0), stop=(m == nblk - 1 and c == 3))
                return ins
            P.op("pe", pv, reads=[("AT", b), ("v", u, 0), ("v", u, 1)], writes=[("ob", u % 2)])
            if m != nblk - 1:
                return
            P.op("act", lambda e: e.activation(out=junk[:], in_=ob[u % 2][:], func=AF.Square, accum_out=ss[u][:]),
                 reads=[("ob", u % 2)], writes=["junk", ("ss", u)])
            P.op("act", lambda e: e.activation(out=ss[u][:], in_=ss[u][:], func=AF.Ln, scale=1.0 / HD, bias=EPS),
                 reads=[("ss", u)], writes=[("ss", u)])
            P.op("act", lambda e: e.activation(out=ss[u][:], in_=ss[u][:], func=AF.Exp, scale=-0.5),
                 reads=[("ss", u)], writes=[("ss", u)])
            P.op("dve", lambda e: e.scalar_tensor_tensor(out=OO[u][:], in0=ob[u % 2][:], scalar=ss[u][:, 0:1], in1=hg_sb[u][:], op0=ALU.mult, op1=ALU.mult),
                 reads=[("ob", u % 2), ("ss", u), ("hg", u)], writes=[("OO", u)])
            r0 = (u * NSLOT + p) * 128
            outs.append(P.dma("sp", oo[r0:r0 + 128, :], OO[u][:], reads=[("OO", u)], writes=[("oo", u, p)]))

        N = len(items)
        S0(0)
        for step in range(N + 3):
            if step + 1 < N:
                S0(step + 1)
            if step < N:
                S1(step)
            if 0 <= step - 1 < N:
                S2(step - 1)
            if 0 <= step - 2 < N:
                S3(step - 2)
            if 0 <= step - 3 < N:
                S4(step - 3)
        P.emit(final_wait_ops=outs)
    return nc


def sb_consts():
    tl = np.arange(128)[:, None]
    cl = np.arange(128)[None, :]
    M = (cl + tl >= 128).astype(np.float32)
    Mneg = np.where(M > 0, 0.0, -30000.0).astype(np.float32)
    return np.concatenate([M, Mneg, np.eye(128, dtype=np.float32)], axis=1)


def sb_unit_inputs(qT_h, kT_h, vT_h, par, NSLOT=32):
    tiles = [2 * p + par for p in range(NSLOT)]
    q = np.concatenate([qT_h[:, 128 * j:128 * j + 128] for j in tiles], axis=1)
    krev = kT_h[:, ::-1]
    vrev = vT_h[:, ::-1]
    sh = 128 if par == 0 else 0
    k = np.zeros((128, KPAD), dtype=kT_h.dtype)
    v = np.zeros((128, KPAD), dtype=vT_h.dtype)
    k[:, :S - sh] = krev[:, sh:]
    v[:, :S - sh] = vrev[:, sh:]
    return np.ascontiguousarray(q), k, np.ascontiguousarray(v.T)

import contextlib
import numpy as np

S = 8192
HD = 128
SCALE = HD ** -0.5
EPS = 1e-6
NT = S // 128


def build_chunk():
    nc = bass.Bass("TRN2", target_bir_lowering=False)
    qT = nc.dram_tensor("qT", [128, S], BF16, kind="ExternalInput").ap()
    kT = nc.dram_tensor("kT", [128, S], BF16, kind="ExternalInput").ap()
    vv = nc.dram_tensor("vv", [S, 128], BF16, kind="ExternalInput").ap()
    bT = nc.dram_tensor("bT", [128, 5 * 128], F32, kind="ExternalInput").ap()
    hg = nc.dram_tensor("hg", [128, 128], F32, kind="ExternalInput").ap()
    idn = nc.dram_tensor("idn", [128, 128], F32, kind="ExternalInput").ap()
    oo = nc.dram_tensor("oo", [S, 128], BF16, kind="ExternalOutput").ap()
    with contextlib.ExitStack() as st:
        sb = lambda n, s, d: st.enter_context(nc.sbuf_tensor(n, s, d))
        q_sb = sb("q_sb", [128, S], BF16)
        k_sb = sb("k_sb", [128, S], BF16)
        v_sb = sb("v_sb", [128, NT, 129], BF16)
        b_sb = sb("b_sb", [128, 640], BF16)
        ident = sb("ident", [128, 128], BF16)
        hg_sb = sb("hg_sb", [128, 128], F32)
        PT = [sb("PT%d" % i, [128, 640], BF16) for i in range(3)]
        o1 = [sb("o1%d" % i, [128, 128], F32) for i in range(2)]
        rd = [sb("rd%d" % i, [128, 1], F32) for i in range(2)]
        ss = [sb("ss%d" % i, [128, 1], F32) for i in range(2)]
        junk = sb("junk", [128, 128], F32)
        OO = [sb("OO%d" % i, [128, 128], BF16) for i in range(2)]
        st0 = [st.enter_context(nc.psum_tensor("st0%d" % i, [128, 512], F32)) for i in range(3)]
        st1 = [st.enter_context(nc.psum_tensor("st1%d" % i, [128, 128], F32)) for i in range(3)]
        ob = [st.enter_context(nc.psum_tensor("ob%d" % i, [128, 129], F32)) for i in range(2)]
        P = Prog(nc)
        for h in range(2):
            c0 = h * (S // 2)
            P.dma("sp", q_sb[:, c0:c0 + S // 2], qT[:, c0:c0 + S // 2], writes=[("qraw", h)])
            P.dma("sp", k_sb[:, c0:c0 + S // 2], kT[:, c0:c0 + S // 2], writes=[("k", h)])
            P.dma("sp", v_sb[:, h * 32:(h + 1) * 32, 0:128],
                  vv[h * 32 * 128:(h + 1) * 32 * 128, :].rearrange("(t p) d -> p t d", p=128), writes=[("v", h)])
        P.op("dve", lambda e: e.memset(v_sb[:, :, 128:129], 1.0), writes=["vone"])
        P.dma("pool", b_sb[:], bT, writes=["b"])
        P.dma("pool", ident[:], idn, writes=["ident"])
        P.dma("sp", hg_sb[:], hg, writes=["hg"])
        for blk in range(16):
            P.op("act", lambda e, blk=blk: e.activation(out=q_sb[:, blk * 512:(blk + 1) * 512], in_=q_sb[:, blk * 512:(blk + 1) * 512],
                                                        func=AF.Copy, scale=SCALE),
                 reads=[("qraw", blk // 8)], writes=[("q", blk)])
        outs = []

        def stageA(q):
            b3 = q % 3
            kks = [kk for kk in range(q - 4, q + 1) if kk >= 0]
            qread = [("q", q // 4)]

            def sc(e):
                for i, kk in enumerate(kks):
                    dl = q - kk
                    dst = st0[b3][:, 128 * i:128 * i + 128] if i < 4 else st1[b3][:, :]
                    e.matmul(dst, lhsT=k_sb[:, 128 * kk:128 * kk + 128], rhs=q_sb[:, 128 * q:128 * q + 128], start=True, stop=False)
                    ins = e.matmul(dst, lhsT=ident[:], rhs=b_sb[:, 128 * dl:128 * dl + 128], start=False, stop=True)
                return ins
            P.op("pe", sc, reads=qread + [("k", 0), ("k", 1), "b", "ident"], writes=[("st0", b3), ("st1", b3)])
            n0 = min(4, len(kks))
            P.op("act", lambda e: e.activation(out=PT[b3][:, 0:128 * n0], in_=st0[b3][:, 0:128 * n0], func=AF.Exp),
                 reads=[("st0", b3)], writes=[("PT0", b3)])
            if len(kks) == 5:
                P.op("act", lambda e: e.activation(out=PT[b3][:, 512:640], in_=st1[b3][:, :], func=AF.Exp),
                     reads=[("st1", b3)], writes=[("PT1", b3)])

        def stageB(q):
            b = q % 2
            b3 = q % 3
            kks = [kk for kk in range(q - 4, q + 1) if kk >= 0]

            def pv(e):
                for i, kk in enumerate(kks):
                    ins = e.matmul(ob[b][:], lhsT=PT[b3][:, 128 * i:128 * i + 128], rhs=v_sb[:, kk, :],
                                   start=(i == 0), stop=(i == len(kks) - 1))
                return ins
            P.op("pe", pv, reads=[("PT0", b3), ("PT1", b3), ("v", 0), ("v", 1), "vone"], writes=[("ob", b)])
            P.op("dve", lambda e: e.reciprocal(out=rd[b][:], in_=ob[b][:, 128:129]), reads=[("ob", b)], writes=[("rd", b)])
            P.op("dve", lambda e: e.tensor_scalar(out=o1[b][:], in0=ob[b][:, 0:128], scalar1=rd[b][:, 0:1], scalar2=None, op0=ALU.mult),
                 reads=[("ob", b), ("rd", b)], writes=[("o1", b)])
            P.op("act", lambda e: e.activation(out=junk[:], in_=o1[b][:], func=AF.Square, accum_out=ss[b][:]),
                 reads=[("o1", b)], writes=["junk", ("ss", b)])
            P.op("act", lambda e: e.activation(out=ss[b][:], in_=ss[b][:], func=AF.Ln, scale=1.0 / HD, bias=EPS), reads=[("ss", b)], writes=[("ss", b)])
            P.op("act", lambda e: e.activation(out=ss[b][:], in_=ss[b][:], func=AF.Exp, scale=-0.5), reads=[("ss", b)], writes=[("ss", b)])
            P.op("dve", lambda e: e.scalar_tensor_tensor(out=OO[b][:], in0=o1[b][:], scalar=ss[b][:, 0:1], in1=hg_sb[:],
                                                         op0=ALU.mult, op1=ALU.mult),
                 reads=[("o1", b), ("ss", b), "hg"], writes=[("OO", b)])
            outs.append(P.dma("sp", oo[128 * q:128 * q + 128, :], OO[b][:], reads=[("OO", b)], writes=[("oo", q)]))

        for step in range(NT + 2):
            if step < NT:
                stageA(step)
            if step - 2 >= 0:
                stageB(step - 2)
        P.emit(final_wait_ops=outs)
    return nc


def chunk_bias(rel_bias_h):
    s = np.arange(128)[:, None]
    t = np.arange(128)[None, :]
    out = np.zeros((128, 5, 128), np.float32)
    for dl in range(5):
        idx = np.clip(128 * dl + t - s, -256, 256) + 256
        blk = rel_bias_h[idx].astype(np.float32)
        if dl == 0:
            blk = np.where((t < 64) & (s >= 64), np.float32(-30000.0), blk)
        if dl == 4:
            blk = np.where((t >= 64) & (s < 64), np.float32(-30000.0), blk)
        out[:, dl, :] = blk
    return out.reshape(128, 640)

import contextlib
import numpy as np

S = 8192
HD = 128
EPS = 1e-6
NIT = 20
IDX_SCALE = (16 ** -0.5) * (64 ** -0.5)
TOPK = 256
NEG = -30000.0
C_P128, C_P64, C_ID, C_PAD, C_DIAG, C_POW = 0, 128, 256, 384, 1408, 1536
CW = 1536 + 32


def build_dsa(NSLOT=8):
    nc = bass.Bass("TRN2", target_bir_lowering=False)
    qa = nc.dram_tensor("qa", [12 * 128, 1024], BF16, kind="ExternalInput").ap()
    iq = nc.dram_tensor("iq", [8 * 128, 1024], BF16, kind="ExternalInput").ap()
    iw = nc.dram_tensor("iw", [1024, 16], BF16, kind="ExternalInput").ap()
    ka = nc.dram_tensor("ka", [2 * 128, S], BF16, kind="ExternalInput").ap()
    va = nc.dram_tensor("va", [S, 256], BF16, kind="ExternalInput").ap()
    ik = nc.dram_tensor("ik", [64, S], BF16, kind="ExternalInput").ap()
    tq = nc.dram_tensor("tq", [128, 2 * 1024], F32, kind="ExternalInput").ap()
    tiq = nc.dram_tensor("tiq", [128, 2 * 1024], F32, kind="ExternalInput").ap()
    tk = nc.dram_tensor("tk", [128, 2 * S], F32, kind="ExternalInput").ap()
    tik = nc.dram_tensor("tik", [128, 2 * S], F32, kind="ExternalInput").ap()
    cst = nc.dram_tensor("cst", [128, CW], F32, kind="ExternalInput").ap()
    hg = nc.dram_tensor("hg", [128, 12 * 128], F32, kind="ExternalInput").ap()
    oo = nc.dram_tensor("oo", [1024, 12 * 128], BF16, kind="ExternalOutput").ap()
    qav = qa.rearrange("(h p) t -> p h t", p=128)
    iqv = iq.rearrange("(h p) t -> p h t", p=128)
    with contextlib.ExitStack() as st:
        sb = lambda n, s, d: st.enter_context(nc.sbuf_tensor(n, s, d))
        csb = sb("csb", [128, CW], F32)
        pl128 = sb("pl128", [128, 128], BF16)
        pl64 = sb("pl64", [128, 128], BF16)
        ident3 = sb("ident3", [128, 384], BF16)
        hg_sb = sb("hg_sb", [128, 1536], F32)
        kaT = [sb("kaT%d" % g, [128, S], BF16) for g in range(2)]
        ikT = sb("ikT", [128, S], BF16)
        va_sb = sb("va_sb", [128, 64, 2, 129], BF16)
        score = sb("score", [128, S], F32)
        Mbs = [sb("Mb%d" % i, [128, S], BF16) for i in range(2)]
        qa_ss = [sb("qa_s%d" % i, [128, 12 * 128], BF16) for i in range(3)]
        iq_s = sb("iq_s", [128, 8 * 128], BF16)
        iw_b = sb("iw_b", [128, 16], BF16)
        iw_s = sb("iw_s", [128, 16], F32)
        dg = sb("dg", [128, 16 * 128], BF16)
        tqs = sb("tqs", [128, 2, 128], F32)
        tiqs = sb("tiqs", [128, 2, 128], F32)
        cosb = [score[:, 512 * i:512 * i + 512] for i in range(2)]
        sinb = [score[:, 512 * (2 + i):512 * (2 + i) + 512] for i in range(2)]
        t1 = [score[:, 512 * (4 + i):512 * (4 + i) + 512] for i in range(2)]
        t2 = [score[:, 512 * (6 + i):512 * (6 + i) + 512] for i in range(2)]
        t1s = [sb("t1s%d" % i, [128, 128], F32) for i in range(2)]
        t2s = [sb("t2s%d" % i, [128, 128], F32) for i in range(2)]
        R = [sb("R%d" % i, [128, 512], BF16) for i in range(4)]
        pT = [sb("pT%d" % i, [128, 384], BF16) for i in range(3)]
        Rm = sb("Rm", [128, 1], F32)
        steps = sb("steps", [128, 32], F32)
        cand = sb("cand", [128, 1], F32)
        cnt = sb("cnt", [128, 1], F32)
        sB = sb("sB", [128, 1], F32)
        tt = sb("tt", [128, 1], F32)
        thr = sb("thr", [128, 1], F32)
        rd = [sb("rd%d" % i, [128, 1], F32) for i in range(3)]
        ss = [sb("ss%d" % i, [128, 1], F32) for i in range(3)]
        o1 = [sb("o1%d" % i, [128, 128], F32) for i in range(3)]
        junk = sb("junk", [128, 128], F32)
        OO = [sb("OO%d" % i, [128, 128], BF16) for i in range(3)]
        rp = st.enter_context(nc.psum_tensor("rp", [128, 512], F32))
        dt = [st.enter_context(nc.psum_tensor("dt%d" % i, [128, 512], F32)) for i in range(4)]
        ac = [st.enter_context(nc.psum_tensor("ac%d" % i, [128, 512], F32)) for i in range(3)]
        P = Prog(nc)
        P.dma("sp", csb[:], cst, writes=["csb"])
        P.dma("sp", hg_sb[:], hg, writes=["hg"])
        P.op("dve", lambda e: e.tensor_copy(out=pl128[:], in_=csb[:, C_P128:C_P128 + 128]), reads=["csb"], writes=["pl128"])
        P.op("dve", lambda e: e.tensor_copy(out=pl64[:], in_=csb[:, C_P64:C_P64 + 128]), reads=["csb"], writes=["pl64"])
        for r in range(3):
            P.op("dve", lambda e, r=r: e.tensor_copy(out=ident3[:, 128 * r:128 * r + 128], in_=csb[:, C_ID:C_ID + 128]),
                 reads=["csb"], writes=[("ident3", r)])
        id3r = [("ident3", r) for r in range(3)]
        identf = csb[:, C_ID:C_ID + 128]
        for g in range(2):
            for h in range(2):
                c0 = h * (S // 2)
                P.dma("sp", kaT[g][:, c0:c0 + S // 2], ka[g * 128:(g + 1) * 128, c0:c0 + S // 2], writes=[("ka", g, c0 // 512 + b) for b in range(8)])
        for h in range(2):
            c0 = h * (S // 2)
            P.dma("sp", ikT[0:64, c0:c0 + S // 2], ik[:, c0:c0 + S // 2], writes=[("ikl", c0 // 512 + b) for b in range(8)])
            P.dma("sp", ikT[64:128, c0:c0 + S // 2], ik[:, c0:c0 + S // 2], writes=[("ikh", c0 // 512 + b) for b in range(8)])
            for g in range(2):
                P.dma("sp", va_sb[:, h * 32:(h + 1) * 32, g, 0:128],
                      va[h * 32 * 128:(h + 1) * 32 * 128, 128 * g:128 * g + 128].rearrange("(t p) d -> p t d", p=128), writes=[("va", h, g)])
        P.op("dve", lambda e: e.memset(va_sb[:, :, :, 128:129], 1.0), writes=["vone"])
        vreads = [("va", h, g) for h in range(2) for g in range(2)] + ["vone"]
        rn = [0]

        def rope(xap, keys, pl, plkey, cos_ap, sin_ap, tabkeys, n, small=False):
            b = rn[0] % 2
            rn[0] += 1
            if small:
                ta, tb_, ka_, kb_ = t1s[b][:, 0:n], t2s[b][:, 0:n], ("t1s", b), ("t2s", b)
            else:
                ta, tb_, ka_, kb_ = t1[b][:, 0:n], t2[b][:, 0:n], ("score", 4 + b), ("score", 6 + b)
            P.op("pe", lambda e: e.matmul(rp[:, 0:n], lhsT=pl[:], rhs=xap, start=True, stop=True),
                 reads=keys + [plkey], writes=["rp"])
            P.op("pool", lambda e: e.tensor_tensor(out=ta, in0=xap, in1=cos_ap, op=ALU.mult),
                 reads=keys + tabkeys, writes=[ka_])
            P.op("dve", lambda e: e.tensor_tensor(out=tb_, in0=rp[:, 0:n], in1=sin_ap, op=ALU.mult),
                 reads=["rp"] + tabkeys, writes=[kb_])
            P.op("pool", lambda e: e.tensor_tensor(out=xap, in0=ta, in1=tb_, op=ALU.add),
                 reads=[ka_, kb_], writes=keys)

        tn = 0
        for blk in range(15, -1, -1):
            c0 = blk * 512
            tb = tn % 2
            tn += 1
            P.dma("sp", cosb[tb], tk[:, c0:c0 + 512], writes=[("score", tb)])
            P.dma("sp", sinb[tb], tk[:, S + c0:S + c0 + 512], writes=[("score", 2 + tb)])
            for g in range(2):
                rope(kaT[g][:, c0:c0 + 512], [("ka", g, blk)], pl128, "pl128", cosb[tb], sinb[tb],
                     [("score", tb), ("score", 2 + tb)], 512)
            tb = tn % 2
            tn += 1
            P.dma("sp", cosb[tb], tik[:, c0:c0 + 512], writes=[("score", tb)])
            P.dma("sp", sinb[tb], tik[:, S + c0:S + c0 + 512], writes=[("score", 2 + tb)])
            rope(ikT[:, c0:c0 + 512], [("ikl", blk), ("ikh", blk)], pl64, "pl64", cosb[tb], sinb[tb],
                 [("score", tb), ("score", 2 + tb)], 512)
        kareads = lambda g, c0, n: [("ka", g, b) for b in range(c0 // 512, (c0 + n - 1) // 512 + 1)]
        ikreads = lambda c0: [("ikl", c0 // 512), ("ikh", c0 // 512)]
        outs = []
        dnc = [0]

        def setup(i):
            qa_s = qa_ss[i % 3]
            qk_ = lambda h: ("qa_s", i % 3, h)
            P.dma("sp", qa_s[:].rearrange("p (h t) -> p h t", h=12), qav[:, :, 128 * i:128 * i + 128], writes=[qk_(h) for h in range(12)])
            P.dma("sp", iq_s[:].rearrange("p (h t) -> p h t", h=8), iqv[:, :, 128 * i:128 * i + 128], writes=[("iq_s", h) for h in range(8)])
            P.dma("sp", iw_b[:], iw[128 * i:128 * i + 128, :], writes=["iw_b"])
            P.dma("sp", tqs[:], tq.rearrange("p (a t) -> p a t", a=2)[:, :, 128 * i:128 * i + 128], writes=["tqs"])
            P.dma("sp", tiqs[:], tiq.rearrange("p (a t) -> p a t", a=2)[:, :, 128 * i:128 * i + 128], writes=["tiqs"])
            P.op("dve", lambda e: e.tensor_copy(out=iw_s[:], in_=iw_b[:]), reads=["iw_b"], writes=["iw_s"])
            for h in range(8):
                rope(iq_s[:, 128 * h:128 * h + 128], [("iq_s", h)], pl64, "pl64", tiqs[:, 0, :], tiqs[:, 1, :], ["tiqs"], 128, small=True)
            for h in range(16):
                P.op("dve", lambda e, h=h: e.tensor_scalar(out=dg[:, 128 * h:128 * h + 128], in0=identf, scalar1=iw_s[:, h:h + 1], scalar2=None, op0=ALU.mult),
                     reads=["csb", "iw_s"], writes=[("dg", h)])
            for h in range(12):
                rope(qa_s[:, 128 * h:128 * h + 128], [qk_(h)], pl128, "pl128", tqs[:, 0, :], tqs[:, 1, :], ["tqs"], 128, small=True)

        def indexer(i):
            r0 = 56 - 8 * i
            idx_items = [(b, h) for b in range(2 * i + 2) for h in range(16)]
            dn0 = dnc[0]

            def idx_dots(k):
                b, h = idx_items[k]
                c0 = 128 * r0 + 512 * b
                db = (dn0 + k) % 4
                hp, pair = h % 2, h // 2
                P.op("pe", lambda e: e.matmul(dt[db][:], lhsT=iq_s[64 * hp:64 * hp + 64, 128 * pair:128 * pair + 128],
                                              rhs=ikT[64 * hp:64 * hp + 64, c0:c0 + 512], start=True, stop=True),
                     reads=[("iq_s", pair)] + ikreads(c0), writes=[("dt", db)])

            def idx_relu(k):
                db = (dn0 + k) % 4
                if k % 3 == 2:
                    P.op("dve", lambda e: e.tensor_scalar(out=R[db][:], in0=dt[db][:], scalar1=0.0, scalar2=None, op0=ALU.max),
                         reads=[("dt", db)], writes=[("R", db)])
                else:
                    P.op("act", lambda e: e.activation(out=R[db][:], in_=dt[db][:], func=AF.Relu),
                         reads=[("dt", db)], writes=[("R", db)])

            def idx_acc(k):
                b, h = idx_items[k]
                db = (dn0 + k) % 4
                ab = b % 2
                P.op("pe", lambda e: e.matmul(ac[ab][:], lhsT=dg[:, 128 * h:128 * h + 128], rhs=R[db][:], start=(h == 0), stop=(h == 15)),
                     reads=[("R", db), ("dg", h)], writes=[("ac", ab)])
                if h == 15:
                    P.op("dve", lambda e: e.tensor_scalar(out=score[:, 512 * b:512 * b + 512], in0=ac[ab][:], scalar1=IDX_SCALE, scalar2=None, op0=ALU.mult),
                         reads=[("ac", ab)], writes=[("score", b)])
            NI = len(idx_items)
            for k in range(0, NI + 2, 2):
                for kk in (k, k + 1):
                    if kk < NI:
                        idx_dots(kk)
                for kk in (k, k + 1):
                    if kk < NI:
                        idx_relu(kk)
                for kk in (k - 2, k - 1):
                    if 0 <= kk < NI:
                        idx_acc(kk)
            dnc[0] += NI

        def bisect(i):
            L = 1024 * (i + 1)
            Mb = Mbs[i % 2]
            mk = ("Mb", i % 2)
            sreads = [("score", b) for b in range(2 * i + 2)]
            P.op("dve", lambda e: e.tensor_reduce(out=Rm[:], in_=score[:, 0:L], axis=AX.X, op=ALU.max, apply_absolute_value=True),
                 reads=sreads, writes=["Rm"])
            P.op("dve", lambda e: e.tensor_scalar(out=Rm[:], in0=Rm[:], scalar1=1.001, scalar2=1e-6, op0=ALU.mult, op1=ALU.add),
                 reads=["Rm"], writes=["Rm"])
            P.op("dve", lambda e: e.tensor_scalar(out=steps[:, 0:32], in0=csb[:, C_POW:C_POW + 32], scalar1=Rm[:, 0:1], scalar2=None, op0=ALU.mult),
                 reads=["Rm", "csb"], writes=["steps"])
            P.op("dve", lambda e: e.tensor_tensor(out=score[:, L - 1024:L], in0=score[:, L - 1024:L], in1=csb[:, C_PAD:C_PAD + 1024], op=ALU.add),
                 reads=sreads + ["csb"], writes=[("score", 2 * i), ("score", 2 * i + 1)])
            P.op("dve", lambda e: e.tensor_tensor(out=score[:, 0:128], in0=score[:, 0:128], in1=csb[:, C_DIAG:C_DIAG + 128], op=ALU.add),
                 reads=[("score", 0), "csb"], writes=[("score", 0)])
            P.op("dve", lambda e: e.memset(cand[:], 0.0), writes=["cand"])
            Lh = L // 2
            yield
            for k in range(1, NIT + 1):
                P.op("dve", lambda e: e.tensor_scalar(out=Mb[:, 0:Lh], in0=score[:, 0:Lh], scalar1=cand[:, 0:1], scalar2=None,
                                                      op0=ALU.is_ge, op1=ALU.add, accum_out=cnt[:]),
                     reads=sreads + ["cand"], writes=[(mk, "A"), "cnt"])
                P.op("act", lambda e: e.activation(out=Mb[:, Lh:L], in_=score[:, Lh:L], func=AF.Sign, scale=-1.0, bias=cand[:, 0:1], accum_out=sB[:]),
                     reads=sreads + ["cand"], writes=[(mk, "B"), "sB"])
                P.op("dve", lambda e: e.scalar_tensor_tensor(out=cnt[:], in0=sB[:], scalar=-0.5, in1=cnt[:], op0=ALU.mult, op1=ALU.add),
                     reads=["sB", "cnt"], writes=["cnt"])
                P.op("dve", lambda e, k=k: e.tensor_scalar(out=tt[:], in0=cnt[:], scalar1=TOPK - 0.5 - Lh / 2.0, scalar2=steps[:, k - 1:k],
                                                           op0=ALU.is_ge, op1=ALU.mult),
                     reads=["cnt", "steps"], writes=["tt"])
                P.op("dve", lambda e, k=k: e.scalar_tensor_tensor(out=cand[:], in0=tt[:], scalar=steps[:, k:k + 1], in1=cand[:],
                                                                  op0=ALU.subtract, op1=ALU.add),
                     reads=["tt", "steps", "cand"], writes=["cand"])
                yield
            P.op("dve", lambda e: e.tensor_tensor(out=thr[:], in0=cand[:], in1=steps[:, NIT:NIT + 1], op=ALU.subtract),
                 reads=["cand", "steps"], writes=["thr"])
            P.op("dve", lambda e: e.tensor_scalar(out=Mb[:, 0:L], in0=score[:, 0:L], scalar1=thr[:, 0:1], scalar2=NEG,
                                                  op0=ALU.is_lt, op1=ALU.mult),
                 reads=sreads + ["thr"], writes=[(mk, "A"), (mk, "B")])
            yield

        def attend(i):
            r0 = 56 - 8 * i
            Mb = Mbs[i % 2]
            mk = ("Mb", i % 2)
            qa_s = qa_ss[i % 3]
            nst = 8 * i + 8
            for g in range(2):
                for hh in range(2):
                    h0 = 6 * g + 3 * hh
                    dn0 = dnc[0]

                    def a_qk(stl, g=g, h0=h0, dn0=dn0):
                        rr = r0 + stl
                        kc0 = 128 * rr
                        db = (dn0 + stl) % 3

                        def qk(e):
                            e.matmul(dt[db][:, 0:384], lhsT=kaT[g][:, kc0:kc0 + 128], rhs=qa_s[:, 128 * h0:128 * h0 + 384], start=True, stop=False)
                            return e.matmul(dt[db][:, 0:384], lhsT=Mb[:, 128 * stl:128 * stl + 128], rhs=ident3[:], start=False, stop=True)
                        P.op("pe", qk, reads=kareads(g, kc0, 128) + [("qa_s", i % 3, h0 + x) for x in range(3)] + [(mk, "A"), (mk, "B")] + id3r, writes=[("dt", db)])
                        P.op("act", lambda e: e.activation(out=pT[db][:], in_=dt[db][:, 0:384], func=AF.Exp),
                             reads=[("dt", db)], writes=[("pT", db)])

                    def a_pv(stl, g=g, dn0=dn0):
                        rr = r0 + stl
                        db = (dn0 + stl) % 3

                        def pv(e):
                            for hl in range(3):
                                ins = e.matmul(ac[hl][:, 0:129], lhsT=pT[db][:, 128 * hl:128 * hl + 128], rhs=va_sb[:, rr, g, :],
                                               start=(stl == 0), stop=(stl == nst - 1))
                            return ins
                        P.op("pe", pv, reads=[("pT", db)] + vreads, writes=[("ac", 0), ("ac", 1), ("ac", 2)])
                    for k in range(nst + 2):
                        if k < nst:
                            a_qk(k)
                        if k - 2 >= 0:
                            a_pv(k - 2)
                        yield
                    dnc[0] += nst
                    for hl in range(3):
                        hd = h0 + hl
                        P.op("dve", lambda e, hl=hl: e.reciprocal(out=rd[hl][:], in_=ac[hl][:, 128:129]), reads=[("ac", hl)], writes=[("rd", hl)])
                        P.op("dve", lambda e, hl=hl: e.tensor_scalar(out=o1[hl][:], in0=ac[hl][:, 0:128], scalar1=rd[hl][:, 0:1], scalar2=None, op0=ALU.mult),
                             reads=[("ac", hl), ("rd", hl)], writes=[("o1", hl)])
                        P.op("act", lambda e, hl=hl: e.activation(out=junk[:], in_=o1[hl][:], func=AF.Square, accum_out=ss[hl][:]),
                             reads=[("o1", hl)], writes=["junk", ("ss", hl)])
                        P.op("act", lambda e, hl=hl: e.activation(out=ss[hl][:], in_=ss[hl][:], func=AF.Ln, scale=1.0 / HD, bias=EPS),
                             reads=[("ss", hl)], writes=[("ss", hl)])
                        P.op("act", lambda e, hl=hl: e.activation(out=ss[hl][:], in_=ss[hl][:], func=AF.Exp, scale=-0.5),
                             reads=[("ss", hl)], writes=[("ss", hl)])
                        P.op("dve", lambda e, hl=hl, hd=hd: e.scalar_tensor_tensor(out=OO[hl][:], in0=o1[hl][:], scalar=ss[hl][:, 0:1],
                                                                                    in1=hg_sb[:, 128 * hd:128 * hd + 128], op0=ALU.mult, op1=ALU.mult),
                             reads=[("o1", hl), ("ss", hl), "hg"], writes=[("OO", hl)])
                        outs.append(P.dma("sp", oo[128 * i:128 * i + 128, 128 * hd:128 * hd + 128], OO[hl][:], reads=[("OO", hl)],
                                          writes=[("oo", i, hd)]))

        def run_all(gen):
            for _ in gen:
                pass

        setup(0)
        for i in range(NSLOT):
            indexer(i)
            if i + 1 < NSLOT:
                setup(i + 1)
            if i == 0:
                run_all(bisect(0))
                continue
            gb = bisect(i)
            ga = attend(i - 1)
            n_att = 4 * (8 * (i - 1) + 8 + 2)
            per = max(1, (n_att + NIT) // (NIT + 1))
            done_a = False
            for _ in gb:
                if not done_a:
                    for _j in range(per):
                        try:
                            next(ga)
                        except StopIteration:
                            done_a = True
                            break
            if not done_a:
                run_all(ga)
        run_all(attend(NSLOT - 1))
        P.emit(final_wait_ops=outs)
    return nc


def rope_tables(pos, d):
    inv = (np.float32(10000.0) ** (-np.arange(0, d, 2, dtype=np.float32) / np.float32(d))).astype(np.float32)
    ang = (pos.astype(np.float32)[:, None] * inv[None, :]).astype(np.float32)
    return np.cos(ang).astype(np.float32).T, np.sin(ang).astype(np.float32).T


def dsa_consts(c):
    cst = np.zeros((128, CW), np.float32)
    k = np.arange(128)
    for kk in range(128):
        if kk >= 64:
            cst[kk, C_P128 + kk - 64] = -1.0
        else:
            cst[kk, C_P128 + kk + 64] = 1.0
        if kk % 64 >= 32:
            cst[kk, C_P64 + kk - 32] = -1.0
        else:
            cst[kk, C_P64 + kk + 32] = 1.0
    cst[:, C_ID:C_ID + 128] = np.eye(128, dtype=np.float32)
    col = np.arange(1024)[None, :]
    cst[:, C_PAD:C_PAD + 1024] = np.where(col >= 128 * (c + 1), np.float32(-1e30), np.float32(0.0))
    tl = np.arange(128)[:, None]
    cl = np.arange(128)[None, :]
    cst[:, C_DIAG:C_DIAG + 128] = np.where((tl < 64) & (cl < 64), np.float32(-1e30), np.float32(0.0))
    cst[:, C_POW:C_POW + 32] = (2.0 ** -np.arange(32, dtype=np.float64)).astype(np.float32)[None, :]
    return cst


def dsa_core_inputs(c, pT, hgain12):
    tiles = [c + 8 * i for i in range(8)]
    tok = np.concatenate([np.arange(128 * j, 128 * j + 128) for j in tiles])
    rprime = np.arange(64)
    ktile = 63 - (rprime + 7 - c)
    key = (128 * ktile[:, None] + (127 - np.arange(128))[None, :]).reshape(-1)
    valid = key >= 0
    keyc = np.where(valid, key, 0)

    def stream(rows):
        out = rows[:, keyc].copy()
        out[:, ~valid] = 0
        return out
    qa = np.ascontiguousarray(pT[0:1536][:, tok])
    iq = np.ascontiguousarray(pT[2048:3072][:, tok])
    iw = np.ascontiguousarray(pT[3136:3152][:, tok].T)
    ka = stream(pT[1536:1792])
    va = np.ascontiguousarray(stream(pT[1792:2048]).T)
    ik = stream(pT[3072:3136])
    sc = np.float32(HD ** -0.5)
    cq, sq = rope_tables(tok.astype(np.float32), 128)
    tq = np.concatenate([np.concatenate([cq, cq], 0) * sc, np.concatenate([sq, sq], 0) * sc], axis=1).astype(np.float32)
    ci, si = rope_tables(tok.astype(np.float32), 64)
    tiq = np.concatenate([np.concatenate([ci, ci, ci, ci], 0), np.concatenate([si, si, si, si], 0)], axis=1).astype(np.float32)
    ck, sk = rope_tables(keyc.astype(np.float32), 128)
    tk = np.concatenate([np.concatenate([ck, ck], 0), np.concatenate([sk, sk], 0)], axis=1).astype(np.float32)
    cik, sik = rope_tables(keyc.astype(np.float32), 64)
    tik = np.concatenate([np.concatenate([cik] * 4, 0), np.concatenate([sik] * 4, 0)], axis=1).astype(np.float32)
    hgb = np.ascontiguousarray(np.broadcast_to(hgain12[None, :], (128, 1536))).astype(np.float32)
    return {"qa": qa, "iq": iq, "iw": iw, "ka": ka, "va": va, "ik": ik, "tq": tq, "tiq": tiq, "tk": tk, "tik": tik,
            "cst": dsa_consts(c), "hg": hgb}, tok


NCORES = 8
CORES = list(range(NCORES))
DFF = 11008
DFFP = 11264
HC = DFFP // NCORES
PW = 10832
PWC = PW // NCORES
ADA_NC = 3072

_PROGS = {}


def _prog(name, fn):
    if name not in _PROGS:
        _PROGS[name] = fn()
    return _PROGS[name]


def _run(nc, in_maps):
    res = run_bass_kernel_spmd(nc, in_maps, core_ids=CORES)
    return res.results


def _vecl(v):
    return np.ascontiguousarray(np.asarray(v, np.float32).reshape(-1, 128).T)


def kernel(x, c, w_ada, b_ada, norm_attn_g, w_in, rel_bias, head_norm_g, w_out,
           norm_ffn_g, w_gate_up, w_down, final_norm_g):
    x = np.asarray(x); c = np.asarray(c)
    depth = w_ada.shape[0]
    nc0 = _prog("k0", build_k0)
    cT = np.ascontiguousarray(np.asarray(c[0], np.float32).reshape(32, 128).T)
    ims = []
    for i in range(NCORES):
        sl = slice(i * ADA_NC, (i + 1) * ADA_NC)
        ims.append({"cT": cT, "w": np.ascontiguousarray(np.asarray(w_ada)[:, :, sl].reshape(depth * D, ADA_NC)),
                    "b": np.ascontiguousarray(np.asarray(b_ada)[:, sl].reshape(1, depth * ADA_NC))})
    r = _run(nc0, ims)
    mod = np.concatenate([r[i]["y"].reshape(depth, ADA_NC) for i in range(NCORES)], axis=1)
    del ims
    xT = np.ascontiguousarray(np.asarray(x[0], np.float32).T)

    def norm_phase(xT, g, sc, sh, final=False):
        ncn = _prog("normF" if final else "norm", lambda: build_norm(final))
        ims = [{"xT": np.ascontiguousarray(xT[:, i * 1024:(i + 1) * 1024]), "gv": _vecl(g), "scv": _vecl(sc), "shv": _vecl(sh)}
               for i in range(NCORES)]
        r = _run(ncn, ims)
        return np.concatenate([r[i]["hT"] for i in range(NCORES)], axis=1)

    for l in range(depth):
        sh1, sc1, g1, sh2, sc2, g2 = np.split(mod[l], 6)
        hg = np.asarray(head_norm_g[l], np.float32)
        hT = norm_phase(xT, norm_attn_g[l], sc1, sh1)
        ncg = _prog("pr", lambda: build_gemm("raw", D, PWC))
        wl = np.asarray(w_in[l])
        r = _run(ncg, [{"aT": hT, "W": np.ascontiguousarray(wl[:, i * PWC:(i + 1) * PWC])} for i in range(NCORES)])
        pT = np.concatenate([r[i]["yT"] for i in range(NCORES)], axis=0)
        del hT, r
        mix = np.zeros((S, D), dtype=pT.dtype)
        nca = _prog("dsa", build_dsa)
        ims, toks = [], []
        for i in range(NCORES):
            im, tok = dsa_core_inputs(i, pT, hg[:1536])
            ims.append(im); toks.append(tok)
        r = _run(nca, ims)
        for i in range(NCORES):
            mix[toks[i], 0:1536] = r[i]["oo"]
        del ims, r
        ncs = _prog("sb", build_sb)
        cst = sb_consts()
        ims = []
        for i in range(NCORES):
            qs, ks, vs, hgs = [], [], [], []
            for u in range(3):
                n = 3 * i + u
                h, par = n // 2, n % 2
                q, k, v = sb_unit_inputs(pT[3152 + 128 * h:3152 + 128 * h + 128], pT[4688 + 128 * h:4688 + 128 * h + 128],
                                         pT[6224 + 128 * h:6224 + 128 * h + 128], par)
                qs.append(q); ks.append(k); vs.append(v)
                hgs.append(np.broadcast_to(hg[(12 + h) * 128:(13 + h) * 128][None, :], (128, 128)))
            ims.append({"qT": np.concatenate(qs, 0), "kT": np.concatenate(ks, 0), "vv": np.concatenate(vs, 0),
                        "hg": np.ascontiguousarray(np.concatenate(hgs, 0)).astype(np.float32), "cst": cst})
        r = _run(ncs, ims)
        for i in range(NCORES):
            o = r[i]["oo"].reshape(3, 32, 128, 128)
            for u in range(3):
                n = 3 * i + u
                h, par = n // 2, n % 2
                for p in range(32):
                    j = 2 * p + par
                    mix[128 * j:128 * j + 128, (12 + h) * 128:(13 + h) * 128] = o[u, p]
        del ims, r
        ncc = _prog("chunk", build_chunk)
        idn = np.eye(128, dtype=np.float32)
        ims = []
        for h in range(NCORES):
            ims.append({"qT": np.ascontiguousarray(pT[7760 + 128 * h:7760 + 128 * h + 128]),
                        "kT": np.ascontiguousarray(pT[8784 + 128 * h:8784 + 128 * h + 128]),
                        "vv": np.ascontiguousarray(pT[9808 + 128 * h:9808 + 128 * h + 128].T),
                        "bT": chunk_bias(np.asarray(rel_bias[l][h], np.float32)), "idn": idn,
                        "hg": np.ascontiguousarray(np.broadcast_to(hg[(24 + h) * 128:(25 + h) * 128][None, :], (128, 128))).astype(np.float32)})
        r = _run(ncc, ims)
        for h in range(NCORES):
            mix[:, (24 + h) * 128:(25 + h) * 128] = r[h]["oo"]
        del ims, r, pT
        mixT = np.ascontiguousarray(mix.T)
        del mix
        nco = _prog("op", lambda: build_gemm("res", D, 512))
        wl = np.asarray(w_out[l])
        r = _run(nco, [{"aT": mixT, "W": np.ascontiguousarray(wl[:, i * 512:(i + 1) * 512]),
                        "xT": np.ascontiguousarray(xT[i * 512:(i + 1) * 512]), "gvec": _vecl(g1[i * 512:(i + 1) * 512])}
                       for i in range(NCORES)])
        xT = np.concatenate([r[i]["yT"] for i in range(NCORES)], axis=0)
        del mixT, r
        h2T = norm_phase(xT, norm_ffn_g[l], sc2, sh2)
        ncu = _prog("gu", lambda: build_gemm("glu", D, HC))
        wgu = np.asarray(w_gate_up[l])
        ims = []
        for i in range(NCORES):
            W = np.zeros((D, 2 * HC), np.float32)
            lo, hi = i * HC, min(DFF, (i + 1) * HC)
            W[:, 0:hi - lo] = wgu[:, lo:hi]
            W[:, HC:HC + hi - lo] = wgu[:, DFF + lo:DFF + hi]
            ims.append({"aT": h2T, "W": W})
        r = _run(ncu, ims)
        actT = np.concatenate([r[i]["yT"] for i in range(NCORES)], axis=0)
        del ims, r, h2T
        ncd = _prog("dn", lambda: build_gemm("res", DFFP, 512))
        wd = np.zeros((DFFP, D), np.float32)
        wd[:DFF] = np.asarray(w_down[l])
        r = _run(ncd, [{"aT": actT, "W": np.ascontiguousarray(wd[:, i * 512:(i + 1) * 512]),
                        "xT": np.ascontiguousarray(xT[i * 512:(i + 1) * 512]), "gvec": _vecl(g2[i * 512:(i + 1) * 512])}
                       for i in range(NCORES)])
        xT = np.concatenate([r[i]["yT"] for i in range(NCORES)], axis=0)
        del actT, wd, r
    zero = np.zeros(D, np.float32)
    oT = norm_phase(xT, final_norm_g, zero, zero, final=True)
    return np.ascontiguousarray(oT.T)[None].astype(np.float32)
```

```python
import contextlib

import numpy as np
import concourse.bass as bass
import concourse.mybir as mybir
from concourse.bass_utils import run_bass_kernel_spmd

F32 = mybir.dt.float32
BF16 = mybir.dt.bfloat16
AF = mybir.ActivationFunctionType
ALU = mybir.AluOpType
AX = mybir.AxisListType

ENGS = ("pe", "act", "dve", "pool", "sp")


class Op:
    __slots__ = ("eng", "fn", "deps", "idx", "has_dep", "tok", "is_dma", "dsem")

    def __init__(self, eng, fn, deps, is_dma):
        self.eng = eng
        self.fn = fn
        self.deps = deps
        self.has_dep = False
        self.tok = None
        self.is_dma = is_dma
        self.dsem = None


class Prog:
    def __init__(self, nc, n_dma_sems=12):
        self.nc = nc
        self.ops = []
        self.last_writer = {}
        self.readers = {}
        self.n_dma_sems = n_dma_sems

    def op(self, eng, fn, reads=(), writes=(), dma=False):
        deps = []
        for b in reads:
            w = self.last_writer.get(b)
            if w is not None:
                deps.append(w)
        for b in writes:
            w = self.last_writer.get(b)
            if w is not None:
                deps.append(w)
            deps.extend(self.readers.get(b, ()))
        o = Op(eng, fn, deps, dma)
        for d in deps:
            d.has_dep = True
        for b in writes:
            self.last_writer[b] = o
            self.readers[b] = []
        for b in reads:
            self.readers.setdefault(b, []).append(o)
        self.ops.append(o)
        return o

    def dma(self, eng, out, in_, reads=(), writes=()):
        return self.op(eng, lambda e: e.dma_start(out=out, in_=in_), reads, writes, dma=True)

    def emit(self, final_wait_ops=()):
        nc = self.nc
        import contextlib
        fw_set = set(id(o) for o in final_wait_ops)
        with contextlib.ExitStack() as st:
            esem = {e: st.enter_context(nc.semaphore("s_" + e)) for e in ENGS}
            dsems = {e: [st.enter_context(nc.semaphore("d_%s%d" % (e, i))) for i in range(self.n_dma_sems)]
                     for e in ("sp", "pool", "act")}
            ecount = {e: 0 for e in ENGS}
            dcount = {e: [0] * self.n_dma_sems for e in dsems}
            drr = {e: 0 for e in dsems}
            per_eng = {e: [] for e in ENGS}
            for o in self.ops:
                per_eng[o.eng].append(o)
                if o.is_dma:
                    i = drr[o.eng] % self.n_dma_sems
                    drr[o.eng] += 1
                    prev = dcount[o.eng][i]
                    dcount[o.eng][i] += 16
                    o.dsem = (dsems[o.eng][i], prev)
                    o.tok = (dsems[o.eng][i], dcount[o.eng][i])
                else:
                    if o.has_dep or id(o) in fw_set:
                        ecount[o.eng] += 1
                        o.tok = (esem[o.eng], ecount[o.eng])
            block = st.enter_context(nc.Block())
            handles = {"pe": block.tensor, "act": block.scalar, "dve": block.vector,
                       "pool": block.gpsimd, "sp": block.sync}

            def make(ename):
                ops = per_eng[ename]

                def body(eng):
                    waited = {}
                    for o in ops:
                        need = {}
                        for d in o.deps:
                            if d.eng == "pe" and ename == "pe" and not d.is_dma:
                                continue
                            s, v = d.tok
                            k = id(s)
                            if waited.get(k, 0) >= v:
                                continue
                            if k not in need or need[k][1] < v:
                                need[k] = (s, v)
                        if o.is_dma:
                            s, prev = o.dsem
                            k = id(s)
                            if prev > 0 and waited.get(k, 0) < prev:
                                if k not in need or need[k][1] < prev:
                                    need[k] = (s, prev)
                        for k, (s, v) in need.items():
                            eng.wait_ge(s, v)
                            waited[k] = v
                        ins = o.fn(eng)
                        if o.tok is not None:
                            s, v = o.tok
                            ins.then_inc(s, 16 if o.is_dma else 1)
                    if ename == "sp":
                        for o in final_wait_ops:
                            s, v = o.tok
                            eng.wait_ge(s, v)
                return body

            for e in ENGS:
                if per_eng[e] or e == "sp":
                    handles[e](make(e))

import contextlib
import numpy as np

D = 4096
S = 8192
EPS = 1e-6


def build_norm(final=False, T=1024):
    nc = bass.Bass("TRN2", target_bir_lowering=False)
    xT = nc.dram_tensor("xT", [D, T], F32, kind="ExternalInput").ap()
    gv = nc.dram_tensor("gv", [128, 32], F32, kind="ExternalInput").ap()
    scv = nc.dram_tensor("scv", [128, 32], F32, kind="ExternalInput").ap()
    shv = nc.dram_tensor("shv", [128, 32], F32, kind="ExternalInput").ap()
    odt = F32 if final else BF16
    hT = nc.dram_tensor("hT", [D, T], odt, kind="ExternalOutput").ap()
    xv = xT.rearrange("(kc p) t -> p kc t", p=128)
    hv = hT.rearrange("(kc p) t -> p kc t", p=128)
    with contextlib.ExitStack() as st:
        sb = lambda n, s, d: st.enter_context(nc.sbuf_tensor(n, s, d))
        g_sb = sb("g_sb", [128, 32], F32)
        sc_sb = sb("sc_sb", [128, 32], F32)
        sh_sb = sb("sh_sb", [128, 32], F32)
        a_sb = sb("a_sb", [128, 32], F32)
        ones = sb("ones", [128, 128], F32)
        xs = sb("xs", [128, 32, 512], F32)
        sq = [sb("sq%d" % i, [128, 512], F32) for i in range(2)]
        rstd = sb("rstd", [128, 512], F32)
        tmp = [sb("tmp%d" % i, [128, 512], F32) for i in range(2)]
        ho = [sb("ho%d" % i, [128, 8, 512], odt) for i in range(2)]
        ps = st.enter_context(nc.psum_tensor("ps", [128, 512], F32))
        P = Prog(nc)
        P.dma("sp", g_sb[:], gv, writes=["g"])
        P.dma("sp", sc_sb[:], scv, writes=["sc"])
        P.dma("sp", sh_sb[:], shv, writes=["sh"])
        P.op("dve", lambda e: e.memset(ones[:], 1.0), writes=["ones"])
        P.op("dve", lambda e: e.scalar_tensor_tensor(out=a_sb[:], in0=sc_sb[:], scalar=1.0, in1=g_sb[:],
                                                     op0=ALU.add, op1=ALU.mult),
             reads=["sc", "g"], writes=["a"])
        outs = []
        for half in range(T // 512):
            t0 = half * 512
            for q in range(4):
                P.dma("sp", xs[:, q * 8:(q + 1) * 8, :], xv[:, q * 8:(q + 1) * 8, t0:t0 + 512], writes=[("xs", q)])
            for kc in range(32):
                s = kc % 2
                P.op("act", lambda e, kc=kc, s=s: e.activation(out=sq[s][:], in_=xs[:, kc, :], func=AF.Square),
                     reads=[("xs", kc // 8)], writes=[("sq", s)])
                P.op("pe", lambda e, kc=kc, s=s: e.matmul(ps[:], lhsT=ones[:], rhs=sq[s][:], start=(kc == 0), stop=(kc == 31)),
                     reads=[("sq", s), "ones"], writes=["ps"])
            P.op("dve", lambda e: e.tensor_scalar(out=rstd[:], in0=ps[:], scalar1=1.0 / D, scalar2=EPS,
                                                  op0=ALU.mult, op1=ALU.add), reads=["ps"], writes=["rstd"])
            P.op("act", lambda e: e.activation(out=rstd[:], in_=rstd[:], func=AF.Sqrt), reads=["rstd"], writes=["rstd"])
            P.op("dve", lambda e: e.reciprocal(out=rstd[:], in_=rstd[:]), reads=["rstd"], writes=["rstd"])
            for kc in range(32):
                s = kc % 2
                hs = (kc // 8) % 2
                P.op("dve", lambda e, kc=kc, s=s: e.tensor_tensor(out=tmp[s][:], in0=xs[:, kc, :], in1=rstd[:], op=ALU.mult),
                     reads=[("xs", kc // 8), "rstd"], writes=[("tmp", s)])
                P.op("act", lambda e, kc=kc, s=s, hs=hs: e.activation(out=ho[hs][:, kc % 8, :], in_=tmp[s][:], func=AF.Identity,
                                                                       bias=sh_sb[:, kc:kc + 1], scale=a_sb[:, kc:kc + 1]),
                     reads=[("tmp", s), "a", "sh"], writes=[("ho", hs, kc % 8)])
                if kc % 8 == 7:
                    q = kc // 8
                    outs.append(P.dma("sp", hv[:, q * 8:(q + 1) * 8, t0:t0 + 512], ho[hs][:],
                                      reads=[("ho", hs, i) for i in range(8)], writes=[("hv", half, q)]))
        P.emit(final_wait_ops=outs)
    return nc


def build_gemm(mode, K, ncols, T=S):
    KC = K // 128
    nc = bass.Bass("TRN2", target_bir_lowering=False)
    wcols = 2 * ncols if mode == "glu" else ncols
    aT = nc.dram_tensor("aT", [K, T], BF16, kind="ExternalInput").ap()
    W = nc.dram_tensor("W", [K, wcols], F32, kind="ExternalInput").ap()
    av = aT.rearrange("(kc p) t -> p kc t", p=128)
    wv = W.rearrange("(kc p) n -> p kc n", p=128)
    if mode == "res":
        xT = nc.dram_tensor("xT", [ncols, T], F32, kind="ExternalInput").ap()
        gvec = nc.dram_tensor("gvec", [128, (ncols + 127) // 128], F32, kind="ExternalInput").ap()
        yT = nc.dram_tensor("yT", [ncols, T], F32, kind="ExternalOutput").ap()
    else:
        yT = nc.dram_tensor("yT", [ncols, T], BF16, kind="ExternalOutput").ap()
    blocks = [(c0, min(128, ncols - c0)) for c0 in range(0, ncols, 128)]
    gsz = 2 if mode == "glu" else 4
    groups = [blocks[i:i + gsz] for i in range(0, len(blocks), gsz)]
    KSUB = 16 if KC <= 32 else 8
    nsub = (KC + KSUB - 1) // KSUB
    NAB = 4
    with contextlib.ExitStack() as st:
        sb = lambda n, s, d: st.enter_context(nc.sbuf_tensor(n, s, d))
        nwb = 2 if len(groups) > 1 else 1
        wb = [sb("wb%d" % i, [128, KC, 512], BF16) for i in range(nwb)]
        ab = [sb("ab%d" % i, [128, KSUB, 512], BF16) for i in range(NAB)]
        pss = [st.enter_context(nc.psum_tensor("ps%d" % i, [128, 512], F32)) for i in range(8)]
        if mode == "res":
            g_sb = sb("g_sb", [128, (ncols + 127) // 128], F32)
            xt = [sb("xt%d" % i, [128, 512], F32) for i in range(16)]
            yo = [sb("yo%d" % i, [128, 512], F32) for i in range(4)]
        elif mode == "glu":
            sg = [sb("sg%d" % i, [128, 512], F32) for i in range(2)]
            yo = [sb("yo%d" % i, [128, 512], BF16) for i in range(4)]
        else:
            yo = [sb("yo%d" % i, [128, 512], BF16) for i in range(4)]
        P = Prog(nc)
        if mode == "res":
            P.dma("sp", g_sb[:], gvec, writes=["g"])
        outs = []
        units = []
        for gi, grp in enumerate(groups):
            for tb in range(T // 512):
                for su in range(nsub):
                    units.append((gi, tb, su))
        ginfo = {}
        for gi, grp in enumerate(groups):
            wl = []
            off = 0
            for (c0, cw) in grp:
                wl.append((c0, cw, off)); off += cw
            if mode == "glu":
                for (c0, cw) in grp:
                    wl.append((ncols + c0, cw, off)); off += cw
            ginfo[gi] = wl
        PFU = NAB - 1
        yon = [0]
        xtn = [0]
        xt_slot = {}

        def issue_load(u):
            gi, tb, su = units[u]
            t0 = tb * 512
            if su == 0 and tb == 0:
                ws = gi % nwb
                for (wc0, cw, o) in ginfo[gi]:
                    for q in range(0, KC, 8):
                        q1 = min(KC, q + 8)
                        P.dma("pool", wb[ws][:, q:q1, o:o + cw], wv[:, q:q1, wc0:wc0 + cw], writes=[("wb", ws, o, q)])
            k0 = su * KSUB
            k1 = min(KC, k0 + KSUB)
            s = u % NAB
            P.dma("sp", ab[s][:, 0:k1 - k0, :], av[:, k0:k1, t0:t0 + 512], writes=[("ab", s)])
            if mode == "res" and su == 0:
                for bi, (c0, cw) in enumerate(groups[gi]):
                    xs_ = xtn[0] % 16
                    xtn[0] += 1
                    xt_slot[(gi, tb, bi)] = xs_
                    P.dma("sp", xt[xs_][0:cw, :], xT[c0:c0 + cw, t0:t0 + 512], writes=[("xt", xs_)])

        for u in range(min(PFU, len(units))):
            issue_load(u)
        for u, (gi, tb, su) in enumerate(units):
            if u + PFU < len(units):
                issue_load(u + PFU)
            grp = groups[gi]
            wl = ginfo[gi]
            ws = gi % nwb
            t0 = tb * 512
            pset = (gi * (T // 512) + tb) % 2
            banks = [pss[pset * 4 + i] for i in range(len(wl))]
            bkeys = [("ps", pset * 4 + i) for i in range(len(wl))]
            wreads = [("wb", ws, o, q) for (_, _, o) in wl for q in range(0, KC, 8)]
            k0 = su * KSUB
            k1 = min(KC, k0 + KSUB)
            s = u % NAB

            def mm(e, s=s, k0=k0, k1=k1, banks=banks, wl=wl, ws=ws):
                ins = None
                for bi, (wc0, cw, o) in enumerate(wl):
                    for kc in range(k0, k1):
                        ins = e.matmul(banks[bi][0:cw, :], lhsT=wb[ws][:, kc, o:o + cw], rhs=ab[s][:, kc - k0, :],
                                       start=(kc == 0), stop=(kc == KC - 1))
                return ins
            P.op("pe", mm, reads=[("ab", s)] + wreads, writes=bkeys)
            if su != nsub - 1:
                continue
            nb = len(grp)
            for bi, (c0, cw) in enumerate(grp):
                ys = yon[0] % 4
                yon[0] += 1
                if mode == "raw":
                    P.op("act", lambda e, bi=bi, cw=cw, ys=ys, banks=banks: e.activation(out=yo[ys][0:cw, :], in_=banks[bi][0:cw, :], func=AF.Copy),
                         reads=[bkeys[bi]], writes=[("yo", ys)])
                elif mode == "glu":
                    s2 = bi % 2
                    P.op("act", lambda e, bi=bi, cw=cw, s2=s2, banks=banks: e.activation(out=sg[s2][0:cw, :], in_=banks[bi][0:cw, :], func=AF.Silu),
                         reads=[bkeys[bi]], writes=[("sg", s2)])
                    P.op("dve", lambda e, bi=bi, cw=cw, s2=s2, ys=ys, banks=banks, nb=nb: e.tensor_tensor(out=yo[ys][0:cw, :], in0=banks[nb + bi][0:cw, :], in1=sg[s2][0:cw, :], op=ALU.mult),
                         reads=[bkeys[nb + bi], ("sg", s2)], writes=[("yo", ys)])
                else:
                    cb = c0 // 128
                    xs_ = xt_slot[(gi, tb, bi)]
                    P.op("dve", lambda e, bi=bi, cw=cw, ys=ys, cb=cb, banks=banks, xs_=xs_: e.scalar_tensor_tensor(
                        out=yo[ys][0:cw, :], in0=banks[bi][0:cw, :], scalar=g_sb[0:cw, cb:cb + 1], in1=xt[xs_][0:cw, :],
                        op0=ALU.mult, op1=ALU.add),
                        reads=[bkeys[bi], ("xt", xs_), "g"], writes=[("yo", ys)])
                outs.append(P.dma("sp", yT[c0:c0 + cw, t0:t0 + 512], yo[ys][0:cw, :], reads=[("yo", ys)],
                                  writes=[("y", c0, tb)]))
        P.emit(final_wait_ops=outs)
    return nc

D = 4096
NCOL = 3072


def build_k0():
    nc = bass.Bass("TRN2", target_bir_lowering=False)
    cT = nc.dram_tensor("cT", [128, 32], F32, kind="ExternalInput").ap()
    w = nc.dram_tensor("w", [2 * D, NCOL], F32, kind="ExternalInput").ap()
    b = nc.dram_tensor("b", [1, 2 * NCOL], F32, kind="ExternalInput").ap()
    y = nc.dram_tensor("y", [1, 2 * NCOL], F32, kind="ExternalOutput").ap()
    import contextlib
    with contextlib.ExitStack() as st:
        sb = lambda n, s, d: st.enter_context(nc.sbuf_tensor(n, s, d))
        cs = sb("cs", [128, 32], F32)
        cs2 = sb("cs2", [128, 32], F32)
        bsb = sb("bsb", [1, 2 * NCOL], F32)
        ysb = sb("ysb", [1, 2 * NCOL], F32)
        wt = [sb("wt%d" % i, [128, 32, 512], F32) for i in range(2)]
        ps = [st.enter_context(nc.psum_tensor("ps%d" % i, [128, 512], F32)) for i in range(2)]
        P = Prog(nc)
        P.dma("sp", cs[:], cT, writes=["cs"])
        P.dma("sp", bsb[:], b, writes=["bsb"])
        P.op("act", lambda e: e.activation(out=cs2[:], in_=cs[:], func=AF.Silu), reads=["cs"], writes=["cs2"])
        nb = 0
        for l in range(2):
            wl = w[l * D:(l + 1) * D, :].rearrange("(kc p) n -> p kc n", p=128)
            for j in range(NCOL // 512):
                s = nb % 2
                for q in range(4):
                    P.dma("sp", wt[s][:, q * 8:(q + 1) * 8, :], wl[:, q * 8:(q + 1) * 8, j * 512:(j + 1) * 512],
                          writes=[("wt", s, q)])

                def mm(e, s=s):
                    for kc in range(32):
                        ins = e.matmul(ps[s][0:1, :], lhsT=cs2[:, kc:kc + 1], rhs=wt[s][:, kc, :],
                                       start=(kc == 0), stop=(kc == 31))
                    return ins
                P.op("pe", mm, reads=["cs2"] + [("wt", s, q) for q in range(4)], writes=[("ps", s)])
                o0 = l * NCOL + j * 512
                P.op("dve", lambda e, s=s, o0=o0: e.tensor_tensor(out=ysb[0:1, o0:o0 + 512], in0=ps[s][0:1, :],
                                                                   in1=bsb[0:1, o0:o0 + 512], op=ALU.add),
                     reads=[("ps", s), "bsb"], writes=[("ysb", nb)])
                nb += 1
        fin = P.dma("sp", y, ysb[:], reads=[("ysb", i) for i in range(nb)], writes=["y"])
        P.emit(final_wait_ops=[fin])
    return nc


import contextlib
import numpy as np

S = 8192
HD = 128
SCALE = HD ** -0.5
EPS = 1e-6
KPAD = S + 512


def build_sb(NU=3, NSLOT=32):
    nc = bass.Bass("TRN2", target_bir_lowering=False)
    qT = nc.dram_tensor("qT", [NU * 128, NSLOT * 128], BF16, kind="ExternalInput").ap()
    kT = nc.dram_tensor("kT", [NU * 128, KPAD], BF16, kind="ExternalInput").ap()
    vv = nc.dram_tensor("vv", [NU * KPAD, 128], BF16, kind="ExternalInput").ap()
    hg = nc.dram_tensor("hg", [NU * 128, 128], F32, kind="ExternalInput").ap()
    cst = nc.dram_tensor("cst", [128, 3 * 128], F32, kind="ExternalInput").ap()
    oo = nc.dram_tensor("oo", [NU * NSLOT * 128, 128], BF16, kind="ExternalOutput").ap()
    with contextlib.ExitStack() as st:
        sb = lambda n, s, d: st.enter_context(nc.sbuf_tensor(n, s, d))
        csb = sb("csb", [128, 384], F32)
        ident = sb("ident", [128, 128], BF16)
        zeros = sb("zeros", [128, 512], F32)
        q_sb = [sb("q%d" % i, [128, NSLOT * 128], BF16) for i in range(NU)]
        k_sb = [sb("k%d" % i, [128, KPAD], BF16) for i in range(NU)]
        v_sb = [sb("v%d" % i, [128, KPAD // 128, 128], BF16) for i in range(NU)]
        hg_sb = [sb("hg%d" % i, [128, 128], F32) for i in range(NU)]
        NS = 3
        NC4 = 4
        E = [sb("E%d" % i, [128, 512], F32) for i in range(NS)]
        SP = [sb("SP%d" % i, [128, 512], F32) for i in range(NS)]
        Cb = [sb("C%d" % i, [128, 512], F32) for i in range(NC4)]
        ARG = [sb("ARG%d" % i, [128, 512], F32) for i in range(NS)]
        A = [sb("A%d" % i, [128, 512], BF16) for i in range(NS)]
        AT = [sb("AT%d" % i, [128, 512], BF16) for i in range(NS)]
        junk = sb("junk", [128, 128], F32)
        ss = [sb("ss%d" % i, [128, 1], F32) for i in range(NU)]
        OO = [sb("OO%d" % i, [128, 128], BF16) for i in range(NU)]
        zb = [st.enter_context(nc.psum_tensor("zb%d" % i, [128, 512], F32)) for i in range(4)]
        tp = [st.enter_context(nc.psum_tensor("tp%d" % i, [128, 512], BF16)) for i in range(2)]
        ob = [st.enter_context(nc.psum_tensor("ob%d" % i, [128, 128], F32)) for i in range(2)]
        Mk = csb[:, 0:128]
        Mneg = csb[:, 128:256]
        P = Prog(nc)
        P.dma("sp", csb[:], cst, writes=["csb"])
        P.op("dve", lambda e: e.tensor_copy(out=ident[:], in_=csb[:, 256:384]), reads=["csb"], writes=["ident"])
        P.op("dve", lambda e: e.memset(zeros[:], 0.0), writes=["zeros"])
        outs = []
        for u in range(NU):
            P.dma("sp", q_sb[u][:], qT[u * 128:(u + 1) * 128, :], writes=[("q", u)])
            for h in range(2):
                c0 = h * (KPAD // 2)
                P.dma("sp", k_sb[u][:, c0:c0 + KPAD // 2], kT[u * 128:(u + 1) * 128, c0:c0 + KPAD // 2], writes=[("k", u, h)])
                nt = KPAD // 128 // 2
                P.dma("sp", v_sb[u][:, h * nt:(h + 1) * nt, :],
                      vv[u * KPAD + h * nt * 128:u * KPAD + (h + 1) * nt * 128, :].rearrange("(t p) d -> p t d", p=128),
                      writes=[("v", u, h)])
            P.dma("sp", hg_sb[u][:], hg[u * 128:(u + 1) * 128, :], writes=[("hg", u)])
        items = []
        last = {}
        def add(u, p, m, nblk, start):
            items.append(dict(u=u, p=p, m=m, nblk=nblk, c0=start + 512 * m, prev=last.get(u)))
            last[u] = len(items) - 1
        for p in range(NSLOT):
            start = 128 * (2 * (NSLOT - 1) - 2 * p)
            nblk = p // 2 + 1
            for m in range(nblk):
                for u in range(min(2, NU)):
                    add(u, p, m, nblk, start)
        for u in range(2, NU):
            for p in range(NSLOT):
                start = 128 * (2 * (NSLOT - 1) - 2 * p)
                nblk = p // 2 + 1
                for m in range(nblk):
                    add(u, p, m, nblk, start)

        def S0(n):
            it = items[n]
            z = n % 4
            u, p, c0 = it["u"], it["p"], it["c0"]
            P.op("pe", lambda e: e.matmul(zb[z][:], lhsT=q_sb[u][:, 128 * p:128 * p + 128], rhs=k_sb[u][:, c0:c0 + 512], start=True, stop=True),
                 reads=[("q", u), ("k", u, 0), ("k", u, 1)], writes=[("zb", z)])

        def S1(n):
            b = n % NS
            z = n % 4
            P.op("act", lambda e: e.activation(out=E[b][:], in_=zb[z][:], func=AF.Exp, scale=SCALE), reads=[("zb", z)], writes=[("E", b)])
            P.op("act", lambda e: e.activation(out=SP[b][:], in_=E[b][:], func=AF.Ln, bias=1.0), reads=[("E", b)], writes=[("SP", b)])

        def S2(n):
            it = items[n]
            b = n % NS
            cb = n % NC4
            m, u = it["m"], it["u"]
            if m == 0:
                P.op("dve", lambda e: e.tensor_tensor(out=SP[b][:, 0:128], in0=SP[b][:, 0:128], in1=Mk, op=ALU.mult),
                     reads=[("SP", b), "csb"], writes=[("SP", b)])
                init = 0.0
                rd = []
            else:
                pb = it["prev"] % NC4
                init = Cb[pb][:, 511:512]
                rd = [("C", pb)]
            P.op("dve", lambda e: e.tensor_tensor_scan(out=Cb[cb][:], data0=SP[b][:], data1=zeros[:], initial=init, op0=ALU.add, op1=ALU.add),
                 reads=[("SP", b), "zeros"] + rd, writes=[("C", cb)])
            P.op("dve", lambda e: e.scalar_tensor_tensor(out=ARG[b][:], in0=zb[n % 4][:], scalar=SCALE, in1=Cb[cb][:], op0=ALU.mult, op1=ALU.subtract),
                 reads=[("zb", n % 4), ("C", cb)], writes=[("ARG", b)])
            if m == 0:
                P.op("dve", lambda e: e.tensor_tensor(out=ARG[b][:, 0:128], in0=ARG[b][:, 0:128], in1=Mneg, op=ALU.add),
                     reads=[("ARG", b), "csb"], writes=[("ARG", b)])

        def S3(n):
            b = n % NS
            t = n % 2
            P.op("act", lambda e: e.activation(out=A[b][:], in_=ARG[b][:], func=AF.Exp), reads=[("ARG", b)], writes=[("A", b)])

            def tr(e):
                for c in range(4):
                    ins = e.transpose(out=tp[t][:, 128 * c:128 * c + 128], in_=A[b][:, 128 * c:128 * c + 128], identity=ident[:])
                return ins
            P.op("pe", tr, reads=[("A", b), "ident"], writes=[("tp", t)])

        def S4(n):
            it = items[n]
            b = n % NS
            t = n % 2
            u, p, c0, m, nblk = it["u"], it["p"], it["c0"], it["m"], it["nblk"]
            if n % 3 == 0:
                P.op("dve", lambda e: e.tensor_copy(out=AT[b][:], in_=tp[t][:]), reads=[("tp", t)], writes=[("AT", b)])
            else:
                P.op("act", lambda e: e.activation(out=AT[b][:], in_=tp[t][:], func=AF.Copy), reads=[("tp", t)], writes=[("AT", b)])

            def pv(e):
                for c in range(4):
                    ins = e.matmul(ob[u % 2][:], lhsT=AT[b][:, 128 * c:128 * c + 128], rhs=v_sb[u][:, c0 // 128 + c, :],
                                   start=(m == 0 and c == 0), stop=(m == nblk - 1 and c == 3))
                return ins
            P.op("pe", pv, reads=[("AT", b), ("v", u, 0), ("v", u, 1)], writes=[("ob", u % 2)])
            if m != nblk - 1:
                return
            P.op("act", lambda e: e.activation(out=junk[:], in_=ob[u % 2][:], func=AF.Square, accum_out=ss[u][:]),
                 reads=[("ob", u % 2)], writes=["junk", ("ss", u)])
            P.op("act", lambda e: e.activation(out=ss[u][:], in_=ss[u][:], func=AF.Ln, scale=1.0 / HD, bias=EPS),
                 reads=[("ss", u)], writes=[("ss", u)])
            P.op("act", lambda e: e.activation(out=ss[u][:], in_=ss[u][:], func=AF.Exp, scale=-0.5),
                 reads=[("ss", u)], writes=[("ss", u)])
            P.op("dve", lambda e: e.scalar_tensor_tensor(out=OO[u][:], in0=ob[u % 2][:], scalar=ss[u][:, 0:1], in1=hg_sb[u][:], op0=ALU.mult, op1=ALU.mult),
                 reads=[("ob", u % 2), ("ss", u), ("hg", u)], writes=[("OO", u)])
            r0 = (u * NSLOT + p) * 128
            outs.append(P.dma("sp", oo[r0:r0 + 128, :], OO[u][:], reads=[("OO", u)], writes=[("oo", u, p)]))

        N = len(items)
        S0(0)
        for step in range(N + 3):
            if step + 1 < N:
                S0(step + 1)
            if step < N:
                S1(step)
            if 0 <= step - 1 < N:
                S2(step - 1)
            if 0 <= step - 2 < N:
                S3(step - 2)
            if 0 <= step - 3 < N:
                S4(step - 3)
        P.emit(final_wait_ops=outs)
    return nc


def sb_consts():
    tl = np.arange(128)[:, None]
    cl = np.arange(128)[None, :]
    M = (cl + tl >= 128).astype(np.float32)
    Mneg = np.where(M > 0, 0.0, -30000.0).astype(np.float32)
    return np.concatenate([M, Mneg, np.eye(128, dtype=np.float32)], axis=1)


def sb_unit_inputs(qT_h, kT_h, vT_h, par, NSLOT=32):
    tiles = [2 * p + par for p in range(NSLOT)]
    q = np.concatenate([qT_h[:, 128 * j:128 * j + 128] for j in tiles], axis=1)
    krev = kT_h[:, ::-1]
    vrev = vT_h[:, ::-1]
    sh = 128 if par == 0 else 0
    k = np.zeros((128, KPAD), dtype=kT_h.dtype)
    v = np.zeros((128, KPAD), dtype=vT_h.dtype)
    k[:, :S - sh] = krev[:, sh:]
    v[:, :S - sh] = vrev[:, sh:]
    return np.ascontiguousarray(q), k, np.ascontiguousarray(v.T)

import contextlib
import numpy as np

S = 8192
HD = 128
SCALE = HD ** -0.5
EPS = 1e-6
NT = S // 128


def build_chunk():
    nc = bass.Bass("TRN2", target_bir_lowering=False)
    qT = nc.dram_tensor("qT", [128, S], BF16, kind="ExternalInput").ap()
    kT = nc.dram_tensor("kT", [128, S], BF16, kind="ExternalInput").ap()
    vv = nc.dram_tensor("vv", [S, 128], BF16, kind="ExternalInput").ap()
    bT = nc.dram_tensor("bT", [128, 5 * 128], F32, kind="ExternalInput").ap()
    hg = nc.dram_tensor("hg", [128, 128], F32, kind="ExternalInput").ap()
    idn = nc.dram_tensor("idn", [128, 128], F32, kind="ExternalInput").ap()
    oo = nc.dram_tensor("oo", [S, 128], BF16, kind="ExternalOutput").ap()
    with contextlib.ExitStack() as st:
        sb = lambda n, s, d: st.enter_context(nc.sbuf_tensor(n, s, d))
        q_sb = sb("q_sb", [128, S], BF16)
        k_sb = sb("k_sb", [128, S], BF16)
        v_sb = sb("v_sb", [128, NT, 129], BF16)
        b_sb = sb("b_sb", [128, 640], BF16)
        ident = sb("ident", [128, 128], BF16)
        hg_sb = sb("hg_sb", [128, 128], F32)
        PT = [sb("PT%d" % i, [128, 640], BF16) for i in range(3)]
        o1 = [sb("o1%d" % i, [128, 128], F32) for i in range(2)]
        rd = [sb("rd%d" % i, [128, 1], F32) for i in range(2)]
        ss = [sb("ss%d" % i, [128, 1], F32) for i in range(2)]
        junk = sb("junk", [128, 128], F32)
        OO = [sb("OO%d" % i, [128, 128], BF16) for i in range(2)]
        st0 = [st.enter_context(nc.psum_tensor("st0%d" % i, [128, 512], F32)) for i in range(3)]
        st1 = [st.enter_context(nc.psum_tensor("st1%d" % i, [128, 128], F32)) for i in range(3)]
        ob = [st.enter_context(nc.psum_tensor("ob%d" % i, [128, 129], F32)) for i in range(2)]
        P = Prog(nc)
        for h in range(2):
            c0 = h * (S // 2)
            P.dma("sp", q_sb[:, c0:c0 + S // 2], qT[:, c0:c0 + S // 2], writes=[("qraw", h)])
            P.dma("sp", k_sb[:, c0:c0 + S // 2], kT[:, c0:c0 + S // 2], writes=[("k", h)])
            P.dma("sp", v_sb[:, h * 32:(h + 1) * 32, 0:128],
                  vv[h * 32 * 128:(h + 1) * 32 * 128, :].rearrange("(t p) d -> p t d", p=128), writes=[("v", h)])
        P.op("dve", lambda e: e.memset(v_sb[:, :, 128:129], 1.0), writes=["vone"])
        P.dma("pool", b_sb[:], bT, writes=["b"])
        P.dma("pool", ident[:], idn, writes=["ident"])
        P.dma("sp", hg_sb[:], hg, writes=["hg"])
        for blk in range(16):
            P.op("act", lambda e, blk=blk: e.activation(out=q_sb[:, blk * 512:(blk + 1) * 512], in_=q_sb[:, blk * 512:(blk + 1) * 512],
                                                        func=AF.Copy, scale=SCALE),
                 reads=[("qraw", blk // 8)], writes=[("q", blk)])
        outs = []

        def stageA(q):
            b3 = q % 3
            kks = [kk for kk in range(q - 4, q + 1) if kk >= 0]
            qread = [("q", q // 4)]

            def sc(e):
                for i, kk in enumerate(kks):
                    dl = q - kk
                    dst = st0[b3][:, 128 * i:128 * i + 128] if i < 4 else st1[b3][:, :]
                    e.matmul(dst, lhsT=k_sb[:, 128 * kk:128 * kk + 128], rhs=q_sb[:, 128 * q:128 * q + 128], start=True, stop=False)
                    ins = e.matmul(dst, lhsT=ident[:], rhs=b_sb[:, 128 * dl:128 * dl + 128], start=False, stop=True)
                return ins
            P.op("pe", sc, reads=qread + [("k", 0), ("k", 1), "b", "ident"], writes=[("st0", b3), ("st1", b3)])
            n0 = min(4, len(kks))
            P.op("act", lambda e: e.activation(out=PT[b3][:, 0:128 * n0], in_=st0[b3][:, 0:128 * n0], func=AF.Exp),
                 reads=[("st0", b3)], writes=[("PT0", b3)])
            if len(kks) == 5:
                P.op("act", lambda e: e.activation(out=PT[b3][:, 512:640], in_=st1[b3][:, :], func=AF.Exp),
                     reads=[("st1", b3)], writes=[("PT1", b3)])

        def stageB(q):
            b = q % 2
            b3 = q % 3
            kks = [kk for kk in range(q - 4, q + 1) if kk >= 0]

            def pv(e):
                for i, kk in enumerate(kks):
                    ins = e.matmul(ob[b][:], lhsT=PT[b3][:, 128 * i:128 * i + 128], rhs=v_sb[:, kk, :],
                                   start=(i == 0), stop=(i == len(kks) - 1))
                return ins
            P.op("pe", pv, reads=[("PT0", b3), ("PT1", b3), ("v", 0), ("v", 1), "vone"], writes=[("ob", b)])
            P.op("dve", lambda e: e.reciprocal(out=rd[b][:], in_=ob[b][:, 128:129]), reads=[("ob", b)], writes=[("rd", b)])
            P.op("dve", lambda e: e.tensor_scalar(out=o1[b][:], in0=ob[b][:, 0:128], scalar1=rd[b][:, 0:1], scalar2=None, op0=ALU.mult),
                 reads=[("ob", b), ("rd", b)], writes=[("o1", b)])
            P.op("act", lambda e: e.activation(out=junk[:], in_=o1[b][:], func=AF.Square, accum_out=ss[b][:]),
                 reads=[("o1", b)], writes=["junk", ("ss", b)])
            P.op("act", lambda e: e.activation(out=ss[b][:], in_=ss[b][:], func=AF.Ln, scale=1.0 / HD, bias=EPS), reads=[("ss", b)], writes=[("ss", b)])
            P.op("act", lambda e: e.activation(out=ss[b][:], in_=ss[b][:], func=AF.Exp, scale=-0.5), reads=[("ss", b)], writes=[("ss", b)])
            P.op("dve", lambda e: e.scalar_tensor_tensor(out=OO[b][:], in0=o1[b][:], scalar=ss[b][:, 0:1], in1=hg_sb[:],
                                                         op0=ALU.mult, op1=ALU.mult),
                 reads=[("o1", b), ("ss", b), "hg"], writes=[("OO", b)])
            outs.append(P.dma("sp", oo[128 * q:128 * q + 128, :], OO[b][:], reads=[("OO", b)], writes=[("oo", q)]))

        for step in range(NT + 2):
            if step < NT:
                stageA(step)
            if step - 2 >= 0:
                stageB(step - 2)
        P.emit(final_wait_ops=outs)
    return nc


def chunk_bias(rel_bias_h):
    s = np.arange(128)[:, None]
    t = np.arange(128)[None, :]
    out = np.zeros((128, 5, 128), np.float32)
    for dl in range(5):
        idx = np.clip(128 * dl + t - s, -256, 256) + 256
        blk = rel_bias_h[idx].astype(np.float32)
        if dl == 0:
            blk = np.where((t < 64) & (s >= 64), np.float32(-30000.0), blk)
        if dl == 4:
            blk = np.where((t >= 64) & (s < 64), np.float32(-30000.0), blk)
        out[:, dl, :] = blk
    return out.reshape(128, 640)

import contextlib
import numpy as np

S = 8192
HD = 128
EPS = 1e-6
NIT = 20
IDX_SCALE = (16 ** -0.5) * (64 ** -0.5)
TOPK = 256
NEG = -30000.0
C_P128, C_P64, C_ID, C_PAD, C_DIAG, C_POW = 0, 128, 256, 384, 1408, 1536
CW = 1536 + 32


def build_dsa(NSLOT=8):
    nc = bass.Bass("TRN2", target_bir_lowering=False)
    qa = nc.dram_tensor("qa", [12 * 128, 1024], BF16, kind="ExternalInput").ap()
    iq = nc.dram_tensor("iq", [8 * 128, 1024], BF16, kind="ExternalInput").ap()
    iw = nc.dram_tensor("iw", [1024, 16], BF16, kind="ExternalInput").ap()
    ka = nc.dram_tensor("ka", [2 * 128, S], BF16, kind="ExternalInput").ap()
    va = nc.dram_tensor("va", [S, 256], BF16, kind="ExternalInput").ap()
    ik = nc.dram_tensor("ik", [64, S], BF16, kind="ExternalInput").ap()
    tq = nc.dram_tensor("tq", [128, 2 * 1024], F32, kind="ExternalInput").ap()
    tiq = nc.dram_tensor("tiq", [128, 2 * 1024], F32, kind="ExternalInput").ap()
    tk = nc.dram_tensor("tk", [128, 2 * S], F32, kind="ExternalInput").ap()
    tik = nc.dram_tensor("tik", [128, 2 * S], F32, kind="ExternalInput").ap()
    cst = nc.dram_tensor("cst", [128, CW], F32, kind="ExternalInput").ap()
    hg = nc.dram_tensor("hg", [128, 12 * 128], F32, kind="ExternalInput").ap()
    oo = nc.dram_tensor("oo", [1024, 12 * 128], BF16, kind="ExternalOutput").ap()
    qav = qa.rearrange("(h p) t -> p h t", p=128)
    iqv = iq.rearrange("(h p) t -> p h t", p=128)
    with contextlib.ExitStack() as st:
        sb = lambda n, s, d: st.enter_context(nc.sbuf_tensor(n, s, d))
        csb = sb("csb", [128, CW], F32)
        pl128 = sb("pl128", [128, 128], BF16)
        pl64 = sb("pl64", [128, 128], BF16)
        ident3 = sb("ident3", [128, 384], BF16)
        hg_sb = sb("hg_sb", [128, 1536], F32)
        kaT = [sb("kaT%d" % g, [128, S], BF16) for g in range(2)]
        ikT = sb("ikT", [128, S], BF16)
        va_sb = sb("va_sb", [128, 64, 2, 129], BF16)
        score = sb("score", [128, S], F32)
        Mbs = [sb("Mb%d" % i, [128, S], BF16) for i in range(2)]
        qa_ss = [sb("qa_s%d" % i, [128, 12 * 128], BF16) for i in range(3)]
        iq_s = sb("iq_s", [128, 8 * 128], BF16)
        iw_b = sb("iw_b", [128, 16], BF16)
        iw_s = sb("iw_s", [128, 16], F32)
        dg = sb("dg", [128, 16 * 128], BF16)
        tqs = sb("tqs", [128, 2, 128], F32)
        tiqs = sb("tiqs", [128, 2, 128], F32)
        cosb = [score[:, 512 * i:512 * i + 512] for i in range(2)]
        sinb = [score[:, 512 * (2 + i):512 * (2 + i) + 512] for i in range(2)]
        t1 = [score[:, 512 * (4 + i):512 * (4 + i) + 512] for i in range(2)]
        t2 = [score[:, 512 * (6 + i):512 * (6 + i) + 512] for i in range(2)]
        t1s = [sb("t1s%d" % i, [128, 128], F32) for i in range(2)]
        t2s = [sb("t2s%d" % i, [128, 128], F32) for i in range(2)]
        R = [sb("R%d" % i, [128, 512], BF16) for i in range(4)]
        pT = [sb("pT%d" % i, [128, 384], BF16) for i in range(3)]
        Rm = sb("Rm", [128, 1], F32)
        steps = sb("steps", [128, 32], F32)
        cand = sb("cand", [128, 1], F32)
        cnt = sb("cnt", [128, 1], F32)
        sB = sb("sB", [128, 1], F32)
        tt = sb("tt", [128, 1], F32)
        thr = sb("thr", [128, 1], F32)
        rd = [sb("rd%d" % i, [128, 1], F32) for i in range(3)]
        ss = [sb("ss%d" % i, [128, 1], F32) for i in range(3)]
        o1 = [sb("o1%d" % i, [128, 128], F32) for i in range(3)]
        junk = sb("junk", [128, 128], F32)
        OO = [sb("OO%d" % i, [128, 128], BF16) for i in range(3)]
        rp = st.enter_context(nc.psum_tensor("rp", [128, 512], F32))
        dt = [st.enter_context(nc.psum_tensor("dt%d" % i, [128, 512], F32)) for i in range(4)]
        ac = [st.enter_context(nc.psum_tensor("ac%d" % i, [128, 512], F32)) for i in range(3)]
        P = Prog(nc)
        P.dma("sp", csb[:], cst, writes=["csb"])
        P.dma("sp", hg_sb[:], hg, writes=["hg"])
        P.op("dve", lambda e: e.tensor_copy(out=pl128[:], in_=csb[:, C_P128:C_P128 + 128]), reads=["csb"], writes=["pl128"])
        P.op("dve", lambda e: e.tensor_copy(out=pl64[:], in_=csb[:, C_P64:C_P64 + 128]), reads=["csb"], writes=["pl64"])
        for r in range(3):
            P.op("dve", lambda e, r=r: e.tensor_copy(out=ident3[:, 128 * r:128 * r + 128], in_=csb[:, C_ID:C_ID + 128]),
                 reads=["csb"], writes=[("ident3", r)])
        id3r = [("ident3", r) for r in range(3)]
        identf = csb[:, C_ID:C_ID + 128]
        for g in range(2):
            for h in range(2):
                c0 = h * (S // 2)
                P.dma("sp", kaT[g][:, c0:c0 + S // 2], ka[g * 128:(g + 1) * 128, c0:c0 + S // 2], writes=[("ka", g, c0 // 512 + b) for b in range(8)])
        for h in range(2):
            c0 = h * (S // 2)
            P.dma("sp", ikT[0:64, c0:c0 + S // 2], ik[:, c0:c0 + S // 2], writes=[("ikl", c0 // 512 + b) for b in range(8)])
            P.dma("sp", ikT[64:128, c0:c0 + S // 2], ik[:, c0:c0 + S // 2], writes=[("ikh", c0 // 512 + b) for b in range(8)])
            for g in range(2):
                P.dma("sp", va_sb[:, h * 32:(h + 1) * 32, g, 0:128],
                      va[h * 32 * 128:(h + 1) * 32 * 128, 128 * g:128 * g + 128].rearrange("(t p) d -> p t d", p=128), writes=[("va", h, g)])
        P.op("dve", lambda e: e.memset(va_sb[:, :, :, 128:129], 1.0), writes=["vone"])
        vreads = [("va", h, g) for h in range(2) for g in range(2)] + ["vone"]
        rn = [0]

        def rope(xap, keys, pl, plkey, cos_ap, sin_ap, tabkeys, n, small=False):
            b = rn[0] % 2
            rn[0] += 1
            if small:
                ta, tb_, ka_, kb_ = t1s[b][:, 0:n], t2s[b][:, 0:n], ("t1s", b), ("t2s", b)
            else:
                ta, tb_, ka_, kb_ = t1[b][:, 0:n], t2[b][:, 0:n], ("score", 4 + b), ("score", 6 + b)
            P.op("pe", lambda e: e.matmul(rp[:, 0:n], lhsT=pl[:], rhs=xap, start=True, stop=True),
                 reads=keys + [plkey], writes=["rp"])
            P.op("pool", lambda e: e.tensor_tensor(out=ta, in0=xap, in1=cos_ap, op=ALU.mult),
                 reads=keys + tabkeys, writes=[ka_])
            P.op("dve", lambda e: e.tensor_tensor(out=tb_, in0=rp[:, 0:n], in1=sin_ap, op=ALU.mult),
                 reads=["rp"] + tabkeys, writes=[kb_])
            P.op("pool", lambda e: e.tensor_tensor(out=xap, in0=ta, in1=tb_, op=ALU.add),
                 reads=[ka_, kb_], writes=keys)

        tn = 0
        for blk in range(15, -1, -1):
            c0 = blk * 512
            tb = tn % 2
            tn += 1
            P.dma("sp", cosb[tb], tk[:, c0:c0 + 512], writes=[("score", tb)])
            P.dma("sp", sinb[tb], tk[:, S + c0:S + c0 + 512], writes=[("score", 2 + tb)])
            for g in range(2):
                rope(kaT[g][:, c0:c0 + 512], [("ka", g, blk)], pl128, "pl128", cosb[tb], sinb[tb],
                     [("score", tb), ("score", 2 + tb)], 512)
            tb = tn % 2
            tn += 1
            P.dma("sp", cosb[tb], tik[:, c0:c0 + 512], writes=[("score", tb)])
            P.dma("sp", sinb[tb], tik[:, S + c0:S + c0 + 512], writes=[("score", 2 + tb)])
            rope(ikT[:, c0:c0 + 512], [("ikl", blk), ("ikh", blk)], pl64, "pl64", cosb[tb], sinb[tb],
                 [("score", tb), ("score", 2 + tb)], 512)
        kareads = lambda g, c0, n: [("ka", g, b) for b in range(c0 // 512, (c0 + n - 1) // 512 + 1)]
        ikreads = lambda c0: [("ikl", c0 // 512), ("ikh", c0 // 512)]
        outs = []
        dnc = [0]

        def setup(i):
            qa_s = qa_ss[i % 3]
            qk_ = lambda h: ("qa_s", i % 3, h)
            P.dma("sp", qa_s[:].rearrange("p (h t) -> p h t", h=12), qav[:, :, 128 * i:128 * i + 128], writes=[qk_(h) for h in range(12)])
            P.dma("sp", iq_s[:].rearrange("p (h t) -> p h t", h=8), iqv[:, :, 128 * i:128 * i + 128], writes=[("iq_s", h) for h in range(8)])
            P.dma("sp", iw_b[:], iw[128 * i:128 * i + 128, :], writes=["iw_b"])
            P.dma("sp", tqs[:], tq.rearrange("p (a t) -> p a t", a=2)[:, :, 128 * i:128 * i + 128], writes=["tqs"])
            P.dma("sp", tiqs[:], tiq.rearrange("p (a t) -> p a t", a=2)[:, :, 128 * i:128 * i + 128], writes=["tiqs"])
            P.op("dve", lambda e: e.tensor_copy(out=iw_s[:], in_=iw_b[:]), reads=["iw_b"], writes=["iw_s"])
            for h in range(8):
                rope(iq_s[:, 128 * h:128 * h + 128], [("iq_s", h)], pl64, "pl64", tiqs[:, 0, :], tiqs[:, 1, :], ["tiqs"], 128, small=True)
            for h in range(16):
                P.op("dve", lambda e, h=h: e.tensor_scalar(out=dg[:, 128 * h:128 * h + 128], in0=identf, scalar1=iw_s[:, h:h + 1], scalar2=None, op0=ALU.mult),
                     reads=["csb", "iw_s"], writes=[("dg", h)])
            for h in range(12):
                rope(qa_s[:, 128 * h:128 * h + 128], [qk_(h)], pl128, "pl128", tqs[:, 0, :], tqs[:, 1, :], ["tqs"], 128, small=True)

        def indexer(i):
            r0 = 56 - 8 * i
            idx_items = [(b, h) for b in range(2 * i + 2) for h in range(16)]
            dn0 = dnc[0]

            def idx_dots(k):
                b, h = idx_items[k]
                c0 = 128 * r0 + 512 * b
                db = (dn0 + k) % 4
                hp, pair = h % 2, h // 2
                P.op("pe", lambda e: e.matmul(dt[db][:], lhsT=iq_s[64 * hp:64 * hp + 64, 128 * pair:128 * pair + 128],
                                              rhs=ikT[64 * hp:64 * hp + 64, c0:c0 + 512], start=True, stop=True),
                     reads=[("iq_s", pair)] + ikreads(c0), writes=[("dt", db)])

            def idx_relu(k):
                db = (dn0 + k) % 4
                if k % 3 == 2:
                    P.op("dve", lambda e: e.tensor_scalar(out=R[db][:], in0=dt[db][:], scalar1=0.0, scalar2=None, op0=ALU.max),
                         reads=[("dt", db)], writes=[("R", db)])
                else:
                    P.op("act", lambda e: e.activation(out=R[db][:], in_=dt[db][:], func=AF.Relu),
                         reads=[("dt", db)], writes=[("R", db)])

            def idx_acc(k):
                b, h = idx_items[k]
                db = (dn0 + k) % 4
                ab = b % 2
                P.op("pe", lambda e: e.matmul(ac[ab][:], lhsT=dg[:, 128 * h:128 * h + 128], rhs=R[db][:], start=(h == 0), stop=(h == 15)),
                     reads=[("R", db), ("dg", h)], writes=[("ac", ab)])
                if h == 15:
                    P.op("dve", lambda e: e.tensor_scalar(out=score[:, 512 * b:512 * b + 512], in0=ac[ab][:], scalar1=IDX_SCALE, scalar2=None, op0=ALU.mult),
                         reads=[("ac", ab)], writes=[("score", b)])
            NI = len(idx_items)
            for k in range(0, NI + 2, 2):
                for kk in (k, k + 1):
                    if kk < NI:
                        idx_dots(kk)
                for kk in (k, k + 1):
                    if kk < NI:
                        idx_relu(kk)
                for kk in (k - 2, k - 1):
                    if 0 <= kk < NI:
                        idx_acc(kk)
            dnc[0] += NI

        def bisect(i):
            L = 1024 * (i + 1)
            Mb = Mbs[i % 2]
            mk = ("Mb", i % 2)
            sreads = [("score", b) for b in range(2 * i + 2)]
            P.op("dve", lambda e: e.tensor_reduce(out=Rm[:], in_=score[:, 0:L], axis=AX.X, op=ALU.max, apply_absolute_value=True),
                 reads=sreads, writes=["Rm"])
            P.op("dve", lambda e: e.tensor_scalar(out=Rm[:], in0=Rm[:], scalar1=1.001, scalar2=1e-6, op0=ALU.mult, op1=ALU.add),
                 reads=["Rm"], writes=["Rm"])
            P.op("dve", lambda e: e.tensor_scalar(out=steps[:, 0:32], in0=csb[:, C_POW:C_POW + 32], scalar1=Rm[:, 0:1], scalar2=None, op0=ALU.mult),
                 reads=["Rm", "csb"], writes=["steps"])
            P.op("dve", lambda e: e.tensor_tensor(out=score[:, L - 1024:L], in0=score[:, L - 1024:L], in1=csb[:, C_PAD:C_PAD + 1024], op=ALU.add),
                 reads=sreads + ["csb"], writes=[("score", 2 * i), ("score", 2 * i + 1)])
            P.op("dve", lambda e: e.tensor_tensor(out=score[:, 0:128], in0=score[:, 0:128], in1=csb[:, C_DIAG:C_DIAG + 128], op=ALU.add),
                 reads=[("score", 0), "csb"], writes=[("score", 0)])
            P.op("dve", lambda e: e.memset(cand[:], 0.0), writes=["cand"])
            Lh = L // 2
            yield
            for k in range(1, NIT + 1):
                P.op("dve", lambda e: e.tensor_scalar(out=Mb[:, 0:Lh], in0=score[:, 0:Lh], scalar1=cand[:, 0:1], scalar2=None,
                                                      op0=ALU.is_ge, op1=ALU.add, accum_out=cnt[:]),
                     reads=sreads + ["cand"], writes=[(mk, "A"), "cnt"])
                P.op("act", lambda e: e.activation(out=Mb[:, Lh:L], in_=score[:, Lh:L], func=AF.Sign, scale=-1.0, bias=cand[:, 0:1], accum_out=sB[:]),
                     reads=sreads + ["cand"], writes=[(mk, "B"), "sB"])
                P.op("dve", lambda e: e.scalar_tensor_tensor(out=cnt[:], in0=sB[:], scalar=-0.5, in1=cnt[:], op0=ALU.mult, op1=ALU.add),
                     reads=["sB", "cnt"], writes=["cnt"])
                P.op("dve", lambda e, k=k: e.tensor_scalar(out=tt[:], in0=cnt[:], scalar1=TOPK - 0.5 - Lh / 2.0, scalar2=steps[:, k - 1:k],
                                                           op0=ALU.is_ge, op1=ALU.mult),
                     reads=["cnt", "steps"], writes=["tt"])
                P.op("dve", lambda e, k=k: e.scalar_tensor_tensor(out=cand[:], in0=tt[:], scalar=steps[:, k:k + 1], in1=cand[:],
                                                                  op0=ALU.subtract, op1=ALU.add),
                     reads=["tt", "steps", "cand"], writes=["cand"])
                yield
            P.op("dve", lambda e: e.tensor_tensor(out=thr[:], in0=cand[:], in1=steps[:, NIT:NIT + 1], op=ALU.subtract),
                 reads=["cand", "steps"], writes=["thr"])
            P.op("dve", lambda e: e.tensor_scalar(out=Mb[:, 0:L], in0=score[:, 0:L], scalar1=thr[:, 0:1], scalar2=NEG,
                                                  op0=ALU.is_lt, op1=ALU.mult),
                 reads=sreads + ["thr"], writes=[(mk, "A"), (mk, "B")])
            yield

        def attend(i):
            r0 = 56 - 8 * i
            Mb = Mbs[i % 2]
            mk = ("Mb", i % 2)
            qa_s = qa_ss[i % 3]
            nst = 8 * i + 8
            for g in range(2):
                for hh in range(2):
                    h0 = 6 * g + 3 * hh
                    dn0 = dnc[0]

                    def a_qk(stl, g=g, h0=h0, dn0=dn0):
                        rr = r0 + stl
                        kc0 = 128 * rr
                        db = (dn0 + stl) % 3

                        def qk(e):
                            e.matmul(dt[db][:, 0:384], lhsT=kaT[g][:, kc0:kc0 + 128], rhs=qa_s[:, 128 * h0:128 * h0 + 384], start=True, stop=False)
                            return e.matmul(dt[db][:, 0:384], lhsT=Mb[:, 128 * stl:128 * stl + 128], rhs=ident3[:], start=False, stop=True)
                        P.op("pe", qk, reads=kareads(g, kc0, 128) + [("qa_s", i % 3, h0 + x) for x in range(3)] + [(mk, "A"), (mk, "B")] + id3r, writes=[("dt", db)])
                        P.op("act", lambda e: e.activation(out=pT[db][:], in_=dt[db][:, 0:384], func=AF.Exp),
                             reads=[("dt", db)], writes=[("pT", db)])

                    def a_pv(stl, g=g, dn0=dn0):
                        rr = r0 + stl
                        db = (dn0 + stl) % 3

                        def pv(e):
                            for hl in range(3):
                                ins = e.matmul(ac[hl][:, 0:129], lhsT=pT[db][:, 128 * hl:128 * hl + 128], rhs=va_sb[:, rr, g, :],
                                               start=(stl == 0), stop=(stl == nst - 1))
                            return ins
                        P.op("pe", pv, reads=[("pT", db)] + vreads, writes=[("ac", 0), ("ac", 1), ("ac", 2)])
                    for k in range(nst + 2):
                        if k < nst:
                            a_qk(k)
                        if k - 2 >= 0:
                            a_pv(k - 2)
                        yield
                    dnc[0] += nst
                    for hl in range(3):
                        hd = h0 + hl
                        P.op("dve", lambda e, hl=hl: e.reciprocal(out=rd[hl][:], in_=ac[hl][:, 128:129]), reads=[("ac", hl)], writes=[("rd", hl)])
                        P.op("dve", lambda e, hl=hl: e.tensor_scalar(out=o1[hl][:], in0=ac[hl][:, 0:128], scalar1=rd[hl][:, 0:1], scalar2=None, op0=ALU.mult),
                             reads=[("ac", hl), ("rd", hl)], writes=[("o1", hl)])
                        P.op("act", lambda e, hl=hl: e.activation(out=junk[:], in_=o1[hl][:], func=AF.Square, accum_out=ss[hl][:]),
                             reads=[("o1", hl)], writes=["junk", ("ss", hl)])
                        P.op("act", lambda e, hl=hl: e.activation(out=ss[hl][:], in_=ss[hl][:], func=AF.Ln, scale=1.0 / HD, bias=EPS),
                             reads=[("ss", hl)], writes=[("ss", hl)])
                        P.op("act", lambda e, hl=hl: e.activation(out=ss[hl][:], in_=ss[hl][:], func=AF.Exp, scale=-0.5),
                             reads=[("ss", hl)], writes=[("ss", hl)])
                        P.op("dve", lambda e, hl=hl, hd=hd: e.scalar_tensor_tensor(out=OO[hl][:], in0=o1[hl][:], scalar=ss[hl][:, 0:1],
                                                                                    in1=hg_sb[:, 128 * hd:128 * hd + 128], op0=ALU.mult, op1=ALU.mult),
                             reads=[("o1", hl), ("ss", hl), "hg"], writes=[("OO", hl)])
                        outs.append(P.dma("sp", oo[128 * i:128 * i + 128, 128 * hd:128 * hd + 128], OO[hl][:], reads=[("OO", hl)],
                                          writes=[("oo", i, hd)]))

        def run_all(gen):
            for _ in gen:
                pass

        setup(0)
        for i in range(NSLOT):
            indexer(i)
            if i + 1 < NSLOT:
                setup(i + 1)
            if i == 0:
                run_all(bisect(0))
                continue
            gb = bisect(i)
            ga = attend(i - 1)
            n_att = 4 * (8 * (i - 1) + 8 + 2)
            per = max(1, (n_att + NIT) // (NIT + 1))
            done_a = False
            for _ in gb:
                if not done_a:
                    for _j in range(per):
                        try:
                            next(ga)
                        except StopIteration:
                            done_a = True
                            break
            if not done_a:
                run_all(ga)
        run_all(attend(NSLOT - 1))
        P.emit(final_wait_ops=outs)
    return nc


def rope_tables(pos, d):
    inv = (np.float32(10000.0) ** (-np.arange(0, d, 2, dtype=np.float32) / np.float32(d))).astype(np.float32)
    ang = (pos.astype(np.float32)[:, None] * inv[None, :]).astype(np.float32)
    return np.cos(ang).astype(np.float32).T, np.sin(ang).astype(np.float32).T


def dsa_consts(c):
    cst = np.zeros((128, CW), np.float32)
    k = np.arange(128)
    for kk in range(128):
        if kk >= 64:
            cst[kk, C_P128 + kk - 64] = -1.0
        else:
            cst[kk, C_P128 + kk + 64] = 1.0
        if kk % 64 >= 32:
            cst[kk, C_P64 + kk - 32] = -1.0
        else:
            cst[kk, C_P64 + kk + 32] = 1.0
    cst[:, C_ID:C_ID + 128] = np.eye(128, dtype=np.float32)
    col = np.arange(1024)[None, :]
    cst[:, C_PAD:C_PAD + 1024] = np.where(col >= 128 * (c + 1), np.float32(-1e30), np.float32(0.0))
    tl = np.arange(128)[:, None]
    cl = np.arange(128)[None, :]
    cst[:, C_DIAG:C_DIAG + 128] = np.where((tl < 64) & (cl < 64), np.float32(-1e30), np.float32(0.0))
    cst[:, C_POW:C_POW + 32] = (2.0 ** -np.arange(32, dtype=np.float64)).astype(np.float32)[None, :]
    return cst


def dsa_core_inputs(c, pT, hgain12):
    tiles = [c + 8 * i for i in range(8)]
    tok = np.concatenate([np.arange(128 * j, 128 * j + 128) for j in tiles])
    rprime = np.arange(64)
    ktile = 63 - (rprime + 7 - c)
    key = (128 * ktile[:, None] + (127 - np.arange(128))[None, :]).reshape(-1)
    valid = key >= 0
    keyc = np.where(valid, key, 0)

    def stream(rows):
        out = rows[:, keyc].copy()
        out[:, ~valid] = 0
        return out
    qa = np.ascontiguousarray(pT[0:1536][:, tok])
    iq = np.ascontiguousarray(pT[2048:3072][:, tok])
    iw = np.ascontiguousarray(pT[3136:3152][:, tok].T)
    ka = stream(pT[1536:1792])
    va = np.ascontiguousarray(stream(pT[1792:2048]).T)
    ik = stream(pT[3072:3136])
    sc = np.float32(HD ** -0.5)
    cq, sq = rope_tables(tok.astype(np.float32), 128)
    tq = np.concatenate([np.concatenate([cq, cq], 0) * sc, np.concatenate([sq, sq], 0) * sc], axis=1).astype(np.float32)
    ci, si = rope_tables(tok.astype(np.float32), 64)
    tiq = np.concatenate([np.concatenate([ci, ci, ci, ci], 0), np.concatenate([si, si, si, si], 0)], axis=1).astype(np.float32)
    ck, sk = rope_tables(keyc.astype(np.float32), 128)
    tk = np.concatenate([np.concatenate([ck, ck], 0), np.concatenate([sk, sk], 0)], axis=1).astype(np.float32)
    cik, sik = rope_tables(keyc.astype(np.float32), 64)
    tik = np.concatenate([np.concatenate([cik] * 4, 0), np.concatenate([sik] * 4, 0)], axis=1).astype(np.float32)
    hgb = np.ascontiguousarray(np.broadcast_to(hgain12[None, :], (128, 1536))).astype(np.float32)
    return {"qa": qa, "iq": iq, "iw": iw, "ka": ka, "va": va, "ik": ik, "tq": tq, "tiq": tiq, "tk": tk, "tik": tik,
            "cst": dsa_consts(c), "hg": hgb}, tok


NCORES = 8
CORES = list(range(NCORES))
DFF = 11008
DFFP = 11264
HC = DFFP // NCORES
PW = 10832
PWC = PW // NCORES
ADA_NC = 3072

_PROGS = {}


def _prog(name, fn):
    if name not in _PROGS:
        _PROGS[name] = fn()
    return _PROGS[name]


def _run(nc, in_maps):
    res = run_bass_kernel_spmd(nc, in_maps, core_ids=CORES)
    return res.results


def _vecl(v):
    return np.ascontiguousarray(np.asarray(v, np.float32).reshape(-1, 128).T)


def kernel(x, c, w_ada, b_ada, norm_attn_g, w_in, rel_bias, head_norm_g, w_out,
           norm_ffn_g, w_gate_up, w_down, final_norm_g):
    x = np.asarray(x); c = np.asarray(c)
    depth = w_ada.shape[0]
    nc0 = _prog("k0", build_k0)
    cT = np.ascontiguousarray(np.asarray(c[0], np.float32).reshape(32, 128).T)
    ims = []
    for i in range(NCORES):
        sl = slice(i * ADA_NC, (i + 1) * ADA_NC)
        ims.append({"cT": cT, "w": np.ascontiguousarray(np.asarray(w_ada)[:, :, sl].reshape(depth * D, ADA_NC)),
                    "b": np.ascontiguousarray(np.asarray(b_ada)[:, sl].reshape(1, depth * ADA_NC))})
    r = _run(nc0, ims)
    mod = np.concatenate([r[i]["y"].reshape(depth, ADA_NC) for i in range(NCORES)], axis=1)
    del ims
    xT = np.ascontiguousarray(np.asarray(x[0], np.float32).T)

    def norm_phase(xT, g, sc, sh, final=False):
        ncn = _prog("normF" if final else "norm", lambda: build_norm(final))
        ims = [{"xT": np.ascontiguousarray(xT[:, i * 1024:(i + 1) * 1024]), "gv": _vecl(g), "scv": _vecl(sc), "shv": _vecl(sh)}
               for i in range(NCORES)]
        r = _run(ncn, ims)
        return np.concatenate([r[i]["hT"] for i in range(NCORES)], axis=1)

    for l in range(depth):
        sh1, sc1, g1, sh2, sc2, g2 = np.split(mod[l], 6)
        hg = np.asarray(head_norm_g[l], np.float32)
        hT = norm_phase(xT, norm_attn_g[l], sc1, sh1)
        ncg = _prog("pr", lambda: build_gemm("raw", D, PWC))
        wl = np.asarray(w_in[l])
        r = _run(ncg, [{"aT": hT, "W": np.ascontiguousarray(wl[:, i * PWC:(i + 1) * PWC])} for i in range(NCORES)])
        pT = np.concatenate([r[i]["yT"] for i in range(NCORES)], axis=0)
        del hT, r
        mix = np.zeros((S, D), dtype=pT.dtype)
        nca = _prog("dsa", build_dsa)
        ims, toks = [], []
        for i in range(NCORES):
            im, tok = dsa_core_inputs(i, pT, hg[:1536])
            ims.append(im); toks.append(tok)
        r = _run(nca, ims)
        for i in range(NCORES):
            mix[toks[i], 0:1536] = r[i]["oo"]
        del ims, r
        ncs = _prog("sb", build_sb)
        cst = sb_consts()
        ims = []
        for i in range(NCORES):
            qs, ks, vs, hgs = [], [], [], []
            for u in range(3):
                n = 3 * i + u
                h, par = n // 2, n % 2
                q, k, v = sb_unit_inputs(pT[3152 + 128 * h:3152 + 128 * h + 128], pT[4688 + 128 * h:4688 + 128 * h + 128],
                                         pT[6224 + 128 * h:6224 + 128 * h + 128], par)
                qs.append(q); ks.append(k); vs.append(v)
                hgs.append(np.broadcast_to(hg[(12 + h) * 128:(13 + h) * 128][None, :], (128, 128)))
            ims.append({"qT": np.concatenate(qs, 0), "kT": np.concatenate(ks, 0), "vv": np.concatenate(vs, 0),
                        "hg": np.ascontiguousarray(np.concatenate(hgs, 0)).astype(np.float32), "cst": cst})
        r = _run(ncs, ims)
        for i in range(NCORES):
            o = r[i]["oo"].reshape(3, 32, 128, 128)
            for u in range(3):
                n = 3 * i + u
                h, par = n // 2, n % 2
                for p in range(32):
                    j = 2 * p + par
                    mix[128 * j:128 * j + 128, (12 + h) * 128:(13 + h) * 128] = o[u, p]
        del ims, r
        ncc = _prog("chunk", build_chunk)
        idn = np.eye(128, dtype=np.float32)
        ims = []
        for h in range(NCORES):
            ims.append({"qT": np.ascontiguousarray(pT[7760 + 128 * h:7760 + 128 * h + 128]),
                        "kT": np.ascontiguousarray(pT[8784 + 128 * h:8784 + 128 * h + 128]),
                        "vv": np.ascontiguousarray(pT[9808 + 128 * h:9808 + 128 * h + 128].T),
                        "bT": chunk_bias(np.asarray(rel_bias[l][h], np.float32)), "idn": idn,
                        "hg": np.ascontiguousarray(np.broadcast_to(hg[(24 + h) * 128:(25 + h) * 128][None, :], (128, 128))).astype(np.float32)})
        r = _run(ncc, ims)
        for h in range(NCORES):
            mix[:, (24 + h) * 128:(25 + h) * 128] = r[h]["oo"]
        del ims, r, pT
        mixT = np.ascontiguousarray(mix.T)
        del mix
        nco = _prog("op", lambda: build_gemm("res", D, 512))
        wl = np.asarray(w_out[l])
        r = _run(nco, [{"aT": mixT, "W": np.ascontiguousarray(wl[:, i * 512:(i + 1) * 512]),
                        "xT": np.ascontiguousarray(xT[i * 512:(i + 1) * 512]), "gvec": _vecl(g1[i * 512:(i + 1) * 512])}
                       for i in range(NCORES)])
        xT = np.concatenate([r[i]["yT"] for i in range(NCORES)], axis=0)
        del mixT, r
        h2T = norm_phase(xT, norm_ffn_g[l], sc2, sh2)
        ncu = _prog("gu", lambda: build_gemm("glu", D, HC))
        wgu = np.asarray(w_gate_up[l])
        ims = []
        for i in range(NCORES):
            W = np.zeros((D, 2 * HC), np.float32)
            lo, hi = i * HC, min(DFF, (i + 1) * HC)
            W[:, 0:hi - lo] = wgu[:, lo:hi]
            W[:, HC:HC + hi - lo] = wgu[:, DFF + lo:DFF + hi]
            ims.append({"aT": h2T, "W": W})
        r = _run(ncu, ims)
        actT = np.concatenate([r[i]["yT"] for i in range(NCORES)], axis=0)
        del ims, r, h2T
        ncd = _prog("dn", lambda: build_gemm("res", DFFP, 512))
        wd = np.zeros((DFFP, D), np.float32)
        wd[:DFF] = np.asarray(w_down[l])
        r = _run(ncd, [{"aT": actT, "W": np.ascontiguousarray(wd[:, i * 512:(i + 1) * 512]),
                        "xT": np.ascontiguousarray(xT[i * 512:(i + 1) * 512]), "gvec": _vecl(g2[i * 512:(i + 1) * 512])}
                       for i in range(NCORES)])
        xT = np.concatenate([r[i]["yT"] for i in range(NCORES)], axis=0)
        del actT, wd, r
    zero = np.zeros(D, np.float32)
    oT = norm_phase(xT, final_norm_g, zero, zero, final=True)
    return np.ascontiguousarray(oT.T)[None].astype(np.float32)
```

```python
import contextlib

import numpy as np
import concourse.bass as bass
import concourse.mybir as mybir
from concourse.bass_utils import run_bass_kernel_spmd

F32 = mybir.dt.float32
BF16 = mybir.dt.bfloat16
AF = mybir.ActivationFunctionType
ALU = mybir.AluOpType
AX = mybir.AxisListType

ENGS = ("pe", "act", "dve", "pool", "sp")


class Op:
    __slots__ = ("eng", "fn", "deps", "idx", "has_dep", "tok", "is_dma", "dsem")

    def __init__(self, eng, fn, deps, is_dma):
        self.eng = eng
        self.fn = fn
        self.deps = deps
        self.has_dep = False
        self.tok = None
        self.is_dma = is_dma
        self.dsem = None


class Prog:
    def __init__(self, nc, n_dma_sems=12):
        self.nc = nc
        self.ops = []
        self.last_writer = {}
        self.readers = {}
        self.n_dma_sems = n_dma_sems

    def op(self, eng, fn, reads=(), writes=(), dma=False):
        deps = []
        for b in reads:
            w = self.last_writer.get(b)
            if w is not None:
                deps.append(w)
        for b in writes:
            w = self.last_writer.get(b)
            if w is not None:
                deps.append(w)
            deps.extend(self.readers.get(b, ()))
        o = Op(eng, fn, deps, dma)
        for d in deps:
            d.has_dep = True
        for b in writes:
            self.last_writer[b] = o
            self.readers[b] = []
        for b in reads:
            self.readers.setdefault(b, []).append(o)
        self.ops.append(o)
        return o

    def dma(self, eng, out, in_, reads=(), writes=()):
        return self.op(eng, lambda e: e.dma_start(out=out, in_=in_), reads, writes, dma=True)

    def emit(self, final_wait_ops=()):
        nc = self.nc
        import contextlib
        fw_set = set(id(o) for o in final_wait_ops)
        with contextlib.ExitStack() as st:
            esem = {e: st.enter_context(nc.semaphore("s_" + e)) for e in ENGS}
            dsems = {e: [st.enter_context(nc.semaphore("d_%s%d" % (e, i))) for i in range(self.n_dma_sems)]
                     for e in ("sp", "pool", "act")}
            ecount = {e: 0 for e in ENGS}
            dcount = {e: [0] * self.n_dma_sems for e in dsems}
            drr = {e: 0 for e in dsems}
            per_eng = {e: [] for e in ENGS}
            for o in self.ops:
                per_eng[o.eng].append(o)
                if o.is_dma:
                    i = drr[o.eng] % self.n_dma_sems
                    drr[o.eng] += 1
                    prev = dcount[o.eng][i]
                    dcount[o.eng][i] += 16
                    o.dsem = (dsems[o.eng][i], prev)
                    o.tok = (dsems[o.eng][i], dcount[o.eng][i])
                else:
                    if o.has_dep or id(o) in fw_set:
                        ecount[o.eng] += 1
                        o.tok = (esem[o.eng], ecount[o.eng])
            block = st.enter_context(nc.Block())
            handles = {"pe": block.tensor, "act": block.scalar, "dve": block.vector,
                       "pool": block.gpsimd, "sp": block.sync}

            def make(ename):
                ops = per_eng[ename]

                def body(eng):
                    waited = {}
                    for o in ops:
                        need = {}
                        for d in o.deps:
                            if d.eng == "pe" and ename == "pe" and not d.is_dma:
                                continue
                            s, v = d.tok
                            k = id(s)
                            if waited.get(k, 0) >= v:
                                continue
                            if k not in need or need[k][1] < v:
                                need[k] = (s, v)
                        if o.is_dma:
                            s, prev = o.dsem
                            k = id(s)
                            if prev > 0 and waited.get(k, 0) < prev:
                                if k not in need or need[k][1] < prev:
                                    need[k] = (s, prev)
                        for k, (s, v) in need.items():
                            eng.wait_ge(s, v)
                            waited[k] = v
                        ins = o.fn(eng)
                        if o.tok is not None:
                            s, v = o.tok
                            ins.then_inc(s, 16 if o.is_dma else 1)
                    if ename == "sp":
                        for o in final_wait_ops:
                            s, v = o.tok
                            eng.wait_ge(s, v)
                return body

            for e in ENGS:
                if per_eng[e] or e == "sp":
                    handles[e](make(e))

import contextlib
import numpy as np

D = 4096
S = 8192
EPS = 1e-6


def build_norm(final=False, T=1024):
    nc = bass.Bass("TRN2", target_bir_lowering=False)
    xT = nc.dram_tensor("xT", [D, T], F32, kind="ExternalInput").ap()
    gv = nc.dram_tensor("gv", [128, 32], F32, kind="ExternalInput").ap()
    scv = nc.dram_tensor("scv", [128, 32], F32, kind="ExternalInput").ap()
    shv = nc.dram_tensor("shv", [128, 32], F32, kind="ExternalInput").ap()
    odt = F32 if final else BF16
    hT = nc.dram_tensor("hT", [D, T], odt, kind="ExternalOutput").ap()
    xv = xT.rearrange("(kc p) t -> p kc t", p=128)
    hv = hT.rearrange("(kc p) t -> p kc t", p=128)
    with contextlib.ExitStack() as st:
        sb = lambda n, s, d: st.enter_context(nc.sbuf_tensor(n, s, d))
        g_sb = sb("g_sb", [128, 32], F32)
        sc_sb = sb("sc_sb", [128, 32], F32)
        sh_sb = sb("sh_sb", [128, 32], F32)
        a_sb = sb("a_sb", [128, 32], F32)
        ones = sb("ones", [128, 128], F32)
        xss = [sb("xs%d" % i, [128, 32, 512], F32) for i in range(2)]
        sq = [sb("sq%d" % i, [128, 512], F32) for i in range(2)]
        rstd = sb("rstd", [128, 512], F32)
        tmp = [sb("tmp%d" % i, [128, 512], F32) for i in range(2)]
        ho = [sb("ho%d" % i, [128, 8, 512], odt) for i in range(2)]
        ps = st.enter_context(nc.psum_tensor("ps", [128, 512], F32))
        P = Prog(nc)
        P.dma("sp", g_sb[:], gv, writes=["g"])
        P.dma("sp", sc_sb[:], scv, writes=["sc"])
        P.dma("sp", sh_sb[:], shv, writes=["sh"])
        P.op("dve", lambda e: e.memset(ones[:], 1.0), writes=["ones"])
        P.op("dve", lambda e: e.scalar_tensor_tensor(out=a_sb[:], in0=sc_sb[:], scalar=1.0, in1=g_sb[:],
                                                     op0=ALU.add, op1=ALU.mult),
             reads=["sc", "g"], writes=["a"])
        outs = []
        for half in range(T // 512):
            for q in range(4):
                P.dma("sp", xss[half % 2][:, q * 8:(q + 1) * 8, :], xv[:, q * 8:(q + 1) * 8, half * 512:half * 512 + 512], writes=[("xs", half % 2, q)])
        for half in range(T // 512):
            t0 = half * 512
            xs = xss[half % 2]
            for kc in range(32):
                s = kc % 2
                P.op("act", lambda e, kc=kc, s=s, xs=xs: e.activation(out=sq[s][:], in_=xs[:, kc, :], func=AF.Square),
                     reads=[("xs", half % 2, kc // 8)], writes=[("sq", s)])
                P.op("pe", lambda e, kc=kc, s=s: e.matmul(ps[:], lhsT=ones[:], rhs=sq[s][:], start=(kc == 0), stop=(kc == 31)),
                     reads=[("sq", s), "ones"], writes=["ps"])
            P.op("dve", lambda e: e.tensor_scalar(out=rstd[:], in0=ps[:], scalar1=1.0 / D, scalar2=EPS,
                                                  op0=ALU.mult, op1=ALU.add), reads=["ps"], writes=["rstd"])
            P.op("act", lambda e: e.activation(out=rstd[:], in_=rstd[:], func=AF.Sqrt), reads=["rstd"], writes=["rstd"])
            P.op("dve", lambda e: e.reciprocal(out=rstd[:], in_=rstd[:]), reads=["rstd"], writes=["rstd"])
            for kc in range(32):
                s = kc % 2
                hs = (kc // 8) % 2
                P.op("dve", lambda e, kc=kc, s=s, xs=xs: e.tensor_tensor(out=tmp[s][:], in0=xs[:, kc, :], in1=rstd[:], op=ALU.mult),
                     reads=[("xs", half % 2, kc // 8), "rstd"], writes=[("tmp", s)])
                P.op("act", lambda e, kc=kc, s=s, hs=hs: e.activation(out=ho[hs][:, kc % 8, :], in_=tmp[s][:], func=AF.Identity,
                                                                       bias=sh_sb[:, kc:kc + 1], scale=a_sb[:, kc:kc + 1]),
                     reads=[("tmp", s), "a", "sh"], writes=[("ho", hs, kc % 8)])
                if kc % 8 == 7:
                    q = kc // 8
                    outs.append(P.dma("sp", hv[:, q * 8:(q + 1) * 8, t0:t0 + 512], ho[hs][:],
                                      reads=[("ho", hs, i) for i in range(8)], writes=[("hv", half, q)]))
        P.emit(final_wait_ops=outs)
    return nc


def build_gemm(mode, K, ncols, T=S):
    KC = K // 128
    nc = bass.Bass("TRN2", target_bir_lowering=False)
    wcols = 2 * ncols if mode == "glu" else ncols
    aT = nc.dram_tensor("aT", [K, T], BF16, kind="ExternalInput").ap()
    W = nc.dram_tensor("W", [K, wcols], F32, kind="ExternalInput").ap()
    av = aT.rearrange("(kc p) t -> p kc t", p=128)
    wv = W.rearrange("(kc p) n -> p kc n", p=128)
    if mode == "res":
        xT = nc.dram_tensor("xT", [ncols, T], F32, kind="ExternalInput").ap()
        gvec = nc.dram_tensor("gvec", [128, (ncols + 127) // 128], F32, kind="ExternalInput").ap()
        yT = nc.dram_tensor("yT", [ncols, T], F32, kind="ExternalOutput").ap()
    else:
        yT = nc.dram_tensor("yT", [ncols, T], BF16, kind="ExternalOutput").ap()
    blocks = [(c0, min(128, ncols - c0)) for c0 in range(0, ncols, 128)]
    gsz = 2 if mode == "glu" else 4
    groups = [blocks[i:i + gsz] for i in range(0, len(blocks), gsz)]
    KSUB = 16 if KC <= 32 else 8
    nsub = (KC + KSUB - 1) // KSUB
    NAB = 4
    with contextlib.ExitStack() as st:
        sb = lambda n, s, d: st.enter_context(nc.sbuf_tensor(n, s, d))
        nwb = 2 if len(groups) > 1 else 1
        wb = [sb("wb%d" % i, [128, KC, 512], BF16) for i in range(nwb)]
        ab = [sb("ab%d" % i, [128, KSUB, 512], BF16) for i in range(NAB)]
        pss = [st.enter_context(nc.psum_tensor("ps%d" % i, [128, 512], F32)) for i in range(8)]
        if mode == "res":
            g_sb = sb("g_sb", [128, (ncols + 127) // 128], F32)
            xt = [sb("xt%d" % i, [128, 512], F32) for i in range(16)]
            yo = [sb("yo%d" % i, [128, 512], F32) for i in range(4)]
        elif mode == "glu":
            sg = [sb("sg%d" % i, [128, 512], F32) for i in range(2)]
            yo = [sb("yo%d" % i, [128, 512], BF16) for i in range(4)]
        else:
            yo = [sb("yo%d" % i, [128, 512], BF16) for i in range(4)]
        P = Prog(nc)
        if mode == "res":
            P.dma("sp", g_sb[:], gvec, writes=["g"])
        outs = []
        units = []
        for gi, grp in enumerate(groups):
            for tb in range(T // 512):
                for su in range(nsub):
                    units.append((gi, tb, su))
        ginfo = {}
        for gi, grp in enumerate(groups):
            wl = []
            off = 0
            for (c0, cw) in grp:
                wl.append((c0, cw, off)); off += cw
            if mode == "glu":
                for (c0, cw) in grp:
                    wl.append((ncols + c0, cw, off)); off += cw
            ginfo[gi] = wl
        PFU = NAB - 1
        yon = [0]
        xtn = [0]
        xt_slot = {}

        def issue_load(u):
            gi, tb, su = units[u]
            t0 = tb * 512
            if su == 0 and tb == 0:
                ws = gi % nwb
                for q in range(0, KC, 8):
                    q1 = min(KC, q + 8)
                    for (wc0, cw, o) in ginfo[gi]:
                        P.dma("pool", wb[ws][:, q:q1, o:o + cw], wv[:, q:q1, wc0:wc0 + cw], writes=[("wb", ws, o, q)])
            k0 = su * KSUB
            k1 = min(KC, k0 + KSUB)
            s = u % NAB
            P.dma("sp", ab[s][:, 0:k1 - k0, :], av[:, k0:k1, t0:t0 + 512], writes=[("ab", s)])
            if mode == "res" and su == 0:
                for bi, (c0, cw) in enumerate(groups[gi]):
                    xs_ = xtn[0] % 16
                    xtn[0] += 1
                    xt_slot[(gi, tb, bi)] = xs_
                    P.dma("sp", xt[xs_][0:cw, :], xT[c0:c0 + cw, t0:t0 + 512], writes=[("xt", xs_)])

        for u in range(min(PFU, len(units))):
            issue_load(u)
        for u, (gi, tb, su) in enumerate(units):
            if u + PFU < len(units):
                issue_load(u + PFU)
            grp = groups[gi]
            wl = ginfo[gi]
            ws = gi % nwb
            t0 = tb * 512
            pset = (gi * (T // 512) + tb) % 2
            banks = [pss[pset * 4 + i] for i in range(len(wl))]
            bkeys = [("ps", pset * 4 + i) for i in range(len(wl))]
            k0 = su * KSUB
            k1 = min(KC, k0 + KSUB)
            wreads = [("wb", ws, o, q) for (_, _, o) in wl for q in range(0, KC, 8) if q < k1 and q + 8 > k0]
            s = u % NAB

            def mm(e, s=s, k0=k0, k1=k1, banks=banks, wl=wl, ws=ws):
                ins = None
                for bi, (wc0, cw, o) in enumerate(wl):
                    for kc in range(k0, k1):
                        ins = e.matmul(banks[bi][0:cw, :], lhsT=wb[ws][:, kc, o:o + cw], rhs=ab[s][:, kc - k0, :],
                                       start=(kc == 0), stop=(kc == KC - 1))
                return ins
            P.op("pe", mm, reads=[("ab", s)] + wreads, writes=bkeys)
            if su != nsub - 1:
                continue
            nb = len(grp)
            for bi, (c0, cw) in enumerate(grp):
                ys = yon[0] % 4
                yon[0] += 1
                if mode == "raw":
                    P.op("act", lambda e, bi=bi, cw=cw, ys=ys, banks=banks: e.activation(out=yo[ys][0:cw, :], in_=banks[bi][0:cw, :], func=AF.Copy),
                         reads=[bkeys[bi]], writes=[("yo", ys)])
                elif mode == "glu":
                    s2 = bi % 2
                    P.op("act", lambda e, bi=bi, cw=cw, s2=s2, banks=banks: e.activation(out=sg[s2][0:cw, :], in_=banks[bi][0:cw, :], func=AF.Silu),
                         reads=[bkeys[bi]], writes=[("sg", s2)])
                    P.op("dve", lambda e, bi=bi, cw=cw, s2=s2, ys=ys, banks=banks, nb=nb: e.tensor_tensor(out=yo[ys][0:cw, :], in0=banks[nb + bi][0:cw, :], in1=sg[s2][0:cw, :], op=ALU.mult),
                         reads=[bkeys[nb + bi], ("sg", s2)], writes=[("yo", ys)])
                else:
                    cb = c0 // 128
                    xs_ = xt_slot[(gi, tb, bi)]
                    P.op("dve", lambda e, bi=bi, cw=cw, ys=ys, cb=cb, banks=banks, xs_=xs_: e.scalar_tensor_tensor(
                        out=yo[ys][0:cw, :], in0=banks[bi][0:cw, :], scalar=g_sb[0:cw, cb:cb + 1], in1=xt[xs_][0:cw, :],
                        op0=ALU.mult, op1=ALU.add),
                        reads=[bkeys[bi], ("xt", xs_), "g"], writes=[("yo", ys)])
                outs.append(P.dma("sp", yT[c0:c0 + cw, t0:t0 + 512], yo[ys][0:cw, :], reads=[("yo", ys)],
                                  writes=[("y", c0, tb)]))
        P.emit(final_wait_ops=outs)
    return nc

D = 4096
NCOL = 3072


def build_k0():
    nc = bass.Bass("TRN2", target_bir_lowering=False)
    cT = nc.dram_tensor("cT", [128, 32], F32, kind="ExternalInput").ap()
    w = nc.dram_tensor("w", [2 * D, NCOL], F32, kind="ExternalInput").ap()
    b = nc.dram_tensor("b", [1, 2 * NCOL], F32, kind="ExternalInput").ap()
    y = nc.dram_tensor("y", [1, 2 * NCOL], F32, kind="ExternalOutput").ap()
    import contextlib
    with contextlib.ExitStack() as st:
        sb = lambda n, s, d: st.enter_context(nc.sbuf_tensor(n, s, d))
        cs = sb("cs", [128, 32], F32)
        cs2 = sb("cs2", [128, 32], F32)
        bsb = sb("bsb", [1, 2 * NCOL], F32)
        ysb = sb("ysb", [1, 2 * NCOL], F32)
        wt = [sb("wt%d" % i, [128, 32, 512], F32) for i in range(2)]
        ps = [st.enter_context(nc.psum_tensor("ps%d" % i, [128, 512], F32)) for i in range(2)]
        P = Prog(nc)
        P.dma("sp", cs[:], cT, writes=["cs"])
        P.dma("sp", bsb[:], b, writes=["bsb"])
        P.op("act", lambda e: e.activation(out=cs2[:], in_=cs[:], func=AF.Silu), reads=["cs"], writes=["cs2"])
        nb = 0
        for l in range(2):
            wl = w[l * D:(l + 1) * D, :].rearrange("(kc p) n -> p kc n", p=128)
            for j in range(NCOL // 512):
                s = nb % 2
                for q in range(4):
                    P.dma("sp", wt[s][:, q * 8:(q + 1) * 8, :], wl[:, q * 8:(q + 1) * 8, j * 512:(j + 1) * 512],
                          writes=[("wt", s, q)])

                def mm(e, s=s):
                    for kc in range(32):
                        ins = e.matmul(ps[s][0:1, :], lhsT=cs2[:, kc:kc + 1], rhs=wt[s][:, kc, :],
                                       start=(kc == 0), stop=(kc == 31))
                    return ins
                P.op("pe", mm, reads=["cs2"] + [("wt", s, q) for q in range(4)], writes=[("ps", s)])
                o0 = l * NCOL + j * 512
                P.op("dve", lambda e, s=s, o0=o0: e.tensor_tensor(out=ysb[0:1, o0:o0 + 512], in0=ps[s][0:1, :],
                                                                   in1=bsb[0:1, o0:o0 + 512], op=ALU.add),
                     reads=[("ps", s), "bsb"], writes=[("ysb", nb)])
                nb += 1
        fin = P.dma("sp", y, ysb[:], reads=[("ysb", i) for i in range(nb)], writes=["y"])
        P.emit(final_wait_ops=[fin])
    return nc


import contextlib
import numpy as np

S = 8192
HD = 128
SCALE = HD ** -0.5
EPS = 1e-6
KPAD = S + 512


def build_sb(NU=3, NSLOT=32):
    nc = bass.Bass("TRN2", target_bir_lowering=False)
    qT = nc.dram_tensor("qT", [NU * 128, NSLOT * 128], BF16, kind="ExternalInput").ap()
    kT = nc.dram_tensor("kT", [NU * 128, KPAD], BF16, kind="ExternalInput").ap()
    vv = nc.dram_tensor("vv", [NU * KPAD, 128], BF16, kind="ExternalInput").ap()
    hg = nc.dram_tensor("hg", [NU * 128, 128], F32, kind="ExternalInput").ap()
    cst = nc.dram_tensor("cst", [128, 3 * 128], F32, kind="ExternalInput").ap()
    oo = nc.dram_tensor("oo", [NU * NSLOT * 128, 128], BF16, kind="ExternalOutput").ap()
    with contextlib.ExitStack() as st:
        sb = lambda n, s, d: st.enter_context(nc.sbuf_tensor(n, s, d))
        csb = sb("csb", [128, 384], F32)
        ident = sb("ident", [128, 128], BF16)
        zeros = sb("zeros", [128, 512], F32)
        q_sb = [sb("q%d" % i, [128, NSLOT * 128], BF16) for i in range(NU)]
        k_sb = [sb("k%d" % i, [128, KPAD], BF16) for i in range(NU)]
        v_sb = [sb("v%d" % i, [128, KPAD // 128, 128], BF16) for i in range(NU)]
        hg_sb = [sb("hg%d" % i, [128, 128], F32) for i in range(NU)]
        NS = 3
        NC4 = 4
        E = [sb("E%d" % i, [128, 512], F32) for i in range(NS)]
        SP = [sb("SP%d" % i, [128, 512], F32) for i in range(NS)]
        Cb = [sb("C%d" % i, [128, 512], F32) for i in range(NC4)]
        ARG = [sb("ARG%d" % i, [128, 512], F32) for i in range(NS)]
        A = [sb("A%d" % i, [128, 512], BF16) for i in range(NS)]
        AT = [sb("AT%d" % i, [128, 512], BF16) for i in range(NS)]
        junk = sb("junk", [128, 128], F32)
        ss = [sb("ss%d" % i, [128, 1], F32) for i in range(NU)]
        OO = [sb("OO%d" % i, [128, 128], BF16) for i in range(NU)]
        zb = [st.enter_context(nc.psum_tensor("zb%d" % i, [128, 512], F32)) for i in range(4)]
        tp = [st.enter_context(nc.psum_tensor("tp%d" % i, [128, 512], BF16)) for i in range(2)]
        ob = [st.enter_context(nc.psum_tensor("ob%d" % i, [128, 128], F32)) for i in range(2)]
        Mk = csb[:, 0:128]
        Mneg = csb[:, 128:256]
        P = Prog(nc)
        P.dma("sp", csb[:], cst, writes=["csb"])
        P.op("dve", lambda e: e.tensor_copy(out=ident[:], in_=csb[:, 256:384]), reads=["csb"], writes=["ident"])
        P.op("dve", lambda e: e.memset(zeros[:], 0.0), writes=["zeros"])
        outs = []
        for u in range(NU):
            P.dma("sp", q_sb[u][:], qT[u * 128:(u + 1) * 128, :], writes=[("q", u)])
            for h in range(2):
                c0 = h * (KPAD // 2)
                P.dma("sp", k_sb[u][:, c0:c0 + KPAD // 2], kT[u * 128:(u + 1) * 128, c0:c0 + KPAD // 2], writes=[("k", u, h)])
                nt = KPAD // 128 // 2
                P.dma("sp", v_sb[u][:, h * nt:(h + 1) * nt, :],
                      vv[u * KPAD + h * nt * 128:u * KPAD + (h + 1) * nt * 128, :].rearrange("(t p) d -> p t d", p=128),
                      writes=[("v", u, h)])
            P.dma("sp", hg_sb[u][:], hg[u * 128:(u + 1) * 128, :], writes=[("hg", u)])
        items = []
        last = {}
        def add(u, p, m, nblk, start):
            items.append(dict(u=u, p=p, m=m, nblk=nblk, c0=start + 512 * m, prev=last.get(u)))
            last[u] = len(items) - 1
        for p in range(NSLOT):
            start = 128 * (2 * (NSLOT - 1) - 2 * p)
            nblk = p // 2 + 1
            for m in range(nblk):
                for u in range(min(2, NU)):
                    add(u, p, m, nblk, start)
        for u in range(2, NU):
            for p in range(NSLOT):
                start = 128 * (2 * (NSLOT - 1) - 2 * p)
                nblk = p // 2 + 1
                for m in range(nblk):
                    add(u, p, m, nblk, start)

        def S0(n):
            it = items[n]
            z = n % 4
            u, p, c0 = it["u"], it["p"], it["c0"]
            P.op("pe", lambda e: e.matmul(zb[z][:], lhsT=q_sb[u][:, 128 * p:128 * p + 128], rhs=k_sb[u][:, c0:c0 + 512], start=True, stop=True),
                 reads=[("q", u), ("k", u, 0), ("k", u, 1)], writes=[("zb", z)])

        def S1(n):
            b = n % NS
            z = n % 4
            P.op("act", lambda e: e.activation(out=E[b][:], in_=zb[z][:], func=AF.Exp, scale=SCALE), reads=[("zb", z)], writes=[("E", b)])
            P.op("act", lambda e: e.activation(out=SP[b][:], in_=E[b][:], func=AF.Ln, bias=1.0), reads=[("E", b)], writes=[("SP", b)])

        def S2(n):
            it = items[n]
            b = n % NS
            cb = n % NC4
            m, u = it["m"], it["u"]
            if m == 0:
                P.op("dve", lambda e: e.tensor_tensor(out=SP[b][:, 0:128], in0=SP[b][:, 0:128], in1=Mk, op=ALU.mult),
                     reads=[("SP", b), "csb"], writes=[("SP", b)])
                init = 0.0
                rd = []
            else:
                pb = it["prev"] % NC4
                init = Cb[pb][:, 511:512]
                rd = [("C", pb)]
            P.op("dve", lambda e: e.tensor_tensor_scan(out=Cb[cb][:], data0=SP[b][:], data1=zeros[:], initial=init, op0=ALU.add, op1=ALU.add),
                 reads=[("SP", b), "zeros"] + rd, writes=[("C", cb)])
            P.op("dve", lambda e: e.scalar_tensor_tensor(out=ARG[b][:], in0=zb[n % 4][:], scalar=SCALE, in1=Cb[cb][:], op0=ALU.mult, op1=ALU.subtract),
                 reads=[("zb", n % 4), ("C", cb)], writes=[("ARG", b)])
            if m == 0:
                P.op("dve", lambda e: e.tensor_tensor(out=ARG[b][:, 0:128], in0=ARG[b][:, 0:128], in1=Mneg, op=ALU.add),
                     reads=[("ARG", b), "csb"], writes=[("ARG", b)])

        def S3(n):
            b = n % NS
            t = n % 2
            P.op("act", lambda e: e.activation(out=A[b][:], in_=ARG[b][:], func=AF.Exp), reads=[("ARG", b)], writes=[("A", b)])

            def tr(e):
                for c in range(4):
                    ins = e.transpose(out=tp[t][:, 128 * c:128 * c + 128], in_=A[b][:, 128 * c:128 * c + 128], identity=ident[:])
                return ins
            P.op("pe", tr, reads=[("A", b), "ident"], writes=[("tp", t)])

        def S4(n):
            it = items[n]
            b = n % NS
            t = n % 2
            u, p, c0, m, nblk = it["u"], it["p"], it["c0"], it["m"], it["nblk"]
            if n % 3 == 0:
                P.op("dve", lambda e: e.tensor_copy(out=AT[b][:], in_=tp[t][:]), reads=[("tp", t)], writes=[("AT", b)])
            else:
                P.op("act", lambda e: e.activation(out=AT[b][:], in_=tp[t][:], func=AF.Copy), reads=[("tp", t)], writes=[("AT", b)])

            def pv(e):
                for c in range(4):
                    ins = e.matmul(ob[u % 2][:], lhsT=AT[b][:, 128 * c:128 * c + 128], rhs=v_sb[u][:, c0 // 128 + c, :],
                                   start=(m == 0 and c == 0), stop=(m == nblk - 1 and c == 3))
                return ins
            P.op("pe", pv, reads=[("AT", b), ("v", u, 0), ("v", u, 1)], writes=[("ob", u % 2)])
            if m != nblk - 1:
                return
            P.op("act", lambda e: e.activation(out=junk[:], in_=ob[u % 2][:], func=AF.Square, accum_out=ss[u][:]),
                 reads=[("ob", u % 2)], writes=["junk", ("ss", u)])
            P.op("act", lambda e: e.activation(out=ss[u][:], in_=ss[u][:], func=AF.Ln, scale=1.0 / HD, bias=EPS),
                 reads=[("ss", u)], writes=[("ss", u)])
            P.op("act", lambda e: e.activation(out=ss[u][:], in_=ss[u][:], func=AF.Exp, scale=-0.5),
                 reads=[("ss", u)], writes=[("ss", u)])
            P.op("dve", lambda e: e.scalar_tensor_tensor(out=OO[u][:], in0=ob[u % 2][:], scalar=ss[u][:, 0:1], in1=hg_sb[u][:], op0=ALU.mult, op1=ALU.mult),
                 reads=[("ob", u % 2), ("ss", u), ("hg", u)], writes=[("OO", u)])
            r0 = (u * NSLOT + p) * 128
            outs.append(P.dma("sp", oo[r0:r0 + 128, :], OO[u][:], reads=[("OO", u)], writes=[("oo", u, p)]))

        N = len(items)
        S0(0)
        for step in range(N + 3):
            if step + 1 < N:
                S0(step + 1)
            if step < N:
                S1(step)
            if 0 <= step - 1 < N:
                S2(step - 1)
            if 0 <= step - 2 < N:
                S3(step - 2)
            if 0 <= step - 3 < N:
                S4(step - 3)
        P.emit(final_wait_ops=outs)
    return nc


def sb_consts():
    tl = np.arange(128)[:, None]
    cl = np.arange(128)[None, :]
    M = (cl + tl >= 128).astype(np.float32)
    Mneg = np.where(M > 0, 0.0, -30000.0).astype(np.float32)
    return np.concatenate([M, Mneg, np.eye(128, dtype=np.float32)], axis=1)


def sb_unit_inputs(qT_h, kT_h, vT_h, par, NSLOT=32):
    tiles = [2 * p + par for p in range(NSLOT)]
    q = np.concatenate([qT_h[:, 128 * j:128 * j + 128] for j in tiles], axis=1)
    krev = kT_h[:, ::-1]
    vrev = vT_h[:, ::-1]
    sh = 128 if par == 0 else 0
    k = np.zeros((128, KPAD), dtype=kT_h.dtype)
    v = np.zeros((128, KPAD), dtype=vT_h.dtype)
    k[:, :S - sh] = krev[:, sh:]
    v[:, :S - sh] = vrev[:, sh:]
    return np.ascontiguousarray(q), k, np.ascontiguousarray(v.T)

import contextlib
import numpy as np

S = 8192
HD = 128
SCALE = HD ** -0.5
EPS = 1e-6
NT = S // 128


def build_chunk():
    nc = bass.Bass("TRN2", target_bir_lowering=False)
    qT = nc.dram_tensor("qT", [128, S], BF16, kind="ExternalInput").ap()
    kT = nc.dram_tensor("kT", [128, S], BF16, kind="ExternalInput").ap()
    vv = nc.dram_tensor("vv", [S, 128], BF16, kind="ExternalInput").ap()
    bT = nc.dram_tensor("bT", [128, 5 * 128], F32, kind="ExternalInput").ap()
    hg = nc.dram_tensor("hg", [128, 128], F32, kind="ExternalInput").ap()
    idn = nc.dram_tensor("idn", [128, 128], F32, kind="ExternalInput").ap()
    oo = nc.dram_tensor("oo", [S, 128], BF16, kind="ExternalOutput").ap()
    with contextlib.ExitStack() as st:
        sb = lambda n, s, d: st.enter_context(nc.sbuf_tensor(n, s, d))
        q_sb = sb("q_sb", [128, S], BF16)
        k_sb = sb("k_sb", [128, S], BF16)
        v_sb = sb("v_sb", [128, NT, 129], BF16)
        b_sb = sb("b_sb", [128, 640], BF16)
        ident = sb("ident", [128, 128], BF16)
        hg_sb = sb("hg_sb", [128, 128], F32)
        PT = [sb("PT%d" % i, [128, 640], BF16) for i in range(3)]
        o1 = [sb("o1%d" % i, [128, 128], F32) for i in range(2)]
        rd = [sb("rd%d" % i, [128, 1], F32) for i in range(2)]
        ss = [sb("ss%d" % i, [128, 1], F32) for i in range(2)]
        junk = sb("junk", [128, 128], F32)
        OO = [sb("OO%d" % i, [128, 128], BF16) for i in range(2)]
        st0 = [st.enter_context(nc.psum_tensor("st0%d" % i, [128, 512], F32)) for i in range(3)]
        st1 = [st.enter_context(nc.psum_tensor("st1%d" % i, [128, 128], F32)) for i in range(3)]
        ob = [st.enter_context(nc.psum_tensor("ob%d" % i, [128, 129], F32)) for i in range(2)]
        P = Prog(nc)
        for h in range(2):
            c0 = h * (S // 2)
            P.dma("sp", q_sb[:, c0:c0 + S // 2], qT[:, c0:c0 + S // 2], writes=[("qraw", h)])
            P.dma("sp", k_sb[:, c0:c0 + S // 2], kT[:, c0:c0 + S // 2], writes=[("k", h)])
            P.dma("sp", v_sb[:, h * 32:(h + 1) * 32, 0:128],
                  vv[h * 32 * 128:(h + 1) * 32 * 128, :].rearrange("(t p) d -> p t d", p=128), writes=[("v", h)])
        P.op("dve", lambda e: e.memset(v_sb[:, :, 128:129], 1.0), writes=["vone"])
        P.dma("pool", b_sb[:], bT, writes=["b"])
        P.dma("pool", ident[:], idn, writes=["ident"])
        P.dma("sp", hg_sb[:], hg, writes=["hg"])
        for blk in range(16):
            P.op("act", lambda e, blk=blk: e.activation(out=q_sb[:, blk * 512:(blk + 1) * 512], in_=q_sb[:, blk * 512:(blk + 1) * 512],
                                                        func=AF.Copy, scale=SCALE),
                 reads=[("qraw", blk // 8)], writes=[("q", blk)])
        outs = []

        def stageA(q):
            b3 = q % 3
            kks = [kk for kk in range(q - 4, q + 1) if kk >= 0]
            qread = [("q", q // 4)]

            def sc(e):
                for i, kk in enumerate(kks):
                    dl = q - kk
                    dst = st0[b3][:, 128 * i:128 * i + 128] if i < 4 else st1[b3][:, :]
                    e.matmul(dst, lhsT=k_sb[:, 128 * kk:128 * kk + 128], rhs=q_sb[:, 128 * q:128 * q + 128], start=True, stop=False)
                    ins = e.matmul(dst, lhsT=ident[:], rhs=b_sb[:, 128 * dl:128 * dl + 128], start=False, stop=True)
                return ins
            P.op("pe", sc, reads=qread + [("k", 0), ("k", 1), "b", "ident"], writes=[("st0", b3), ("st1", b3)])
            n0 = min(4, len(kks))
            P.op("act", lambda e: e.activation(out=PT[b3][:, 0:128 * n0], in_=st0[b3][:, 0:128 * n0], func=AF.Exp),
                 reads=[("st0", b3)], writes=[("PT0", b3)])
            if len(kks) == 5:
                P.op("act", lambda e: e.activation(out=PT[b3][:, 512:640], in_=st1[b3][:, :], func=AF.Exp),
                     reads=[("st1", b3)], writes=[("PT1", b3)])

        def stageB(q):
            b = q % 2
            b3 = q % 3
            kks = [kk for kk in range(q - 4, q + 1) if kk >= 0]

            def pv(e):
                for i, kk in enumerate(kks):
                    ins = e.matmul(ob[b][:], lhsT=PT[b3][:, 128 * i:128 * i + 128], rhs=v_sb[:, kk, :],
                                   start=(i == 0), stop=(i == len(kks) - 1))
                return ins
            P.op("pe", pv, reads=[("PT0", b3), ("PT1", b3), ("v", 0), ("v", 1), "vone"], writes=[("ob", b)])
            P.op("dve", lambda e: e.reciprocal(out=rd[b][:], in_=ob[b][:, 128:129]), reads=[("ob", b)], writes=[("rd", b)])
            P.op("dve", lambda e: e.tensor_scalar(out=o1[b][:], in0=ob[b][:, 0:128], scalar1=rd[b][:, 0:1], scalar2=None, op0=ALU.mult),
                 reads=[("ob", b), ("rd", b)], writes=[("o1", b)])
            P.op("act", lambda e: e.activation(out=junk[:], in_=o1[b][:], func=AF.Square, accum_out=ss[b][:]),
                 reads=[("o1", b)], writes=["junk", ("ss", b)])
            P.op("act", lambda e: e.activation(out=ss[b][:], in_=ss[b][:], func=AF.Ln, scale=1.0 / HD, bias=EPS), reads=[("ss", b)], writes=[("ss", b)])
            P.op("act", lambda e: e.activation(out=ss[b][:], in_=ss[b][:], func=AF.Exp, scale=-0.5), reads=[("ss", b)], writes=[("ss", b)])
            P.op("dve", lambda e: e.scalar_tensor_tensor(out=OO[b][:], in0=o1[b][:], scalar=ss[b][:, 0:1], in1=hg_sb[:],
                                                         op0=ALU.mult, op1=ALU.mult),
                 reads=[("o1", b), ("ss", b), "hg"], writes=[("OO", b)])
            outs.append(P.dma("sp", oo[128 * q:128 * q + 128, :], OO[b][:], reads=[("OO", b)], writes=[("oo", q)]))

        for step in range(NT + 2):
            if step < NT:
                stageA(step)
            if step - 2 >= 0:
                stageB(step - 2)
        P.emit(final_wait_ops=outs)
    return nc


def chunk_bias(rel_bias_h):
    s = np.arange(128)[:, None]
    t = np.arange(128)[None, :]
    out = np.zeros((128, 5, 128), np.float32)
    for dl in range(5):
        idx = np.clip(128 * dl + t - s, -256, 256) + 256
        blk = rel_bias_h[idx].astype(np.float32)
        if dl == 0:
            blk = np.where((t < 64) & (s >= 64), np.float32(-30000.0), blk)
        if dl == 4:
            blk = np.where((t >= 64) & (s < 64), np.float32(-30000.0), blk)
        out[:, dl, :] = blk
    return out.reshape(128, 640)

import contextlib
import numpy as np

S = 8192
HD = 128
EPS = 1e-6
NIT = 20
IDX_SCALE = (16 ** -0.5) * (64 ** -0.5)
TOPK = 256
NEG = -30000.0
C_P128, C_P64, C_ID, C_PAD, C_DIAG, C_POW = 0, 128, 256, 384, 1408, 1536
CW = 1536 + 32


def build_dsa(NSLOT=8):
    nc = bass.Bass("TRN2", target_bir_lowering=False)
    qa = nc.dram_tensor("qa", [12 * 128, 1024], BF16, kind="ExternalInput").ap()
    iq = nc.dram_tensor("iq", [8 * 128, 1024], BF16, kind="ExternalInput").ap()
    iw = nc.dram_tensor("iw", [1024, 16], BF16, kind="ExternalInput").ap()
    ka = nc.dram_tensor("ka", [2 * 128, S], BF16, kind="ExternalInput").ap()
    va = nc.dram_tensor("va", [S, 256], BF16, kind="ExternalInput").ap()
    ik = nc.dram_tensor("ik", [64, S], BF16, kind="ExternalInput").ap()
    tq = nc.dram_tensor("tq", [128, 2 * 1024], F32, kind="ExternalInput").ap()
    tiq = nc.dram_tensor("tiq", [128, 2 * 1024], F32, kind="ExternalInput").ap()
    tk = nc.dram_tensor("tk", [128, 2 * S], F32, kind="ExternalInput").ap()
    tik = nc.dram_tensor("tik", [128, 2 * S], F32, kind="ExternalInput").ap()
    cst = nc.dram_tensor("cst", [128, CW], F32, kind="ExternalInput").ap()
    hg = nc.dram_tensor("hg", [128, 12 * 128], F32, kind="ExternalInput").ap()
    oo = nc.dram_tensor("oo", [1024, 12 * 128], BF16, kind="ExternalOutput").ap()
    qav = qa.rearrange("(h p) t -> p h t", p=128)
    iqv = iq.rearrange("(h p) t -> p h t", p=128)
    with contextlib.ExitStack() as st:
        sb = lambda n, s, d: st.enter_context(nc.sbuf_tensor(n, s, d))
        csb = sb("csb", [128, CW], F32)
        pl128 = sb("pl128", [128, 128], BF16)
        pl64 = sb("pl64", [128, 128], BF16)
        ident3 = sb("ident3", [128, 384], BF16)
        hg_sb = sb("hg_sb", [128, 1536], F32)
        kaT = [sb("kaT%d" % g, [128, S], BF16) for g in range(2)]
        ikT = sb("ikT", [128, S], BF16)
        va_sb = sb("va_sb", [128, 64, 2, 129], BF16)
        score = sb("score", [128, S], F32)
        Mbs = [sb("Mb%d" % i, [128, S], BF16) for i in range(2)]
        qa_ss = [sb("qa_s%d" % i, [128, 12 * 128], BF16) for i in range(3)]
        iq_s = sb("iq_s", [128, 8 * 128], BF16)
        iw_b = sb("iw_b", [128, 16], BF16)
        iw_s = sb("iw_s", [128, 16], F32)
        dg = sb("dg", [128, 16 * 128], BF16)
        tqs = sb("tqs", [128, 2, 128], F32)
        tiqs = sb("tiqs", [128, 2, 128], F32)
        cosb = [score[:, 512 * i:512 * i + 512] for i in range(2)]
        sinb = [score[:, 512 * (2 + i):512 * (2 + i) + 512] for i in range(2)]
        t1 = [score[:, 512 * (4 + i):512 * (4 + i) + 512] for i in range(2)]
        t2 = [score[:, 512 * (6 + i):512 * (6 + i) + 512] for i in range(2)]
        t1s = [sb("t1s%d" % i, [128, 128], F32) for i in range(2)]
        t2s = [sb("t2s%d" % i, [128, 128], F32) for i in range(2)]
        R = [sb("R%d" % i, [128, 512], BF16) for i in range(4)]
        pT = [sb("pT%d" % i, [128, 384], BF16) for i in range(3)]
        Rm = sb("Rm", [128, 1], F32)
        steps = sb("steps", [128, 32], F32)
        cand = sb("cand", [128, 1], F32)
        cnt = sb("cnt", [128, 1], F32)
        sB = sb("sB", [128, 1], F32)
        tt = sb("tt", [128, 1], F32)
        thr = sb("thr", [128, 1], F32)
        rd = [sb("rd%d" % i, [128, 1], F32) for i in range(3)]
        ss = [sb("ss%d" % i, [128, 1], F32) for i in range(3)]
        o1 = [sb("o1%d" % i, [128, 128], F32) for i in range(3)]
        junk = sb("junk", [128, 128], F32)
        OO = [sb("OO%d" % i, [128, 128], BF16) for i in range(3)]
        rp = st.enter_context(nc.psum_tensor("rp", [128, 512], F32))
        dt = [st.enter_context(nc.psum_tensor("dt%d" % i, [128, 512], F32)) for i in range(4)]
        ac = [st.enter_context(nc.psum_tensor("ac%d" % i, [128, 512], F32)) for i in range(3)]
        P = Prog(nc)
        P.dma("sp", csb[:], cst, writes=["csb"])
        P.dma("sp", hg_sb[:], hg, writes=["hg"])
        P.op("dve", lambda e: e.tensor_copy(out=pl128[:], in_=csb[:, C_P128:C_P128 + 128]), reads=["csb"], writes=["pl128"])
        P.op("dve", lambda e: e.tensor_copy(out=pl64[:], in_=csb[:, C_P64:C_P64 + 128]), reads=["csb"], writes=["pl64"])
        for r in range(3):
            P.op("dve", lambda e, r=r: e.tensor_copy(out=ident3[:, 128 * r:128 * r + 128], in_=csb[:, C_ID:C_ID + 128]),
                 reads=["csb"], writes=[("ident3", r)])
        id3r = [("ident3", r) for r in range(3)]
        identf = csb[:, C_ID:C_ID + 128]
        for g in range(2):
            for h in range(2):
                c0 = h * (S // 2)
                P.dma("sp", kaT[g][:, c0:c0 + S // 2], ka[g * 128:(g + 1) * 128, c0:c0 + S // 2], writes=[("ka", g, c0 // 512 + b) for b in range(8)])
        for h in range(2):
            c0 = h * (S // 2)
            P.dma("sp", ikT[0:64, c0:c0 + S // 2], ik[:, c0:c0 + S // 2], writes=[("ikl", c0 // 512 + b) for b in range(8)])
            P.dma("sp", ikT[64:128, c0:c0 + S // 2], ik[:, c0:c0 + S // 2], writes=[("ikh", c0 // 512 + b) for b in range(8)])
            for g in range(2):
                P.dma("sp", va_sb[:, h * 32:(h + 1) * 32, g, 0:128],
                      va[h * 32 * 128:(h + 1) * 32 * 128, 128 * g:128 * g + 128].rearrange("(t p) d -> p t d", p=128), writes=[("va", h, g)])
        P.op("dve", lambda e: e.memset(va_sb[:, :, :, 128:129], 1.0), writes=["vone"])
        vreads = [("va", h, g) for h in range(2) for g in range(2)] + ["vone"]
        rn = [0]

        def rope(xap, keys, pl, plkey, cos_ap, sin_ap, tabkeys, n, small=False):
            b = rn[0] % 2
            rn[0] += 1
            if small:
                ta, tb_, ka_, kb_ = t1s[b][:, 0:n], t2s[b][:, 0:n], ("t1s", b), ("t2s", b)
            else:
                ta, tb_, ka_, kb_ = t1[b][:, 0:n], t2[b][:, 0:n], ("score", 4 + b), ("score", 6 + b)
            P.op("pe", lambda e: e.matmul(rp[:, 0:n], lhsT=pl[:], rhs=xap, start=True, stop=True),
                 reads=keys + [plkey], writes=["rp"])
            P.op("pool", lambda e: e.tensor_tensor(out=ta, in0=xap, in1=cos_ap, op=ALU.mult),
                 reads=keys + tabkeys, writes=[ka_])
            P.op("dve", lambda e: e.tensor_tensor(out=tb_, in0=rp[:, 0:n], in1=sin_ap, op=ALU.mult),
                 reads=["rp"] + tabkeys, writes=[kb_])
            P.op("pool", lambda e: e.tensor_tensor(out=xap, in0=ta, in1=tb_, op=ALU.add),
                 reads=[ka_, kb_], writes=keys)

        tn = 0
        for blk in range(15, -1, -1):
            c0 = blk * 512
            tb = tn % 2
            tn += 1
            P.dma("sp", cosb[tb], tk[:, c0:c0 + 512], writes=[("score", tb)])
            P.dma("sp", sinb[tb], tk[:, S + c0:S + c0 + 512], writes=[("score", 2 + tb)])
            for g in range(2):
                rope(kaT[g][:, c0:c0 + 512], [("ka", g, blk)], pl128, "pl128", cosb[tb], sinb[tb],
                     [("score", tb), ("score", 2 + tb)], 512)
            tb = tn % 2
            tn += 1
            P.dma("sp", cosb[tb], tik[:, c0:c0 + 512], writes=[("score", tb)])
            P.dma("sp", sinb[tb], tik[:, S + c0:S + c0 + 512], writes=[("score", 2 + tb)])
            rope(ikT[:, c0:c0 + 512], [("ikl", blk), ("ikh", blk)], pl64, "pl64", cosb[tb], sinb[tb],
                 [("score", tb), ("score", 2 + tb)], 512)
        kareads = lambda g, c0, n: [("ka", g, b) for b in range(c0 // 512, (c0 + n - 1) // 512 + 1)]
        ikreads = lambda c0: [("ikl", c0 // 512), ("ikh", c0 // 512)]
        outs = []
        dnc = [0]

        def setup(i):
            qa_s = qa_ss[i % 3]
            qk_ = lambda h: ("qa_s", i % 3, h)
            P.dma("sp", qa_s[:].rearrange("p (h t) -> p h t", h=12), qav[:, :, 128 * i:128 * i + 128], writes=[qk_(h) for h in range(12)])
            P.dma("sp", iq_s[:].rearrange("p (h t) -> p h t", h=8), iqv[:, :, 128 * i:128 * i + 128], writes=[("iq_s", h) for h in range(8)])
            P.dma("sp", iw_b[:], iw[128 * i:128 * i + 128, :], writes=["iw_b"])
            P.dma("sp", tqs[:], tq.rearrange("p (a t) -> p a t", a=2)[:, :, 128 * i:128 * i + 128], writes=["tqs"])
            P.dma("sp", tiqs[:], tiq.rearrange("p (a t) -> p a t", a=2)[:, :, 128 * i:128 * i + 128], writes=["tiqs"])
            P.op("dve", lambda e: e.tensor_copy(out=iw_s[:], in_=iw_b[:]), reads=["iw_b"], writes=["iw_s"])
            for h in range(8):
                rope(iq_s[:, 128 * h:128 * h + 128], [("iq_s", h)], pl64, "pl64", tiqs[:, 0, :], tiqs[:, 1, :], ["tiqs"], 128, small=True)
            for h in range(16):
                P.op("dve", lambda e, h=h: e.tensor_scalar(out=dg[:, 128 * h:128 * h + 128], in0=identf, scalar1=iw_s[:, h:h + 1], scalar2=None, op0=ALU.mult),
                     reads=["csb", "iw_s"], writes=[("dg", h)])
            for h in range(12):
                rope(qa_s[:, 128 * h:128 * h + 128], [qk_(h)], pl128, "pl128", tqs[:, 0, :], tqs[:, 1, :], ["tqs"], 128, small=True)

        def indexer(i):
            r0 = 56 - 8 * i
            idx_items = [(b, h) for b in range(2 * i + 2) for h in range(16)]
            dn0 = dnc[0]

            def idx_dots(k):
                b, h = idx_items[k]
                c0 = 128 * r0 + 512 * b
                db = (dn0 + k) % 4
                hp, pair = h % 2, h // 2
                P.op("pe", lambda e: e.matmul(dt[db][:], lhsT=iq_s[64 * hp:64 * hp + 64, 128 * pair:128 * pair + 128],
                                              rhs=ikT[64 * hp:64 * hp + 64, c0:c0 + 512], start=True, stop=True),
                     reads=[("iq_s", pair)] + ikreads(c0), writes=[("dt", db)])

            def idx_relu(k):
                db = (dn0 + k) % 4
                if k % 3 == 2:
                    P.op("dve", lambda e: e.tensor_scalar(out=R[db][:], in0=dt[db][:], scalar1=0.0, scalar2=None, op0=ALU.max),
                         reads=[("dt", db)], writes=[("R", db)])
                else:
                    P.op("act", lambda e: e.activation(out=R[db][:], in_=dt[db][:], func=AF.Relu),
                         reads=[("dt", db)], writes=[("R", db)])

            def idx_acc(k):
                b, h = idx_items[k]
                db = (dn0 + k) % 4
                ab = b % 2
                P.op("pe", lambda e: e.matmul(ac[ab][:], lhsT=dg[:, 128 * h:128 * h + 128], rhs=R[db][:], start=(h == 0), stop=(h == 15)),
                     reads=[("R", db), ("dg", h)], writes=[("ac", ab)])
                if h == 15:
                    P.op("dve", lambda e: e.tensor_scalar(out=score[:, 512 * b:512 * b + 512], in0=ac[ab][:], scalar1=IDX_SCALE, scalar2=None, op0=ALU.mult),
                         reads=[("ac", ab)], writes=[("score", b)])
            NI = len(idx_items)
            for k in range(0, NI + 2, 2):
                for kk in (k, k + 1):
                    if kk < NI:
                        idx_dots(kk)
                for kk in (k, k + 1):
                    if kk < NI:
                        idx_relu(kk)
                for kk in (k - 2, k - 1):
                    if 0 <= kk < NI:
                        idx_acc(kk)
            dnc[0] += NI

        def bisect(i):
            L = 1024 * (i + 1)
            Mb = Mbs[i % 2]
            mk = ("Mb", i % 2)
            sreads = [("score", b) for b in range(2 * i + 2)]
            P.op("dve", lambda e: e.tensor_reduce(out=Rm[:], in_=score[:, 0:L], axis=AX.X, op=ALU.max, apply_absolute_value=True),
                 reads=sreads, writes=["Rm"])
            P.op("dve", lambda e: e.tensor_scalar(out=Rm[:], in0=Rm[:], scalar1=1.001, scalar2=1e-6, op0=ALU.mult, op1=ALU.add),
                 reads=["Rm"], writes=["Rm"])
            P.op("dve", lambda e: e.tensor_scalar(out=steps[:, 0:32], in0=csb[:, C_POW:C_POW + 32], scalar1=Rm[:, 0:1], scalar2=None, op0=ALU.mult),
                 reads=["Rm", "csb"], writes=["steps"])
            P.op("dve", lambda e: e.tensor_tensor(out=score[:, L - 1024:L], in0=score[:, L - 1024:L], in1=csb[:, C_PAD:C_PAD + 1024], op=ALU.add),
                 reads=sreads + ["csb"], writes=[("score", 2 * i), ("score", 2 * i + 1)])
            P.op("dve", lambda e: e.tensor_tensor(out=score[:, 0:128], in0=score[:, 0:128], in1=csb[:, C_DIAG:C_DIAG + 128], op=ALU.add),
                 reads=[("score", 0), "csb"], writes=[("score", 0)])
            P.op("dve", lambda e: e.memset(cand[:], 0.0), writes=["cand"])
            Lh = L // 2
            yield
            for k in range(1, NIT + 1):
                P.op("dve", lambda e: e.tensor_scalar(out=Mb[:, 0:Lh], in0=score[:, 0:Lh], scalar1=cand[:, 0:1], scalar2=None,
                                                      op0=ALU.is_ge, op1=ALU.add, accum_out=cnt[:]),
                     reads=sreads + ["cand"], writes=[(mk, "A"), "cnt"])
                P.op("act", lambda e: e.activation(out=Mb[:, Lh:L], in_=score[:, Lh:L], func=AF.Sign, scale=-1.0, bias=cand[:, 0:1], accum_out=sB[:]),
                     reads=sreads + ["cand"], writes=[(mk, "B"), "sB"])
                P.op("dve", lambda e: e.scalar_tensor_tensor(out=cnt[:], in0=sB[:], scalar=-0.5, in1=cnt[:], op0=ALU.mult, op1=ALU.add),
                     reads=["sB", "cnt"], writes=["cnt"])
                P.op("dve", lambda e, k=k: e.tensor_scalar(out=tt[:], in0=cnt[:], scalar1=TOPK - 0.5 - Lh / 2.0, scalar2=steps[:, k - 1:k],
                                                           op0=ALU.is_ge, op1=ALU.mult),
                     reads=["cnt", "steps"], writes=["tt"])
                P.op("dve", lambda e, k=k: e.scalar_tensor_tensor(out=cand[:], in0=tt[:], scalar=steps[:, k:k + 1], in1=cand[:],
                                                                  op0=ALU.subtract, op1=ALU.add),
                     reads=["tt", "steps", "cand"], writes=["cand"])
                yield
            P.op("dve", lambda e: e.tensor_tensor(out=thr[:], in0=cand[:], in1=steps[:, NIT:NIT + 1], op=ALU.subtract),
                 reads=["cand", "steps"], writes=["thr"])
            P.op("dve", lambda e: e.tensor_scalar(out=Mb[:, 0:L], in0=score[:, 0:L], scalar1=thr[:, 0:1], scalar2=NEG,
                                                  op0=ALU.is_lt, op1=ALU.mult),
                 reads=sreads + ["thr"], writes=[(mk, "A"), (mk, "B")])
            yield

        def attend(i):
            r0 = 56 - 8 * i
            Mb = Mbs[i % 2]
            mk = ("Mb", i % 2)
            qa_s = qa_ss[i % 3]
            nst = 8 * i + 8
            for g in range(2):
                for hh in range(2):
                    h0 = 6 * g + 3 * hh
                    dn0 = dnc[0]

                    def a_qk(stl, g=g, h0=h0, dn0=dn0):
                        rr = r0 + stl
                        kc0 = 128 * rr
                        db = (dn0 + stl) % 3

                        def qk(e):
                            e.matmul(dt[db][:, 0:384], lhsT=kaT[g][:, kc0:kc0 + 128], rhs=qa_s[:, 128 * h0:128 * h0 + 384], start=True, stop=False)
                            return e.matmul(dt[db][:, 0:384], lhsT=Mb[:, 128 * stl:128 * stl + 128], rhs=ident3[:], start=False, stop=True)
                        P.op("pe", qk, reads=kareads(g, kc0, 128) + [("qa_s", i % 3, h0 + x) for x in range(3)] + [(mk, "A"), (mk, "B")] + id3r, writes=[("dt", db)])
                        P.op("act", lambda e: e.activation(out=pT[db][:], in_=dt[db][:, 0:384], func=AF.Exp),
                             reads=[("dt", db)], writes=[("pT", db)])

                    def a_pv(stl, g=g, dn0=dn0):
                        rr = r0 + stl
                        db = (dn0 + stl) % 3

                        def pv(e):
                            for hl in range(3):
                                ins = e.matmul(ac[hl][:, 0:129], lhsT=pT[db][:, 128 * hl:128 * hl + 128], rhs=va_sb[:, rr, g, :],
                                               start=(stl == 0), stop=(stl == nst - 1))
                            return ins
                        P.op("pe", pv, reads=[("pT", db)] + vreads, writes=[("ac", 0), ("ac", 1), ("ac", 2)])
                    for k in range(nst + 2):
                        if k < nst:
                            a_qk(k)
                        if k - 2 >= 0:
                            a_pv(k - 2)
                        yield
                    dnc[0] += nst
                    for hl in range(3):
                        hd = h0 + hl
                        P.op("dve", lambda e, hl=hl: e.reciprocal(out=rd[hl][:], in_=ac[hl][:, 128:129]), reads=[("ac", hl)], writes=[("rd", hl)])
                        P.op("dve", lambda e, hl=hl: e.tensor_scalar(out=o1[hl][:], in0=ac[hl][:, 0:128], scalar1=rd[hl][:, 0:1], scalar2=None, op0=ALU.mult),
                             reads=[("ac", hl), ("rd", hl)], writes=[("o1", hl)])
                        P.op("act", lambda e, hl=hl: e.activation(out=junk[:], in_=o1[hl][:], func=AF.Square, accum_out=ss[hl][:]),
                             reads=[("o1", hl)], writes=["junk", ("ss", hl)])
                        P.op("act", lambda e, hl=hl: e.activation(out=ss[hl][:], in_=ss[hl][:], func=AF.Ln, scale=1.0 / HD, bias=EPS),
                             reads=[("ss", hl)], writes=[("ss", hl)])
                        P.op("act", lambda e, hl=hl: e.activation(out=ss[hl][:], in_=ss[hl][:], func=AF.Exp, scale=-0.5),
                             reads=[("ss", hl)], writes=[("ss", hl)])
                        P.op("dve", lambda e, hl=hl, hd=hd: e.scalar_tensor_tensor(out=OO[hl][:], in0=o1[hl][:], scalar=ss[hl][:, 0:1],
                                                                                    in1=hg_sb[:, 128 * hd:128 * hd + 128], op0=ALU.mult, op1=ALU.mult),
                             reads=[("o1", hl), ("ss", hl), "hg"], writes=[("OO", hl)])
                        outs.append(P.dma("sp", oo[128 * i:128 * i + 128, 128 * hd:128 * hd + 128], OO[hl][:], reads=[("OO", hl)],
                                          writes=[("oo", i, hd)]))

        def run_all(gen):
            for _ in gen:
                pass

        setup(0)
        for i in range(NSLOT):
            indexer(i)
            if i + 1 < NSLOT:
                setup(i + 1)
            if i == 0:
                run_all(bisect(0))
                continue
            gb = bisect(i)
            ga = attend(i - 1)
            n_att = 4 * (8 * (i - 1) + 8 + 2)
            per = max(1, (n_att + NIT) // (NIT + 1))
            done_a = False
            for _ in gb:
                if not done_a:
                    for _j in range(per):
                        try:
                            next(ga)
                        except StopIteration:
                            done_a = True
                            break
            if not done_a:
                run_all(ga)
        run_all(attend(NSLOT - 1))
        P.emit(final_wait_ops=outs)
    return nc


def rope_tables(pos, d):
    inv = (np.float32(10000.0) ** (-np.arange(0, d, 2, dtype=np.float32) / np.float32(d))).astype(np.float32)
    ang = (pos.astype(np.float32)[:, None] * inv[None, :]).astype(np.float32)
    return np.cos(ang).astype(np.float32).T, np.sin(ang).astype(np.float32).T


def dsa_consts(c):
    cst = np.zeros((128, CW), np.float32)
    k = np.arange(128)
    for kk in range(128):
        if kk >= 64:
            cst[kk, C_P128 + kk - 64] = -1.0
        else:
            cst[kk, C_P128 + kk + 64] = 1.0
        if kk % 64 >= 32:
            cst[kk, C_P64 + kk - 32] = -1.0
        else:
            cst[kk, C_P64 + kk + 32] = 1.0
    cst[:, C_ID:C_ID + 128] = np.eye(128, dtype=np.float32)
    col = np.arange(1024)[None, :]
    cst[:, C_PAD:C_PAD + 1024] = np.where(col >= 128 * (c + 1), np.float32(-1e30), np.float32(0.0))
    tl = np.arange(128)[:, None]
    cl = np.arange(128)[None, :]
    cst[:, C_DIAG:C_DIAG + 128] = np.where((tl < 64) & (cl < 64), np.float32(-1e30), np.float32(0.0))
    cst[:, C_POW:C_POW + 32] = (2.0 ** -np.arange(32, dtype=np.float64)).astype(np.float32)[None, :]
    return cst


def dsa_core_inputs(c, pT, hgain12):
    tiles = [c + 8 * i for i in range(8)]
    tok = np.concatenate([np.arange(128 * j, 128 * j + 128) for j in tiles])
    rprime = np.arange(64)
    ktile = 63 - (rprime + 7 - c)
    key = (128 * ktile[:, None] + (127 - np.arange(128))[None, :]).reshape(-1)
    valid = key >= 0
    keyc = np.where(valid, key, 0)

    def stream(rows):
        out = rows[:, keyc].copy()
        out[:, ~valid] = 0
        return out
    qa = np.ascontiguousarray(pT[0:1536][:, tok])
    iq = np.ascontiguousarray(pT[2048:3072][:, tok])
    iw = np.ascontiguousarray(pT[3136:3152][:, tok].T)
    ka = stream(pT[1536:1792])
    va = np.ascontiguousarray(stream(pT[1792:2048]).T)
    ik = stream(pT[3072:3136])
    sc = np.float32(HD ** -0.5)
    cq, sq = rope_tables(tok.astype(np.float32), 128)
    tq = np.concatenate([np.concatenate([cq, cq], 0) * sc, np.concatenate([sq, sq], 0) * sc], axis=1).astype(np.float32)
    ci, si = rope_tables(tok.astype(np.float32), 64)
    tiq = np.concatenate([np.concatenate([ci, ci, ci, ci], 0), np.concatenate([si, si, si, si], 0)], axis=1).astype(np.float32)
    ck, sk = rope_tables(keyc.astype(np.float32), 128)
    tk = np.concatenate([np.concatenate([ck, ck], 0), np.concatenate([sk, sk], 0)], axis=1).astype(np.float32)
    cik, sik = rope_tables(keyc.astype(np.float32), 64)
    tik = np.concatenate([np.concatenate([cik] * 4, 0), np.concatenate([sik] * 4, 0)], axis=1).astype(np.float32)
    hgb = np.ascontiguousarray(np.broadcast_to(hgain12[None, :], (128, 1536))).astype(np.float32)
    return {"qa": qa, "iq": iq, "iw": iw, "ka": ka, "va": va, "ik": ik, "tq": tq, "tiq": tiq, "tk": tk, "tik": tik,
            "cst": dsa_consts(c), "hg": hgb}, tok


NCORES = 8
CORES = list(range(NCORES))
DFF = 11008
DFFP = 11264
HC = DFFP // NCORES
PW = 10832
PWC = PW // NCORES
ADA_NC = 3072

_PROGS = {}


def _prog(name, fn):
    if name not in _PROGS:
        _PROGS[name] = fn()
    return _PROGS[name]


def _run(nc, in_maps):
    res = run_bass_kernel_spmd(nc, in_maps, core_ids=CORES)
    return res.results


def _vecl(v):
    return np.ascontiguousarray(np.asarray(v, np.float32).reshape(-1, 128).T)


def kernel(x, c, w_ada, b_ada, norm_attn_g, w_in, rel_bias, head_norm_g, w_out,
           norm_ffn_g, w_gate_up, w_down, final_norm_g):
    x = np.asarray(x); c = np.asarray(c)
    depth = w_ada.shape[0]
    nc0 = _prog("k0", build_k0)
    cT = np.ascontiguousarray(np.asarray(c[0], np.float32).reshape(32, 128).T)
    ims = []
    for i in range(NCORES):
        sl = slice(i * ADA_NC, (i + 1) * ADA_NC)
        ims.append({"cT": cT, "w": np.ascontiguousarray(np.asarray(w_ada)[:, :, sl].reshape(depth * D, ADA_NC)),
                    "b": np.ascontiguousarray(np.asarray(b_ada)[:, sl].reshape(1, depth * ADA_NC))})
    r = _run(nc0, ims)
    mod = np.concatenate([r[i]["y"].reshape(depth, ADA_NC) for i in range(NCORES)], axis=1)
    del ims
    xT = np.ascontiguousarray(np.asarray(x[0], np.float32).T)

    def norm_phase(xT, g, sc, sh, final=False):
        ncn = _prog("normF" if final else "norm", lambda: build_norm(final))
        ims = [{"xT": np.ascontiguousarray(xT[:, i * 1024:(i + 1) * 1024]), "gv": _vecl(g), "scv": _vecl(sc), "shv": _vecl(sh)}
               for i in range(NCORES)]
        r = _run(ncn, ims)
        return np.concatenate([r[i]["hT"] for i in range(NCORES)], axis=1)

    for l in range(depth):
        sh1, sc1, g1, sh2, sc2, g2 = np.split(mod[l], 6)
        hg = np.asarray(head_norm_g[l], np.float32)
        hT = norm_phase(xT, norm_attn_g[l], sc1, sh1)
        ncg = _prog("pr", lambda: build_gemm("raw", D, PWC))
        wl = np.asarray(w_in[l])
        r = _run(ncg, [{"aT": hT, "W": np.ascontiguousarray(wl[:, i * PWC:(i + 1) * PWC])} for i in range(NCORES)])
        pT = np.concatenate([r[i]["yT"] for i in range(NCORES)], axis=0)
        del hT, r
        mix = np.zeros((S, D), dtype=pT.dtype)
        nca = _prog("dsa", build_dsa)
        ims, toks = [], []
        for i in range(NCORES):
            im, tok = dsa_core_inputs(i, pT, hg[:1536])
            ims.append(im); toks.append(tok)
        r = _run(nca, ims)
        for i in range(NCORES):
            mix[toks[i], 0:1536] = r[i]["oo"]
        del ims, r
        ncs = _prog("sb", build_sb)
        cst = sb_consts()
        ims = []
        for i in range(NCORES):
            qs, ks, vs, hgs = [], [], [], []
            for u in range(3):
                n = 3 * i + u
                h, par = n // 2, n % 2
                q, k, v = sb_unit_inputs(pT[3152 + 128 * h:3152 + 128 * h + 128], pT[4688 + 128 * h:4688 + 128 * h + 128],
                                         pT[6224 + 128 * h:6224 + 128 * h + 128], par)
                qs.append(q); ks.append(k); vs.append(v)
                hgs.append(np.broadcast_to(hg[(12 + h) * 128:(13 + h) * 128][None, :], (128, 128)))
            ims.append({"qT": np.concatenate(qs, 0), "kT": np.concatenate(ks, 0), "vv": np.concatenate(vs, 0),
                        "hg": np.ascontiguousarray(np.concatenate(hgs, 0)).astype(np.float32), "cst": cst})
        r = _run(ncs, ims)
        for i in range(NCORES):
            o = r[i]["oo"].reshape(3, 32, 128, 128)
            for u in range(3):
                n = 3 * i + u
                h, par = n // 2, n % 2
                for p in range(32):
                    j = 2 * p + par
                    mix[128 * j:128 * j + 128, (12 + h) * 128:(13 + h) * 128] = o[u, p]
        del ims, r
        ncc = _prog("chunk", build_chunk)
        idn = np.eye(128, dtype=np.float32)
        ims = []
        for h in range(NCORES):
            ims.append({"qT": np.ascontiguousarray(pT[7760 + 128 * h:7760 + 128 * h + 128]),
                        "kT": np.ascontiguousarray(pT[8784 + 128 * h:8784 + 128 * h + 128]),
                        "vv": np.ascontiguousarray(pT[9808 + 128 * h:9808 + 128 * h + 128].T),
                        "bT": chunk_bias(np.asarray(rel_bias[l][h], np.float32)), "idn": idn,
                        "hg": np.ascontiguousarray(np.broadcast_to(hg[(24 + h) * 128:(25 + h) * 128][None, :], (128, 128))).astype(np.float32)})
        r = _run(ncc, ims)
        for h in range(NCORES):
            mix[:, (24 + h) * 128:(25 + h) * 128] = r[h]["oo"]
        del ims, r, pT
        mixT = np.ascontiguousarray(mix.T)
        del mix
        nco = _prog("op", lambda: build_gemm("res", D, 512))
        wl = np.asarray(w_out[l])
        r = _run(nco, [{"aT": mixT, "W": np.ascontiguousarray(wl[:, i * 512:(i + 1) * 512]),
                        "xT": np.ascontiguousarray(xT[i * 512:(i + 1) * 512]), "gvec": _vecl(g1[i * 512:(i + 1) * 512])}
                       for i in range(NCORES)])
        xT = np.concatenate([r[i]["yT"] for i in range(NCORES)], axis=0)
        del mixT, r
        h2T = norm_phase(xT, norm_ffn_g[l], sc2, sh2)
        ncu = _prog("gu", lambda: build_gemm("glu", D, HC))
        wgu = np.asarray(w_gate_up[l])
        ims = []
        for i in range(NCORES):
            W = np.zeros((D, 2 * HC), np.float32)
            lo, hi = i * HC, min(DFF, (i + 1) * HC)
            W[:, 0:hi - lo] = wgu[:, lo:hi]
            W[:, HC:HC + hi - lo] = wgu[:, DFF + lo:DFF + hi]
            ims.append({"aT": h2T, "W": W})
        r = _run(ncu, ims)
        actT = np.concatenate([r[i]["yT"] for i in range(NCORES)], axis=0)
        del ims, r, h2T
        ncd = _prog("dn", lambda: build_gemm("res", DFFP, 512))
        wd = np.zeros((DFFP, D), np.float32)
        wd[:DFF] = np.asarray(w_down[l])
        r = _run(ncd, [{"aT": actT, "W": np.ascontiguousarray(wd[:, i * 512:(i + 1) * 512]),
                        "xT": np.ascontiguousarray(xT[i * 512:(i + 1) * 512]), "gvec": _vecl(g2[i * 512:(i + 1) * 512])}
                       for i in range(NCORES)])
        xT = np.concatenate([r[i]["yT"] for i in range(NCORES)], axis=0)
        del actT, wd, r
    zero = np.zeros(D, np.float32)
    oT = norm_phase(xT, final_norm_g, zero, zero, final=True)
    return np.ascontiguousarray(oT.T)[None].astype(np.float32)
```
